# Optimizing a Trainium2 kernel written in Bass

```python
import jax, jax.numpy as jnp
from jax import lax
import numpy as np

D_MODEL = 1024
BATCH = 32
SEQ = 256
DEPTH = 1
DEC_BATCH = 8
DEC_SEQ = 4096
PAST_LEN = 512

GRID_W = 64
H_MLA = 8
D_NOPE = 64
D_ROPE = 32
D_QK = D_NOPE + D_ROPE
D_V = 64
Q_RANK = 256
KV_RANK = 128
H_RET = 8
D_RET = 64
RET_CHUNK = 128
D_MIX = H_MLA * D_V + H_RET * D_RET
D_RET_ALL = H_RET * D_RET
D_IN = Q_RANK + KV_RANK + D_ROPE + 4 * D_RET_ALL
SPLITS = (Q_RANK, Q_RANK + KV_RANK, Q_RANK + KV_RANK + D_ROPE,
          Q_RANK + KV_RANK + D_ROPE + D_RET_ALL,
          Q_RANK + KV_RANK + D_ROPE + 2 * D_RET_ALL,
          Q_RANK + KV_RANK + D_ROPE + 3 * D_RET_ALL)
N_EXPERTS = 64
TOP_K = 6
D_EXPERT = 256
D_SHARED = 256
ROUTED_SCALE = 2.5
ROPE_BASE = 10000.0
Q_BLOCK = 128
EPS = 1e-6

kernel_name = 'hybrid_mla_retention_moe_diffusion_step'


def rms_norm(x, g):
    xf = x.astype(jnp.float32)
    y = xf * lax.rsqrt(jnp.mean(xf * xf, axis=-1, keepdims=True) + EPS)
    return (y * g.astype(jnp.float32)).astype(x.dtype)


def rotate_axis(x, pos):
    p = x.shape[-1] // 2
    inv = 1.0 / (ROPE_BASE ** (jnp.arange(p, dtype=jnp.float32) / p))
    ang = pos.astype(jnp.float32)[:, None] * inv[None, :]
    cos, sin = jnp.cos(ang), jnp.sin(ang)
    xf = x.astype(jnp.float32)
    x1, x2 = xf[..., :p], xf[..., p:]
    return jnp.concatenate([x1 * cos - x2 * sin, x1 * sin + x2 * cos], axis=-1).astype(x.dtype)


def rope_2d(x, row, col):
    half = x.shape[-1] // 2
    return jnp.concatenate([rotate_axis(x[..., :half], row), rotate_axis(x[..., half:], col)], axis=-1)


def rope_on_rope_dims(t, row, col):
    return jnp.concatenate([t[..., :D_NOPE], rope_2d(t[..., D_NOPE:], row, col)], axis=-1)


def grid_positions(n_tokens):
    rows = n_tokens // GRID_W
    row = jnp.repeat(jnp.arange(rows, dtype=jnp.int32), GRID_W)
    col = jnp.tile(jnp.arange(GRID_W, dtype=jnp.int32), rows)
    return row, col


def adaln(cond, w_ada, b_ada):
    mod = jax.nn.silu(cond) @ w_ada + b_ada
    return [jnp.expand_dims(m, -2) for m in jnp.split(mod, 6, axis=-1)]


def split_heads(t, n_heads):
    B, L, _ = t.shape
    return t.reshape(B, L, n_heads, -1).transpose(0, 2, 1, 3)


def merge_heads(t):
    B, H, L, d = t.shape
    return t.transpose(0, 2, 1, 3).reshape(B, L, H * d)


def mla_queries(cq, q_norm_g, w_uq, q_gain):
    q = split_heads(rms_norm(cq, q_norm_g) @ w_uq, H_MLA)
    return rms_norm(q, q_gain)


def mla_keys_values(ckv_n, kr, w_ukv, k_gain):
    B, L, _ = ckv_n.shape
    kv = split_heads(ckv_n @ w_ukv, H_MLA)
    k_nope, v = kv[..., :D_NOPE], kv[..., D_NOPE:]
    k_rope = jnp.broadcast_to(kr[:, None], (B, H_MLA, L, D_ROPE))
    k = rms_norm(jnp.concatenate([k_nope, k_rope], axis=-1), k_gain)
    return k, v


def softmax_attention(q, k, v):
    B, H, Lq, dk = q.shape
    dv = v.shape[-1]
    nb = Lq // Q_BLOCK
    scale = dk ** -0.5
    kf, vf = k.astype(jnp.float32), v.astype(jnp.float32)
    qb = q.astype(jnp.float32).reshape(B, H, nb, Q_BLOCK, dk).transpose(2, 0, 1, 3, 4)

    def block(qblk):
        p = jax.nn.softmax(jnp.einsum('bhqd,bhkd->bhqk', qblk, kf) * scale, axis=-1)
        return jnp.einsum('bhqk,bhkd->bhqd', p, vf)

    o = lax.map(block, qb)
    return o.transpose(1, 2, 0, 3, 4).reshape(B, H, Lq, dv).astype(v.dtype)


def retention_chunked(q, k, v, log_gamma, s0, include_diag):
    B, H, L, dk = q.shape
    dv = v.shape[-1]
    C = RET_CHUNK
    n = L // C
    qc = q.astype(jnp.float32).reshape(B, H, n, C, dk)
    kc = k.astype(jnp.float32).reshape(B, H, n, C, dk)
    vc = v.astype(jnp.float32).reshape(B, H, n, C, dv)
    lg = log_gamma.astype(jnp.float32)
    idx = jnp.arange(C, dtype=jnp.float32)
    diff = idx[:, None] - idx[None, :]
    mask = (diff >= 0) if include_diag else (diff > 0)
    decay = jnp.where(mask[None], jnp.exp(lg[:, None, None] * jnp.where(mask, diff, 0.0)[None]), 0.0)
    scores = jnp.einsum('bhncd,bhnkd->bhnck', qc, kc) * decay[None, :, None]
    intra = jnp.einsum('bhnck,bhnke->bhnce', scores, vc)
    k_decay = jnp.exp(lg[:, None] * (C - 1 - idx)[None])
    kv_chunk = jnp.einsum('bhnkd,hk,bhnke->nbhde', kc, k_decay, vc)
    chunk_decay = jnp.exp(lg * C)[None, :, None, None]

    def step(S, kv):
        return chunk_decay * S + kv, S

    s_final, s_prev = lax.scan(step, s0.astype(jnp.float32), kv_chunk)
    q_decay = jnp.exp(lg[:, None] * (idx + 1)[None])
    cross = jnp.einsum('bhncd,hc,nbhde->bhnce', qc, q_decay, s_prev)
    return (intra + cross).reshape(B, H, L, dv), s_final


def bidir_retention(q, k, v, lg_f, lg_b, s0_f, s0_b):
    o_f, s_f = retention_chunked(q, k, v, lg_f, s0_f, True)
    o_b, s_b = retention_chunked(jnp.flip(q, 2), jnp.flip(k, 2), jnp.flip(v, 2), lg_b, s0_b, False)
    return (o_f + jnp.flip(o_b, 2)).astype(v.dtype), s_f, s_b


def moe_ffn(h, router_w, router_bias, w_gate, w_up, w_down, sh_gate, sh_up, sh_down):
    B, L, D = h.shape
    t = h.reshape(B * L, D)
    scores = jax.nn.sigmoid((t @ router_w).astype(jnp.float32))
    _, idx = lax.top_k(scores + router_bias.astype(jnp.float32), TOP_K)
    sel = jnp.take_along_axis(scores, idx, axis=-1)
    wts = ROUTED_SCALE * sel / jnp.sum(sel, axis=-1, keepdims=True)
    gates = jnp.einsum('tk,tke->et', wts, jax.nn.one_hot(idx, N_EXPERTS, dtype=jnp.float32)).astype(t.dtype)
    shared = (jax.nn.silu(t @ sh_gate) * (t @ sh_up)) @ sh_down

    def expert(acc, xs):
        wg, wu, wd, g = xs
        return acc + g[:, None] * ((jax.nn.silu(t @ wg) * (t @ wu)) @ wd), None

    y, _ = lax.scan(expert, shared, (w_gate, w_up, w_down, gates))
    return y.reshape(B, L, D)


def trunk_layer(x, cond, lp, ctx):
    B, L, _ = x.shape
    sh1, sc1, g1, sh2, sc2, g2 = adaln(cond, lp['w_ada'], lp['b_ada'])
    h = rms_norm(x, lp['norm1_g']) * (1.0 + sc1) + sh1
    z = h @ lp['w_in']
    cq, ckv, kr, rq, rk, rv, rg = jnp.split(z, SPLITS, axis=-1)
    ckv_n = rms_norm(ckv, lp['mla_kv_norm_g'])
    q = mla_queries(cq, lp['mla_q_norm_g'], lp['w_uq'], lp['mla_q_gain'])
    k, v = mla_keys_values(ckv_n, kr, lp['w_ukv'], lp['mla_k_gain'])
    rq = split_heads(rq, H_RET)
    rk = split_heads(rk, H_RET) * D_RET ** -0.5
    rv = split_heads(rv, H_RET)
    if ctx is None:
        s0_f = jnp.zeros((B, H_RET, D_RET, D_RET), jnp.float32)
        s0_b = jnp.zeros((B, H_RET, D_RET, D_RET), jnp.float32)
    else:
        ckv_c, kr_c, s0_f, s0_b = ctx
        row, col = grid_positions(L)
        q = rope_on_rope_dims(q, row, col)
        k = rope_on_rope_dims(k, row, col)
        rq = rope_2d(rq, row, col)
        rk = rope_2d(rk, row, col)
        k_c, v_c = mla_keys_values(ckv_c, kr_c, lp['w_ukv'], lp['mla_k_gain'])
        k = jnp.concatenate([k, k_c.astype(k.dtype)], axis=2)
        v = jnp.concatenate([v, v_c.astype(v.dtype)], axis=2)
    o_mla = merge_heads(softmax_attention(q, k, v))
    lg_f = -jnp.exp(lp['ret_decay_fwd'].astype(jnp.float32))
    lg_b = -jnp.exp(lp['ret_decay_bwd'].astype(jnp.float32))
    o_ret, s_f, s_b = bidir_retention(rq, rk, rv, lg_f, lg_b, s0_f, s0_b)
    o_ret = merge_heads(rms_norm(o_ret, lp['ret_norm_g'])) * jax.nn.silu(rg)
    x = x + g1 * (jnp.concatenate([o_mla, o_ret], axis=-1) @ lp['w_o'])
    h2 = rms_norm(x, lp['norm2_g']) * (1.0 + sc2) + sh2
    x = x + g2 * moe_ffn(h2, lp['router_w'], lp['router_bias'], lp['exp_w_gate'], lp['exp_w_up'],
                         lp['exp_w_down'], lp['sh_w_gate'], lp['sh_w_up'], lp['sh_w_down'])
    return x, (ckv_n, kr, s_f, s_b)


def setup_inputs(seed: int = 0) -> dict:
    key = jax.random.key(seed)
    ks = jax.random.split(key, 31)

    def nrm(k, shape, s=1.0):
        return s * jax.random.normal(k, shape, jnp.float32)

    decay_base = jnp.asarray(np.log(-np.log1p(-(2.0 ** (-5.0 - np.arange(H_RET))))), jnp.float32)
    d_in_s = D_MODEL ** -0.5
    return {
        'x_prompt': nrm(ks[0], (BATCH, SEQ, D_MODEL)),
        'x_sample': nrm(ks[1], (DEC_BATCH, DEC_SEQ, D_MODEL)),
        'cache_mla_ckv': nrm(ks[2], (DEC_BATCH, DEPTH, PAST_LEN, KV_RANK)),
        'cache_mla_krope': nrm(ks[3], (DEC_BATCH, DEPTH, PAST_LEN, D_ROPE)),
        'state_ret_fwd': nrm(ks[4], (DEC_BATCH, DEPTH, H_RET, D_RET, D_RET), 0.5),
        'state_ret_bwd': nrm(ks[5], (DEC_BATCH, DEPTH, H_RET, D_RET, D_RET), 0.5),
        'c': nrm(ks[6], (DEC_BATCH, D_MODEL)),
        'c_ctx': nrm(ks[7], (D_MODEL,)),
        'w_ada': nrm(ks[8], (DEPTH, D_MODEL, 6 * D_MODEL), 0.5 * d_in_s),
        'b_ada': nrm(ks[9], (DEPTH, 6 * D_MODEL), 0.02),
        'norm1_g': 1.0 + nrm(ks[10], (DEPTH, D_MODEL), 0.1),
        'w_in': nrm(ks[11], (DEPTH, D_MODEL, D_IN), d_in_s),
        'mla_q_norm_g': 1.0 + nrm(ks[12], (DEPTH, Q_RANK), 0.1),
        'w_uq': nrm(ks[13], (DEPTH, Q_RANK, H_MLA * D_QK), Q_RANK ** -0.5),
        'mla_kv_norm_g': 1.0 + nrm(ks[14], (DEPTH, KV_RANK), 0.1),
        'w_ukv': nrm(ks[15], (DEPTH, KV_RANK, H_MLA * (D_NOPE + D_V)), KV_RANK ** -0.5),
        'mla_q_gain': 1.0 + nrm(ks[16], (DEPTH, D_QK), 0.1),
        'mla_k_gain': 1.0 + nrm(ks[17], (DEPTH, D_QK), 0.1),
        'ret_decay_fwd': decay_base[None] + nrm(ks[18], (DEPTH, H_RET), 0.01),
        'ret_decay_bwd': decay_base[None] + nrm(ks[19], (DEPTH, H_RET), 0.01),
        'ret_norm_g': 1.0 + nrm(ks[20], (DEPTH, D_RET), 0.1),
        'w_o': nrm(ks[21], (DEPTH, D_MIX, D_MODEL), D_MIX ** -0.5),
        'norm2_g': 1.0 + nrm(ks[22], (DEPTH, D_MODEL), 0.1),
        'router_w': nrm(ks[23], (DEPTH, D_MODEL, N_EXPERTS), d_in_s),
        'router_bias': nrm(ks[24], (DEPTH, N_EXPERTS), 0.01),
        'exp_w_gate': nrm(ks[25], (DEPTH, N_EXPERTS, D_MODEL, D_EXPERT), d_in_s),
        'exp_w_up': nrm(ks[26], (DEPTH, N_EXPERTS, D_MODEL, D_EXPERT), d_in_s),
        'exp_w_down': nrm(ks[27], (DEPTH, N_EXPERTS, D_EXPERT, D_MODEL), D_EXPERT ** -0.5),
        'sh_w_gate': nrm(ks[28], (DEPTH, D_MODEL, D_SHARED), d_in_s),
        'sh_w_up': nrm(ks[29], (DEPTH, D_MODEL, D_SHARED), d_in_s),
        'sh_w_down': nrm(ks[30], (DEPTH, D_SHARED, D_MODEL), D_SHARED ** -0.5),
    }


def reference(x_prompt, x_sample, cache_mla_ckv, cache_mla_krope, state_ret_fwd, state_ret_bwd,
              c, c_ctx, w_ada, b_ada, norm1_g, w_in, mla_q_norm_g, w_uq, mla_kv_norm_g, w_ukv,
              mla_q_gain, mla_k_gain, ret_decay_fwd, ret_decay_bwd, ret_norm_g, w_o, norm2_g,
              router_w, router_bias, exp_w_gate, exp_w_up, exp_w_down, sh_w_gate, sh_w_up, sh_w_down):
    layers = [dict(w_ada=w_ada[l], b_ada=b_ada[l], norm1_g=norm1_g[l], w_in=w_in[l],
                   mla_q_norm_g=mla_q_norm_g[l], w_uq=w_uq[l], mla_kv_norm_g=mla_kv_norm_g[l],
                   w_ukv=w_ukv[l], mla_q_gain=mla_q_gain[l], mla_k_gain=mla_k_gain[l],
                   ret_decay_fwd=ret_decay_fwd[l], ret_decay_bwd=ret_decay_bwd[l],
                   ret_norm_g=ret_norm_g[l], w_o=w_o[l], norm2_g=norm2_g[l],
                   router_w=router_w[l], router_bias=router_bias[l], exp_w_gate=exp_w_gate[l],
                   exp_w_up=exp_w_up[l], exp_w_down=exp_w_down[l], sh_w_gate=sh_w_gate[l],
                   sh_w_up=sh_w_up[l], sh_w_down=sh_w_down[l])
              for l in range(DEPTH)]

    y_prompt = x_prompt
    ckv_list, kr_list, sf_list, sb_list = [], [], [], []
    for l in range(DEPTH):
        y_prompt, (ckv_l, kr_l, sf_l, sb_l) = trunk_layer(y_prompt, c_ctx, layers[l], None)
        ckv_list.append(ckv_l)
        kr_list.append(kr_l)
        sf_list.append(sf_l)
        sb_list.append(sb_l)

    y_sample = x_sample
    for l in range(DEPTH):
        ctx = (cache_mla_ckv[:, l], cache_mla_krope[:, l], state_ret_fwd[:, l], state_ret_bwd[:, l])
        y_sample, _ = trunk_layer(y_sample, c, layers[l], ctx)

    new_mla_ckv = jnp.stack(ckv_list, axis=1)
    new_mla_krope = jnp.stack(kr_list, axis=1)
    new_ret_fwd = jnp.stack(sf_list, axis=1)
    new_ret_bwd = jnp.stack(sb_list, axis=1)
    return (y_prompt, y_sample, new_mla_ckv, new_mla_krope, new_ret_fwd, new_ret_bwd)
```

```python
import contextlib
import numpy as np
import concourse.bass as bass
import concourse.mybir as mybir
from concourse.bass_utils import run_bass_kernel_spmd

F32 = mybir.dt.float32
BF16 = mybir.dt.bfloat16
I32 = mybir.dt.int32
AF = mybir.ActivationFunctionType
ALU = mybir.AluOpType
AX = mybir.AxisListType

NT = 40
NTS = 32
TOK = 5120
EPS = 1e-6
NKC = 44
NST = 304
NSLOT = NST * 128


class Buf:
    def __init__(self, t, name):
        self.t = t
        self.name = name
        self.wr = None
        self.rd = {}
        self.ds = {}


class Eng:
    def __init__(self, nc, name, e, self_sync=True):
        self.nc = nc
        self.name = name
        self.e = e
        self.self_sync = self_sync
        self.sem = nc.alloc_semaphore("s_" + name)
        self.cnt = 0
        self.seen = {}
        self.pend_r = []
        self.pend_w = []

    def wait(self, tk):
        if tk is None:
            return
        sem, val, key = tk
        if key == self.name and not self.self_sync:
            return
        if self.seen.get(key, 0) >= val:
            return
        self.e.wait_ge(sem, val)
        self.seen[key] = val


class KB:
    def __init__(self, nc):
        self.nc = nc
        self.PE = Eng(nc, "pe", nc.tensor, self_sync=False)
        self.ACT = Eng(nc, "act", nc.scalar)
        self.DVE = Eng(nc, "dve", nc.vector)
        self.POOL = Eng(nc, "pool", nc.gpsimd)
        self.SP = Eng(nc, "sp", nc.sync)
        self.engs = [self.PE, self.ACT, self.DVE, self.POOL, self.SP]
        self.dma_tk = {}
        self.bsem = nc.alloc_semaphore("s_bar")
        self.bcnt = 0
        self.nsem = 0
        self.uid = 0
        self.free_sems = {"hw": [], "sw": []}
        self.bg_tk = {}

    def op(self, E, fn, r=(), w=(), mark=True):
        for b in r:
            E.wait(b.wr)
        for b in w:
            E.wait(b.wr)
            for tk in list(b.rd.values()):
                E.wait(tk)
        inst = fn(E.e)
        if mark:
            E.cnt += 1
            inst.then_inc(E.sem, 1)
            tk = (E.sem, E.cnt, E.name)
            for b in E.pend_r:
                b.rd[E.name] = tk
            for b in E.pend_w:
                b.wr = tk
                b.rd = {}
            E.pend_r = []
            E.pend_w = []
            for b in r:
                b.rd[E.name] = tk
            for b in w:
                b.wr = tk
                b.rd = {}
        else:
            E.pend_r.extend(r)
            E.pend_w.extend(w)
        return inst

    def P(self, fn, r=(), w=(), mark=True):
        return self.op(self.PE, fn, r, w, mark)

    def A(self, fn, r=(), w=()):
        return self.op(self.ACT, fn, r, w)

    def V(self, fn, r=(), w=()):
        return self.op(self.DVE, fn, r, w)

    def G(self, fn, r=(), w=()):
        return self.op(self.POOL, fn, r, w)

    def _dma_common(self, Q, r, w, issue, bg=False):
        for b in r:
            Q.wait(b.wr)
        for b in w:
            Q.wait(b.wr)
            for tk in list(b.rd.values()):
                Q.wait(tk)
        prim = w[0] if len(w) else r[0]
        kind = "sw" if Q is self.POOL else "hw"
        if kind not in prim.ds:
            if kind == "hw" and self.free_sems[kind]:
                prim.ds[kind] = self.free_sems[kind].pop()
            else:
                prim.ds[kind] = [self.nc.alloc_semaphore("d%d" % self.nsem), "dsem_%d" % self.nsem, 0]
                self.nsem += 1
        ent = prim.ds[kind]
        inst = issue(Q.e)
        ent[2] += 16
        inst.then_inc(ent[0], 16)
        tk = (ent[0], ent[2], ent[1])
        (self.bg_tk if bg else self.dma_tk)[ent[1]] = tk
        for b in r:
            b.rd[ent[1]] = tk
        for b in w:
            b.wr = tk
            b.rd = {}
        return inst

    def dma(self, Q, out, in_, r=(), w=(), noncontig=False, bg=False):
        def issue(e):
            if noncontig:
                with self.nc.allow_non_contiguous_dma(reason="tiny transposed load"):
                    return e.dma_start(out=out, in_=in_)
            return e.dma_start(out=out, in_=in_)
        return self._dma_common(Q, r, w, issue, bg)

    def igather(self, out, src, idx, r=(), w=(), bounds=None):
        if bounds is None:
            return self._dma_common(self.POOL, r, w, lambda e: e.indirect_dma_start(
                out=out, out_offset=None, in_=src, in_offset=bass.IndirectOffsetOnAxis(ap=idx, axis=0)))
        return self._dma_common(self.POOL, r, w, lambda e: e.indirect_dma_start(
            out=out, out_offset=None, in_=src, in_offset=bass.IndirectOffsetOnAxis(ap=idx, axis=0),
            bounds_check=bounds, oob_is_err=False))

    def iscatter(self, dst, idx, in_, r=(), w=()):
        return self._dma_common(self.POOL, r, w, lambda e: e.indirect_dma_start(
            out=dst, out_offset=bass.IndirectOffsetOnAxis(ap=idx, axis=0), in_=in_, in_offset=None))

    def wait_bg(self):
        for tk in list(self.bg_tk.values()):
            self.SP.wait(tk)
        self.bg_tk = {}

    def barrier(self):
        SP = self.SP
        for E in self.engs:
            assert not E.pend_r and not E.pend_w, E.name
            if E is not SP and E.cnt > 0:
                SP.wait((E.sem, E.cnt, E.name))
        for tk in list(self.dma_tk.values()):
            SP.wait(tk)
        self.dma_tk = {}
        self.bcnt += 1
        SP.e.sem_inc(self.bsem, 1)
        for E in self.engs:
            if E is not SP:
                E.e.wait_ge(self.bsem, self.bcnt)

    @contextlib.contextmanager
    def scope(self):
        es = contextlib.ExitStack()
        kb = self
        bufs = []

        class S:
            def sb(self_, name, shape, dt):
                kb.uid += 1
                nm = "%s_%d" % (name, kb.uid)
                t = es.enter_context(kb.nc.sbuf_tensor(nm, shape, dt))
                b = Buf(t, nm)
                bufs.append(b)
                return b

            def ps(self_, name, shape, dt):
                kb.uid += 1
                nm = "%s_%d" % (name, kb.uid)
                t = es.enter_context(kb.nc.psum_tensor(nm, shape, dt))
                return Buf(t, nm)

        with es:
            yield S()
            self.barrier()
            for b in bufs:
                for kind, ent in b.ds.items():
                    self.free_sems[kind].append(ent)


def run_interleaved(jobs, nslots, stagger=0):
    pending = list(jobs)
    live = [None] * nslots
    rnd = 0
    while True:
        progressed = False
        rnd += 1
        for sl in range(nslots):
            if live[sl] is None and pending and rnd > sl * stagger:
                fn, a = pending.pop(0)
                live[sl] = fn(a, sl)
            if live[sl] is not None:
                progressed = True
                try:
                    next(live[sl])
                except StopIteration:
                    live[sl] = None
        if not progressed and not pending:
            break
        if not progressed and rnd > nslots * stagger + 2:
            break


def bc(ap, shape, axis):
    return ap.unsqueeze(axis).to_broadcast(shape)


def build_program():
    nc = bass.Bass("TRN2", target_bir_lowering=False)

    def din(name, shape, dt=F32):
        return nc.dram_tensor(name, list(shape), dt, kind="ExternalInput").ap()

    def dout(name, shape, dt=F32):
        return nc.dram_tensor(name, list(shape), dt, kind="ExternalOutput").ap()

    def dscr(name, shape, dt):
        return nc.dram_tensor(name, list(shape), dt, kind="Internal").ap()

    x = din("x", [TOK, 1024])
    cckv = din("cckv", [512, 128])
    ckr = din("ckr", [512, 32])
    sf_in = din("sf", [8, 64, 64])
    sb_in = din("sb", [8, 64, 64])
    cond = din("cond", [2, 1024])
    w_ada = din("w_ada", [1, 1024, 6144])
    b_ada = din("b_ada", [1, 6144])
    norm1_g = din("norm1_g", [1, 1024])
    w_in = din("w_in", [1, 1024, 2464])
    mla_q_norm_g = din("mla_q_norm_g", [1, 256])
    w_uq = din("w_uq", [1, 256, 768])
    mla_kv_norm_g = din("mla_kv_norm_g", [1, 128])
    w_ukv = din("w_ukv", [1, 128, 1024])
    mla_q_gain = din("mla_q_gain", [1, 96])
    mla_k_gain = din("mla_k_gain", [1, 96])
    ret_decay_fwd = din("ret_decay_fwd", [1, 8])
    ret_decay_bwd = din("ret_decay_bwd", [1, 8])
    ret_norm_g = din("ret_norm_g", [1, 64])
    w_o = din("w_o", [1, 1024, 1024])
    norm2_g = din("norm2_g", [1, 1024])
    router_w = din("router_w", [1, 1024, 64])
    router_bias = din("router_bias", [1, 64])
    exp_w_gate = din("exp_w_gate", [1, 64, 1024, 256])
    exp_w_up = din("exp_w_up", [1, 64, 1024, 256])
    exp_w_down = din("exp_w_down", [1, 64, 256, 1024])
    sh_w_gate = din("sh_w_gate", [1, 1024, 256])
    sh_w_up = din("sh_w_up", [1, 1024, 256])
    sh_w_down = din("sh_w_down", [1, 256, 1024])
    ident = din("c_ident", [128, 128])
    ropeq = din("c_ropeq", [NTS, 128, 2, 32])
    roper = din("c_roper", [NTS, 128, 2, 64])
    dexp = din("c_dexp", [2, 128, 128])
    dmask = din("c_dmask", [2, 128, 128])
    ecols = din("c_ecols", [128, 4])
    ltri_c = din("c_ltri", [128, 128])
    iota_c = din("c_iota", [128, 64])

    y = dout("y", [TOK, 1024])
    ockv = dout("ockv", [1024, 128])
    okr = dout("okr", [1024, 32])
    orf = dout("orf", [4, 8, 64, 64])
    orb = dout("orb", [4, 8, 64, 64])

    mod_d = dscr("mod_d", [2, 6144], F32)
    qT_d = dscr("qT_d", [8, 96, TOK], BF16)
    kT_d = dscr("kT_d", [8, 96, NKC * 128], BF16)
    v_d = dscr("v_d", [8, 128, NKC, 65], BF16)
    rqT_d = dscr("rqT_d", [NT, 64, 8, 128], BF16)
    rkT_d = dscr("rkT_d", [NT, 64, 8, 128], BF16)
    rk_d = dscr("rk_d", [NT, 128, 512], BF16)
    rv_d = dscr("rv_d", [NT, 128, 512], BF16)
    rg_d = dscr("rg_d", [NT, 128, 512], F32)
    mixT_d = dscr("mixT_d", [128, 8, TOK], BF16)
    x1_d = dscr("x1_d", [TOK, 1024], F32)
    h2T_d = dscr("h2T_d", [128, 8, TOK], BF16)
    gates_d = dscr("gates_d", [TOK, 64], F32)
    h2_d = dscr("h2_d", [TOK, 1024], BF16)
    wall_d = dscr("wall_d", [64, 128, 6144], BF16)
    sinfo_d = dscr("sinfo_d", [NSLOT, 2], I32)
    pos6_d = dscr("pos6_d", [128, NT * 6], I32)
    g6_d = dscr("g6_d", [128, NT * 6], F32)
    so_d = dscr("so_d", [NSLOT + TOK, 1024], BF16)

    K = KB(nc)
    SP, POOL = K.SP, K.POOL
    wall_b = Buf(None, "wall_b")
    sinfo_b = Buf(None, "sinfo_b")

    rs_cache = {}

    def rstd_of(S, ssq, n, name):
        shape = list(ssq.t[:].shape)
        ck = (id(S), name)
        if ck not in rs_cache:
            rs_cache[ck] = (S.sb(name + "_v", shape, F32), S.sb(name + "_s", shape, F32), S.sb(name + "_r", shape, F32))
        v, s, o = rs_cache[ck]
        K.V(lambda e: e.tensor_scalar(out=v.t[:], in0=ssq.t[:], scalar1=1.0 / n, scalar2=EPS,
                                      op0=ALU.mult, op1=ALU.add), r=[ssq], w=[v])
        K.A(lambda e: e.activation(out=s.t[:], in_=v.t[:], func=AF.Sqrt), r=[v], w=[s])
        K.V(lambda e: e.reciprocal(out=o.t[:], in_=s.t[:]), r=[s], w=[o])
        return o

    with K.scope() as S:
        cT = S.sb("cT", [128, 2, 8], F32)
        for c_ in range(2):
            K.dma(SP, cT.t[:, c_, :], cond[c_].rearrange("(k p) -> p k", p=128), w=[cT], noncontig=True)
        scT = S.sb("scT", [128, 2, 8], F32)
        K.A(lambda e: e.activation(out=scT.t[:], in_=cT.t[:], func=AF.Silu), r=[cT], w=[scT])
        bada = S.sb("bada", [2, 6144], F32)
        K.dma(SP, bada.t[:], b_ada[0:1, :].partition_broadcast(2), w=[bada])
        n1g = S.sb("n1g", [2, 1024], F32)
        K.dma(SP, n1g.t[:], norm1_g[0:1, :].partition_broadcast(2), w=[n1g])
        n2g = S.sb("n2g", [2, 1024], F32)
        K.dma(SP, n2g.t[:], norm2_g[0:1, :].partition_broadcast(2), w=[n2g])
        mod = S.sb("mod", [2, 6144], F32)
        wb = [S.sb("wa%d" % i, [128, 8, 512], F32) for i in range(3)]
        pp = [S.ps("pa%d" % i, [2, 512], F32) for i in range(2)]
        for g in range(12):
            wt = wb[g % 3]
            p = pp[g % 2]
            K.dma(SP, wt.t[:], w_ada[0, :, g * 512:(g + 1) * 512].rearrange("(k p) n -> p k n", p=128), w=[wt])
            for k in range(8):
                K.P(lambda e: e.matmul(p.t[:], lhsT=scT.t[:, :, k], rhs=wt.t[:, k, :],
                                       start=(k == 0), stop=(k == 7)), r=[scT, wt], w=[p], mark=(k == 7))
            K.V(lambda e: e.tensor_tensor(out=mod.t[:, g * 512:(g + 1) * 512], in0=p.t[:],
                                          in1=bada.t[:, g * 512:(g + 1) * 512], op=ALU.add), r=[p, bada], w=[mod])
        md = S.sb("md", [2, 6144], F32)

        def seg(b, i):
            return b.t[:, i * 1024:(i + 1) * 1024]
        K.V(lambda e: e.scalar_tensor_tensor(out=seg(md, 0), in0=seg(mod, 1), scalar=1.0, in1=n1g.t[:],
                                             op0=ALU.add, op1=ALU.mult), r=[mod, n1g], w=[md])
        K.V(lambda e: e.tensor_copy(out=seg(md, 1), in_=seg(mod, 0)), r=[mod], w=[md])
        K.V(lambda e: e.tensor_copy(out=seg(md, 2), in_=seg(mod, 2)), r=[mod], w=[md])
        K.V(lambda e: e.scalar_tensor_tensor(out=seg(md, 3), in0=seg(mod, 4), scalar=1.0, in1=n2g.t[:],
                                             op0=ALU.add, op1=ALU.mult), r=[mod, n2g], w=[md])
        K.V(lambda e: e.tensor_copy(out=seg(md, 4), in_=seg(mod, 3)), r=[mod], w=[md])
        K.V(lambda e: e.tensor_copy(out=seg(md, 5), in_=seg(mod, 5)), r=[mod], w=[md])
        K.dma(SP, mod_d[:, :], md.t[:], r=[md])

    def load_bc(S, name, src_row, n):
        b = S.sb(name, [128, n], F32)
        K.dma(SP, b.t[:], src_row.partition_broadcast(128), w=[b])
        return b

    def mod_bc(S, name, ci, i):
        return load_bc(S, name, mod_d[ci:ci + 1, i * 1024:(i + 1) * 1024], 1024)

    def make_ident(S):
        idf = S.sb("idf", [128, 128], F32)
        K.dma(SP, idf.t[:], ident[:, :], w=[idf])
        idb = S.sb("idb", [128, 128], BF16)
        K.V(lambda e: e.tensor_copy(out=idb.t[:], in_=idf.t[:]), r=[idf], w=[idb])
        return idb

    def tile_ci(t):
        return 0 if t < NTS else 1

    with K.scope() as S:
        idb = make_ident(S)
        win = S.sb("win", [128, 8, 2464], BF16)
        for k in range(8):
            K.dma(POOL, win.t[:, k, :], w_in[0, k * 128:(k + 1) * 128, :], w=[win])
        wuq = S.sb("wuq", [128, 2, 768], BF16)
        K.dma(POOL, wuq.t[:], w_uq[0].rearrange("(k p) n -> p k n", p=128), w=[wuq])
        wukv = S.sb("wukv", [128, 1024], BF16)
        K.dma(POOL, wukv.t[:], w_ukv[0], w=[wukv])
        precast = []
        for e_ in range(64):
            gu_v = wall_d[e_][:, 0:4096].rearrange("p (k n) -> p k n", k=8)
            precast.append((gu_v[:, :, 0:256], exp_w_gate[0, e_].rearrange("(k p) n -> p k n", p=128)))
            precast.append((gu_v[:, :, 256:512], exp_w_up[0, e_].rearrange("(k p) n -> p k n", p=128)))
            precast.append((wall_d[e_][:, 4096:6144].rearrange("p (c n) -> p c n", c=2),
                            exp_w_down[0, e_].rearrange("(c p) n -> p c n", p=128)))

        def issue_precast(n):
            for _ in range(n):
                if precast:
                    o_, i_ = precast.pop(0)
                    K.dma(POOL, o_, i_, r=[wall_b], bg=True)
        gm1b = [mod_bc(S, "gm1b%d" % ci, ci, 0) for ci in range(2)]
        sh1b = [mod_bc(S, "sh1b%d" % ci, ci, 1) for ci in range(2)]
        gqb = load_bc(S, "gqb", mla_q_norm_g[0:1, :], 256)
        gkvb = load_bc(S, "gkvb", mla_kv_norm_g[0:1, :], 128)
        qgb = load_bc(S, "qgb", mla_q_gain[0:1, :], 96)
        kgb = load_bc(S, "kgb", mla_k_gain[0:1, :], 96)

        NSL = 2

        def mk(name, shape, dt):
            return [S.sb("%s_s%d" % (name, i), shape, dt) for i in range(NSL)]
        junk = mk("junk", [128, 1024], BF16)
        xt = mk("xt", [128, 1024], F32)
        tmpf = mk("tmpf", [128, 1024], F32)
        hb = mk("hb", [128, 1024], BF16)
        hT = mk("hT", [128, 8, 128], BF16)
        ssq_x = mk("ssq_x", [128, 1], F32)
        ssq_q = mk("ssq_q", [128, 1], F32)
        ssq_kv = mk("ssq_kv", [128, 1], F32)
        cqn = mk("cqn", [128, 256], BF16)
        ckvn = mk("ckvn", [128, 128], F32)
        ckvnb = mk("ckvnb", [128, 128], BF16)
        krf = mk("krf", [128, 32], F32)
        cT3 = mk("cT3", [128, 3, 128], BF16)
        sqf = mk("sqf", [128, 768], F32)
        ssqh = mk("ssqh", [128, 8], F32)
        qn = mk("qn", [128, 8, 96], F32)
        kcat = mk("kcat", [128, 8, 96], F32)
        ra = mk("ra", [128, 8, 64], F32)
        rb = mk("rb", [128, 8, 64], F32)
        qb = mk("qb", [128, 8, 96], BF16)
        qT = mk("qT", [96, 8, 128], BF16)
        kT = mk("kT", [96, 8, 128], BF16)
        vaug = mk("vaug", [128, 8, 65], BF16)
        for i in range(NSL):
            K.V(lambda e: e.memset(vaug[i].t[:], 1.0), w=[vaug[i]])
        rf = mk("rf", [128, 8, 64], F32)
        rqb = mk("rqb", [128, 8, 64], BF16)
        rkb = mk("rkb", [128, 8, 64], BF16)
        rqT = mk("rqT", [64, 8, 128], BF16)
        rkT = mk("rkT", [64, 8, 128], BF16)
        rvb = mk("rvb", [128, 512], BF16)
        rgf = mk("rgf", [128, 512], F32)
        rpq = mk("rpq", [128, 2, 32], F32)
        rpr = mk("rpr", [128, 2, 64], F32)

        tp = [S.ps("tp%d" % i, [128, 8, 128], BF16) for i in range(NSL)]
        zp = [S.ps("zp%d" % i, [128, 512], F32) for i in range(NSL)]
        big = [S.ps("big%d" % i, [128, 1024], F32) for i in range(NSL)]

        def rope(i, xb, xv, R, tab):
            hf = R // 4
            rav = ra[i].t[:, :, 0:R]
            rbv = rb[i].t[:, :, 0:R]
            K.V(lambda e: e.tensor_tensor(out=rav, in0=xv, in1=bc(tab.t[:, 0, :], [128, 8, R], 1), op=ALU.mult),
                r=[xb, tab], w=[ra[i]])
            x5 = xv.rearrange("p h (a f j) -> p h a f j", a=2, f=2)
            r5 = rbv.rearrange("p h (a f j) -> p h a f j", a=2, f=2)
            s5 = tab.t[:, 1, :].rearrange("p (a f j) -> p a f j", a=2, f=2)
            for f in range(2):
                K.V(lambda e: e.tensor_tensor(out=r5[:, :, :, f, :], in0=x5[:, :, :, 1 - f, :],
                                              in1=bc(s5[:, :, f, :], [128, 8, 2, hf], 1), op=ALU.mult),
                    r=[xb, tab], w=[rb[i]])
            K.V(lambda e: e.tensor_tensor(out=xv, in0=rav, in1=rbv, op=ALU.add), r=[ra[i], rb[i]], w=[xb])

        def qk_proc(i, srcb, srcv, gainb, tab, outT, dst_ap):
            K.A(lambda e: e.activation(out=sqf[i].t[:].rearrange("p (h d) -> p h d", h=8), in_=srcv, func=AF.Square),
                r=[srcb], w=[sqf[i]])
            K.V(lambda e: e.reduce_sum(out=ssqh[i].t[:], in_=sqf[i].t[:].rearrange("p (h d) -> p h d", h=8), axis=AX.X),
                r=[sqf[i]], w=[ssqh[i]])
            yield
            rs = rstd_of(S, ssqh[i], 96, "rsh%d" % i)
            yield
            K.V(lambda e: e.tensor_tensor(out=qn[i].t[:], in0=srcv, in1=bc(rs.t[:, :], [128, 8, 96], 2), op=ALU.mult),
                r=[srcb, rs], w=[qn[i]])
            K.V(lambda e: e.tensor_tensor(out=qn[i].t[:], in0=qn[i].t[:], in1=bc(gainb.t[:, :], [128, 8, 96], 1),
                                          op=ALU.mult), r=[qn[i], gainb], w=[qn[i]])
            yield
            if tab is not None:
                rope(i, qn[i], qn[i].t[:, :, 64:96], 32, tab)
                yield
            K.A(lambda e: e.copy(out=qb[i].t[:], in_=qn[i].t[:]), r=[qn[i]], w=[qb[i]])
            yield
            t_ = tp[i]
            for h in range(8):
                K.P(lambda e: e.transpose(out=t_.t[0:96, h, :], in_=qb[i].t[:, h, :], identity=idb.t[:]),
                    r=[qb[i], idb], w=[t_], mark=(h == 7))
            K.A(lambda e: e.copy(out=outT.t[:], in_=t_.t[0:96, :, :]), r=[t_], w=[outT])
            K.dma(SP, dst_ap, outT.t[:], r=[outT])
            yield

        def kv_part(i, tab, gc):
            kvp = big[i]
            for cg in range(2):
                K.P(lambda e: e.matmul(kvp.t[:, cg * 512:(cg + 1) * 512], lhsT=cT3[i].t[:, 2, :],
                                       rhs=wukv.t[:, cg * 512:(cg + 1) * 512], start=True, stop=True),
                    r=[cT3[i], wukv], w=[kvp], mark=(cg == 1))
            yield
            kv3 = kvp.t[:].rearrange("p (h d) -> p h d", h=8)
            K.A(lambda e: e.copy(out=kcat[i].t[:, :, 0:64], in_=kv3[:, :, 0:64]), r=[kvp], w=[kcat[i]])
            K.V(lambda e: e.tensor_copy(out=kcat[i].t[:, :, 64:96], in_=bc(krf[i].t[:, :], [128, 8, 32], 1)),
                r=[krf[i]], w=[kcat[i]])
            K.A(lambda e: e.copy(out=vaug[i].t[:, :, 0:64], in_=kv3[:, :, 64:128]), r=[kvp], w=[vaug[i]])
            K.dma(SP, v_d[:, :, gc, :].rearrange("h p d -> p h d"), vaug[i].t[:], r=[vaug[i]])
            yield
            yield from qk_proc(i, kcat[i], kcat[i].t[:], kgb, tab, kT[i],
                               kT_d[:, :, gc * 128:(gc + 1) * 128].rearrange("h d t -> d h t"))

        def tileA(t, i):
            ci = tile_ci(t)
            sample = t < NTS
            gc = t if sample else 36 + (t - NTS)
            issue_precast(5)
            K.dma(SP, xt[i].t[:], x[t * 128:(t + 1) * 128, :], w=[xt[i]])
            if sample:
                K.dma(SP, rpq[i].t[:], ropeq[t], w=[rpq[i]])
                K.dma(SP, rpr[i].t[:], roper[t], w=[rpr[i]])
            yield
            K.A(lambda e: e.activation(out=junk[i].t[:], in_=xt[i].t[:], func=AF.Square, accum_out=ssq_x[i].t[:]),
                r=[xt[i]], w=[junk[i], ssq_x[i]])
            yield
            rs = rstd_of(S, ssq_x[i], 1024, "rsx%d" % i)
            yield
            K.V(lambda e: e.scalar_tensor_tensor(out=tmpf[i].t[:], in0=xt[i].t[:], scalar=rs.t[:, 0:1],
                                                 in1=gm1b[ci].t[:], op0=ALU.mult, op1=ALU.mult),
                r=[xt[i], rs, gm1b[ci]], w=[tmpf[i]])
            K.G(lambda e: e.tensor_tensor(out=hb[i].t[:], in0=tmpf[i].t[:], in1=sh1b[ci].t[:], op=ALU.add),
                r=[tmpf[i], sh1b[ci]], w=[hb[i]])
            yield
            t_ = tp[i]
            for k in range(8):
                K.P(lambda e: e.transpose(out=t_.t[:, k, :], in_=hb[i].t[:, k * 128:(k + 1) * 128], identity=idb.t[:]),
                    r=[hb[i], idb], w=[t_], mark=(k == 7))
            K.A(lambda e: e.copy(out=hT[i].t[:], in_=t_.t[:]), r=[t_], w=[hT[i]])
            yield

            def zgroup(c0, c1):
                z = zp[i]
                for k in range(8):
                    K.P(lambda e: e.matmul(z.t[:, 0:c1 - c0], lhsT=hT[i].t[:, k, :], rhs=win.t[:, k, c0:c1],
                                           start=(k == 0), stop=(k == 7)), r=[hT[i], win], w=[z], mark=(k == 7))
                return z

            z0 = zgroup(0, 416)
            yield
            K.A(lambda e: e.activation(out=junk[i].t[:, 0:256], in_=z0.t[:, 0:256], func=AF.Square,
                                       accum_out=ssq_q[i].t[:]), r=[z0], w=[junk[i], ssq_q[i]])
            K.A(lambda e: e.activation(out=junk[i].t[:, 256:384], in_=z0.t[:, 256:384], func=AF.Square,
                                       accum_out=ssq_kv[i].t[:]), r=[z0], w=[junk[i], ssq_kv[i]])
            yield
            rq_ = rstd_of(S, ssq_q[i], 256, "rsq%d" % i)
            rkv_ = rstd_of(S, ssq_kv[i], 128, "rskv%d" % i)
            yield
            K.V(lambda e: e.scalar_tensor_tensor(out=cqn[i].t[:], in0=z0.t[:, 0:256], scalar=rq_.t[:, 0:1],
                                                 in1=gqb.t[:], op0=ALU.mult, op1=ALU.mult),
                r=[z0, rq_, gqb], w=[cqn[i]])
            K.V(lambda e: e.scalar_tensor_tensor(out=ckvn[i].t[:], in0=z0.t[:, 256:384], scalar=rkv_.t[:, 0:1],
                                                 in1=gkvb.t[:], op0=ALU.mult, op1=ALU.mult),
                r=[z0, rkv_, gkvb], w=[ckvn[i]])
            K.A(lambda e: e.copy(out=krf[i].t[:], in_=z0.t[:, 384:416]), r=[z0], w=[krf[i]])
            K.A(lambda e: e.copy(out=ckvnb[i].t[:], in_=ckvn[i].t[:]), r=[ckvn[i]], w=[ckvnb[i]])
            if not sample:
                pr = (t - NTS) * 128
                K.dma(SP, ockv[pr:pr + 128, :], ckvn[i].t[:], r=[ckvn[i]])
                K.dma(SP, okr[pr:pr + 128, :], krf[i].t[:], r=[krf[i]])
            yield
            K.P(lambda e: e.transpose(out=t_.t[:, 0, :], in_=cqn[i].t[:, 0:128], identity=idb.t[:]),
                r=[cqn[i], idb], w=[t_], mark=False)
            K.P(lambda e: e.transpose(out=t_.t[:, 1, :], in_=cqn[i].t[:, 128:256], identity=idb.t[:]),
                r=[cqn[i], idb], w=[t_], mark=False)
            K.P(lambda e: e.transpose(out=t_.t[:, 2, :], in_=ckvnb[i].t[:], identity=idb.t[:]),
                r=[ckvnb[i], idb], w=[t_])
            K.A(lambda e: e.copy(out=cT3[i].t[:], in_=t_.t[:, 0:3, :]), r=[t_], w=[cT3[i]])
            yield
            qp = big[i]
            for (c0, c1) in ((0, 512), (512, 768)):
                for k in range(2):
                    K.P(lambda e: e.matmul(qp.t[:, c0:c1], lhsT=cT3[i].t[:, k, :], rhs=wuq.t[:, k, c0:c1],
                                           start=(k == 0), stop=(k == 1)), r=[cT3[i], wuq], w=[qp],
                        mark=(k == 1 and c0 == 512))
            yield
            yield from qk_proc(i, qp, qp.t[:, 0:768].rearrange("p (h d) -> p h d", h=8), qgb,
                               rpq[i] if sample else None, qT[i],
                               qT_d[:, :, t * 128:(t + 1) * 128].rearrange("h d t -> d h t"))
            yield from kv_part(i, rpq[i] if sample else None, gc)

            for which, (c0, c1) in enumerate(((416, 928), (928, 1440))):
                z = zgroup(c0, c1)
                yield
                sc = 1.0 if which == 0 else 0.125
                K.A(lambda e: e.activation(out=rf[i].t[:].rearrange("p h d -> p (h d)"), in_=z.t[:], func=AF.Copy,
                                           scale=sc), r=[z], w=[rf[i]])
                yield
                if sample:
                    rope(i, rf[i], rf[i].t[:], 64, rpr[i])
                    yield
                ob = rqb[i] if which == 0 else rkb[i]
                K.A(lambda e: e.copy(out=ob.t[:], in_=rf[i].t[:]), r=[rf[i]], w=[ob])
                yield
                for h in range(8):
                    K.P(lambda e: e.transpose(out=t_.t[0:64, h, :], in_=ob.t[:, h, :], identity=idb.t[:]),
                        r=[ob, idb], w=[t_], mark=(h == 7))
                oT = rqT[i] if which == 0 else rkT[i]
                K.A(lambda e: e.copy(out=oT.t[:], in_=t_.t[0:64, :, :]), r=[t_], w=[oT])
                K.dma(SP, (rqT_d if which == 0 else rkT_d)[t], oT.t[:], r=[oT])
                if which == 1:
                    K.dma(SP, rk_d[t], rkb[i].t[:].rearrange("p h d -> p (h d)"), r=[rkb[i]])
                yield
            z = zgroup(1440, 1952)
            yield
            K.A(lambda e: e.copy(out=rvb[i].t[:], in_=z.t[:]), r=[z], w=[rvb[i]])
            K.dma(SP, rv_d[t], rvb[i].t[:], r=[rvb[i]])
            yield
            z = zgroup(1952, 2464)
            yield
            K.A(lambda e: e.activation(out=rgf[i].t[:], in_=z.t[:], func=AF.Silu), r=[z], w=[rgf[i]])
            K.dma(SP, rg_d[t], rgf[i].t[:], r=[rgf[i]])
            yield

        def tileCtx(j, i):
            K.dma(SP, ckvn[i].t[:], cckv[j * 128:(j + 1) * 128, :], w=[ckvn[i]])
            K.dma(SP, krf[i].t[:], ckr[j * 128:(j + 1) * 128, :], w=[krf[i]])
            yield
            K.A(lambda e: e.copy(out=ckvnb[i].t[:], in_=ckvn[i].t[:]), r=[ckvn[i]], w=[ckvnb[i]])
            yield
            t_ = tp[i]
            K.P(lambda e: e.transpose(out=t_.t[:, 2, :], in_=ckvnb[i].t[:], identity=idb.t[:]),
                r=[ckvnb[i], idb], w=[t_])
            K.A(lambda e: e.copy(out=cT3[i].t[:, 2, :], in_=t_.t[:, 2, :]), r=[t_], w=[cT3[i]])
            yield
            yield from kv_part(i, None, 32 + j)

        jobs = [(tileA, t) for t in range(NT)] + [(tileCtx, j) for j in range(4)]
        run_interleaved(jobs, NSL, stagger=0)
        issue_precast(1000)

    with K.scope() as S:
        idb = make_ident(S)
        lg = []
        for d, src in enumerate((ret_decay_fwd, ret_decay_bwd)):
            raw = load_bc(S, "lgraw%d" % d, src[0:1, :], 8)
            ex = S.sb("lgex%d" % d, [128, 8], F32)
            K.A(lambda e: e.activation(out=ex.t[:], in_=raw.t[:], func=AF.Exp), r=[raw], w=[ex])
            l_ = S.sb("lg%d" % d, [128, 8], F32)
            K.V(lambda e: e.tensor_single_scalar(out=l_.t[:], in_=ex.t[:], scalar=-1.0, op=ALU.mult), r=[ex], w=[l_])
            lg.append(l_)
        ec = S.sb("ec", [128, 4], F32)
        K.dma(SP, ec.t[:], ecols[:, :], w=[ec])
        rngb = load_bc(S, "rngb", ret_norm_g[0:1, :], 64)
        maskT, kdec, qdec, cdec = [], [], [], []
        for d in range(2):
            de = S.sb("de%d" % d, [128, 128], F32)
            K.dma(SP, de.t[:], dexp[d], w=[de])
            dm = S.sb("dm%d" % d, [128, 128], F32)
            K.dma(SP, dm.t[:], dmask[d], w=[dm])
            mt = S.sb("mt%d" % d, [128, 8, 128], F32)
            for h in range(8):
                K.A(lambda e: e.activation(out=mt.t[:, h, :], in_=de.t[:], func=AF.Exp, scale=lg[d].t[:, h:h + 1]),
                    r=[de, lg[d]], w=[mt])
            K.V(lambda e: e.tensor_tensor(out=mt.t[:], in0=mt.t[:], in1=bc(dm.t[:, :], [128, 8, 128], 1),
                                          op=ALU.mult), r=[mt, dm], w=[mt])
            maskT.append(mt)
            kd_ = S.sb("kdec%d" % d, [128, 8], F32)
            K.A(lambda e: e.activation(out=kd_.t[:], in_=lg[d].t[:], func=AF.Exp, scale=ec.t[:, 2 * d:2 * d + 1]),
                r=[lg[d], ec], w=[kd_])
            qd_ = S.sb("qdec%d" % d, [128, 8], F32)
            K.A(lambda e: e.activation(out=qd_.t[:], in_=lg[d].t[:], func=AF.Exp,
                                       scale=ec.t[:, 2 * d + 1:2 * d + 2]), r=[lg[d], ec], w=[qd_])
            cd_ = S.sb("cdec%d" % d, [128, 8], F32)
            K.A(lambda e: e.activation(out=cd_.t[:], in_=lg[d].t[:], func=AF.Exp, scale=128.0), r=[lg[d]], w=[cd_])
            kdec.append(kd_)
            qdec.append(qd_)
            cdec.append(cd_)

        OF = S.sb("OF", [128, NTS, 512], F32)
        NRB = 3
        rqTc = [S.sb("rqTc%d" % i, [64, 8, 128], BF16) for i in range(NRB)]
        rkTc = [S.sb("rkTc%d" % i, [64, 8, 128], BF16) for i in range(NRB)]
        rkc = [S.sb("rkc%d" % i, [128, 8, 64], BF16) for i in range(NRB)]
        rvc = [S.sb("rvc%d" % i, [128, 8, 64], BF16) for i in range(NRB)]
        rgc = [S.sb("rgc%d" % i, [128, 512], F32) for i in range(NRB)]
        STm = [S.sb("STm%d" % i, [128, 8, 128], BF16) for i in range(2)]
        kdb = [S.sb("kdb%d" % i, [128, 8, 64], BF16) for i in range(2)]
        t1 = S.sb("t1", [128, 8, 64], F32)
        ob_ = S.sb("ob_", [128, 8, 64], F32)
        Sst = S.sb("Sst", [64, 8, 64], F32)
        Sbf = S.sb("Sbf", [64, 8, 64], BF16)
        sq2 = S.sb("sq2", [128, 512], F32)
        ssq2 = S.sb("ssq2", [128, 8], F32)
        mixr = S.sb("mixr", [128, 512], BF16)
        mT = [S.sb("mTr%d" % i, [128, 4, 128], BF16) for i in range(2)]
        STp = [S.ps("STp%d" % i, [128, 8, 128], F32) for i in range(2)]
        INp = S.ps("INp", [128, 8, 64], F32)
        CRp = S.ps("CRp", [128, 8, 64], F32)
        KVp = S.ps("KVp", [64, 8, 64], F32)
        tpr = S.ps("tpr", [128, 4, 128], BF16)

        seqs = [(0, NTS, None)] + [(NTS + 2 * p, 2, p) for p in range(4)]
        units = []
        for (t0, n, pidx) in seqs:
            for d in range(2):
                order = list(range(n)) if d == 0 else list(range(n - 1, -1, -1))
                for k_, c in enumerate(order):
                    units.append(dict(t=t0 + c, c=c, d=d, pidx=pidx, first=(k_ == 0), last=(k_ == n - 1)))

        def pre_r(u):
            U = units[u]
            t, d = U["t"], U["d"]
            i = u % NRB
            K.dma(SP, rqTc[i].t[:], rqT_d[t], w=[rqTc[i]])
            K.dma(SP, rkTc[i].t[:], rkT_d[t], w=[rkTc[i]])
            K.dma(SP, rkc[i].t[:].rearrange("p h d -> p (h d)"), rk_d[t], w=[rkc[i]])
            K.dma(SP, rvc[i].t[:].rearrange("p h d -> p (h d)"), rv_d[t], w=[rvc[i]])
            if d == 1:
                K.dma(SP, rgc[i].t[:], rg_d[t], w=[rgc[i]])
            sp_ = STp[u % 2]
            for h in range(8):
                K.P(lambda e: e.matmul(sp_.t[:, h, :], lhsT=rkTc[i].t[:, h, :], rhs=rqTc[i].t[:, h, :],
                                       start=True, stop=True), r=[rkTc[i], rqTc[i]], w=[sp_], mark=(h == 7))
            K.V(lambda e: e.tensor_tensor(out=STm[u % 2].t[:], in0=sp_.t[:], in1=maskT[d].t[:], op=ALU.mult),
                r=[sp_, maskT[d]], w=[STm[u % 2]])
            K.V(lambda e: e.tensor_tensor(out=kdb[u % 2].t[:], in0=rkc[i].t[:], in1=bc(kdec[d].t[:, :], [128, 8, 64], 2),
                                          op=ALU.mult), r=[rkc[i], kdec[d]], w=[kdb[u % 2]])

        def main_r(u):
            U = units[u]
            t, c, d, pidx = U["t"], U["c"], U["d"], U["pidx"]
            i = u % NRB
            sm = STm[u % 2]
            kd_ = kdb[u % 2]
            if U["first"]:
                if pidx is None:
                    K.dma(SP, Sst.t[:], (sf_in if d == 0 else sb_in).rearrange("h d e -> d h e"), w=[Sst])
                else:
                    K.V(lambda e: e.memset(Sst.t[:], 0.0), w=[Sst])
                K.A(lambda e: e.copy(out=Sbf.t[:], in_=Sst.t[:]), r=[Sst], w=[Sbf])
            for h in range(8):
                K.P(lambda e: e.matmul(INp.t[:, h, :], lhsT=sm.t[:, h, :], rhs=rvc[i].t[:, h, :],
                                       start=True, stop=True), r=[sm, rvc[i]], w=[INp], mark=(h == 7))
            for h in range(8):
                K.P(lambda e: e.matmul(KVp.t[:, h, :], lhsT=kd_.t[:, h, :], rhs=rvc[i].t[:, h, :],
                                       start=True, stop=True), r=[kd_, rvc[i]], w=[KVp], mark=(h == 7))
            for h in range(8):
                K.P(lambda e: e.matmul(CRp.t[:, h, :], lhsT=rqTc[i].t[:, h, :], rhs=Sbf.t[:, h, :],
                                       start=True, stop=True), r=[rqTc[i], Sbf], w=[CRp], mark=(h == 7))
            K.V(lambda e: e.tensor_tensor(out=Sst.t[:], in0=Sst.t[:], in1=bc(cdec[d].t[0:64, :], [64, 8, 64], 2),
                                          op=ALU.mult), r=[Sst, cdec[d]], w=[Sst])
            K.V(lambda e: e.tensor_tensor(out=t1.t[:], in0=CRp.t[:], in1=bc(qdec[d].t[:, :], [128, 8, 64], 2),
                                          op=ALU.mult), r=[CRp, qdec[d]], w=[t1])
            K.V(lambda e: e.tensor_tensor(out=Sst.t[:], in0=Sst.t[:], in1=KVp.t[:], op=ALU.add),
                r=[Sst, KVp], w=[Sst])
            K.A(lambda e: e.copy(out=Sbf.t[:], in_=Sst.t[:]), r=[Sst], w=[Sbf])
            ofv = OF.t[:, c, :].rearrange("p (h d) -> p h d", h=8)
            if d == 0:
                K.V(lambda e: e.tensor_tensor(out=ofv, in0=t1.t[:], in1=INp.t[:], op=ALU.add),
                    r=[t1, INp], w=[OF])
            else:
                K.V(lambda e: e.tensor_tensor(out=ob_.t[:], in0=t1.t[:], in1=INp.t[:], op=ALU.add),
                    r=[t1, INp], w=[ob_])
                K.V(lambda e: e.tensor_tensor(out=ob_.t[:], in0=ob_.t[:], in1=ofv, op=ALU.add),
                    r=[ob_, OF], w=[ob_])
                o2 = ob_.t[:].rearrange("p h d -> p (h d)")
                K.A(lambda e: e.activation(out=sq2.t[:], in_=o2, func=AF.Square), r=[ob_], w=[sq2])
                K.V(lambda e: e.reduce_sum(out=ssq2.t[:], in_=sq2.t[:].rearrange("p (h d) -> p h d", h=8),
                                           axis=AX.X), r=[sq2], w=[ssq2])
                rs = rstd_of(S, ssq2, 64, "rso")
                K.V(lambda e: e.tensor_tensor(out=ob_.t[:], in0=ob_.t[:], in1=bc(rs.t[:, :], [128, 8, 64], 2),
                                              op=ALU.mult), r=[ob_, rs], w=[ob_])
                K.V(lambda e: e.tensor_tensor(out=ob_.t[:], in0=ob_.t[:], in1=bc(rngb.t[:, :], [128, 8, 64], 1),
                                              op=ALU.mult), r=[ob_, rngb], w=[ob_])
                K.V(lambda e: e.tensor_tensor(out=mixr.t[:], in0=o2, in1=rgc[i].t[:], op=ALU.mult),
                    r=[ob_, rgc[i]], w=[mixr])
                for j in range(4):
                    K.P(lambda e: e.transpose(out=tpr.t[:, j, :], in_=mixr.t[:, j * 128:(j + 1) * 128],
                                              identity=idb.t[:]), r=[mixr, idb], w=[tpr], mark=(j == 3))
                K.A(lambda e: e.copy(out=mT[u % 2].t[:], in_=tpr.t[:]), r=[tpr], w=[mT[u % 2]])
                K.dma(SP, mixT_d[:, 4:8, t * 128:(t + 1) * 128], mT[u % 2].t[:], r=[mT[u % 2]])
            if U["last"] and pidx is not None:
                K.dma(SP, (orf if d == 0 else orb)[pidx].rearrange("h d e -> d h e"), Sst.t[:], r=[Sst])

        pre_r(0)
        for u in range(len(units)):
            if u + 1 < len(units):
                pre_r(u + 1)
            main_r(u)

    with K.scope() as S:
        onesf = S.sb("onesf", [128, 64], F32)
        K.V(lambda e: e.memset(onesf.t[:], 1.0), w=[onesf])
        kTh = [S.sb("kTh%d" % i, [96, 36 * 128], BF16) for i in range(2)]
        vh = [S.sb("vh%d" % i, [128, 36, 65], BF16) for i in range(2)]
        qTh = [S.sb("qTh%d" % i, [96, 4096], BF16) for i in range(2)]
        pt = [S.sb("pt%d" % i, [128, 2, 512], BF16) for i in range(3)]
        rcb = S.sb("rcb", [128, 512], F32)
        bcs = S.sb("bcs", [64, 512], F32)
        bcr = S.sb("bcr", [64, 512], F32)
        ot = [S.sb("ot%d" % i, [64, 512], BF16) for i in range(2)]
        spp = [S.ps("spp%d" % i, [128, 2, 512], F32) for i in range(3)]
        accp = [S.ps("accp%d" % i, [65, 512], F32) for i in range(2)]
        scale = float(96 ** -0.5)
        seqs = [(0, 4096, 0, 36, 512)] + [(4096 + 256 * p, 256, 36 + 2 * p, 2, 256) for p in range(4)]
        hc = 0
        gcnt = 0
        slot_ctr = [0]
        pending_tail = [None]

        def alloc_slot():
            slot_ctr[0] += 1
            return slot_ctr[0] % 3

        for (c0, L, kc0, nkc, QG) in seqs:
            for h in range(8):
                i = hc % 2
                hc += 1
                K.dma(SP, kTh[i].t[:, 0:nkc * 128], kT_d[h, :, kc0 * 128:(kc0 + nkc) * 128], w=[kTh[i]])
                K.dma(SP, vh[i].t[:, 0:nkc, :], v_d[h, :, kc0:kc0 + nkc, :], w=[vh[i]])
                K.dma(SP, qTh[i].t[:, 0:L], qT_d[h, :, c0:c0 + L], w=[qTh[i]])
                nu = nkc // 2
                for qg in range(L // QG):
                    acc = accp[gcnt % 2]
                    oo = ot[gcnt % 2]
                    gcnt += 1
                    uslot = {}

                    def emit_s(u):
                        j = alloc_slot()
                        uslot[u] = j
                        for c in range(2):
                            kc = 2 * u + c
                            K.P(lambda e: e.matmul(spp[j].t[:, c, 0:QG], lhsT=kTh[i].t[:, kc * 128:(kc + 1) * 128],
                                                   rhs=qTh[i].t[:, qg * QG:(qg + 1) * QG], start=True, stop=True),
                                r=[kTh[i], qTh[i]], w=[spp[j]], mark=(c == 1))
                        K.A(lambda e: e.activation(out=pt[j].t[:, :, 0:QG], in_=spp[j].t[:, :, 0:QG], func=AF.Exp,
                                                   scale=scale), r=[spp[j]], w=[pt[j]])

                    def emit_pv(u):
                        j = uslot[u]
                        for c in range(2):
                            kc = 2 * u + c
                            K.P(lambda e: e.matmul(acc.t[:, 0:QG], lhsT=vh[i].t[:, kc, :], rhs=pt[j].t[:, c, 0:QG],
                                                   start=(kc == 0), stop=(kc == nkc - 1)),
                                r=[vh[i], pt[j]], w=[acc], mark=(kc == nkc - 1))

                    emit_s(0)
                    if nu > 1:
                        emit_s(1)
                    for u in range(nu):
                        if u + 2 < nu:
                            emit_s(u + 2)
                        emit_pv(u)
                        if u == min(1, nu - 1) and pending_tail[0] is not None:
                            pending_tail[0](uslot[u])
                            pending_tail[0] = None

                    def tail(j, acc=acc, oo=oo, QG=QG, h=h, col=c0 + qg * QG):
                        bcp = spp[j]
                        K.A(lambda e: e.copy(out=rcb.t[64:65, 0:QG], in_=acc.t[64:65, 0:QG]), r=[acc], w=[rcb])
                        K.P(lambda e: e.matmul(bcp.t[0:64, 0, 0:QG], lhsT=onesf.t[64:65, 0:64], rhs=rcb.t[64:65, 0:QG],
                                               start=True, stop=True), r=[onesf, rcb], w=[bcp])
                        K.A(lambda e: e.copy(out=bcs.t[:, 0:QG], in_=bcp.t[0:64, 0, 0:QG]), r=[bcp], w=[bcs])
                        K.V(lambda e: e.reciprocal(out=bcr.t[:, 0:QG], in_=bcs.t[:, 0:QG]), r=[bcs], w=[bcr])
                        K.V(lambda e: e.tensor_tensor(out=oo.t[:, 0:QG], in0=acc.t[0:64, 0:QG], in1=bcr.t[:, 0:QG],
                                                      op=ALU.mult), r=[acc, bcr], w=[oo])
                        K.dma(SP, mixT_d[(h % 2) * 64:(h % 2) * 64 + 64, h // 2, col:col + QG], oo.t[:, 0:QG], r=[oo])
                    assert pending_tail[0] is None
                    pending_tail[0] = tail
                    last_slot = uslot[nu - 1]
        if pending_tail[0] is not None:
            pending_tail[0](last_slot)
            pending_tail[0] = None

    with K.scope() as S:
        idb = make_ident(S)
        idf2 = S.sb("idf2", [128, 128], F32)
        K.dma(SP, idf2.t[:], ident[:, :], w=[idf2])
        wo = S.sb("wo", [128, 8, 1024], BF16)
        for k in range(8):
            K.dma(POOL, wo.t[:, k, :], w_o[0, k * 128:(k + 1) * 128, :], w=[wo])
        rwb = S.sb("rwb", [128, 8, 64], BF16)
        K.dma(POOL, rwb.t[:], router_w[0].rearrange("(k p) n -> p k n", p=128), w=[rwb])
        rbb = load_bc(S, "rbb", router_bias[0:1, :], 64)
        g1b = [mod_bc(S, "g1b%d" % ci, ci, 2) for ci in range(2)]
        gm2b = [mod_bc(S, "gm2b%d" % ci, ci, 3) for ci in range(2)]
        sh2b = [mod_bc(S, "sh2b%d" % ci, ci, 4) for ci in range(2)]
        ltf = S.sb("ltf", [128, 128], F32)
        K.dma(SP, ltf.t[:], ltri_c[:, :], w=[ltf])
        ltb = S.sb("ltb", [128, 128], BF16)
        K.V(lambda e: e.tensor_copy(out=ltb.t[:], in_=ltf.t[:]), r=[ltf], w=[ltb])
        oneb = S.sb("oneb", [128, 128], BF16)
        K.V(lambda e: e.memset(oneb.t[:], 1.0), w=[oneb])
        iof = S.sb("iof", [128, 64], F32)
        K.dma(SP, iof.t[:], iota_c[:, :], w=[iof])
        NSL = 2

        def mk(name, shape, dt):
            return [S.sb("%s_c%d" % (name, i), shape, dt) for i in range(NSL)]
        xt = mk("xc", [128, 1024], F32)
        mTl = mk("mTl", [128, 8, 128], BF16)
        x1t = mk("x1t", [128, 1024], F32)
        tmpf = mk("tmpc", [128, 1024], F32)
        junk = mk("junkc", [128, 1024], BF16)
        ssq_x = mk("ssqc", [128, 1], F32)
        h2b = mk("h2b", [128, 1024], BF16)
        h2T = mk("h2T", [128, 8, 128], BF16)
        scs = mk("scs", [128, 64], F32)
        msk = mk("msk", [128, 64], F32)
        mskb = mk("mskb", [128, 64], BF16)
        gmv = mk("gmv", [128, 64], F32)
        den = mk("den", [128, 1], F32)
        rden = mk("rden", [128, 1], F32)
        sel_all = S.sb("sel_all", [128, NT, 64], F32)
        m8_all = S.sb("m8_all", [128, NT, 8], F32)
        gts_all = S.sb("gts_all", [128, NT, 64], F32)
        rank_all = S.sb("rank_all", [128, NT, 64], F32)
        carry = S.sb("carry", [128, 64], F32)
        K.V(lambda e: e.memset(carry.t[:], 0.0), w=[carry])
        op_ = [S.ps("op_%d" % i, [128, 1024], F32) for i in range(NSL)]
        tpc_ = [S.ps("tpc_%d" % i, [128, 8, 128], BF16) for i in range(NSL)]
        sml = [S.ps("sml%d" % i, [128, 3, 64], F32) for i in range(NSL)]

        def tileC(t, i):
            ci = tile_ci(t)
            K.dma(SP, xt[i].t[:], x[t * 128:(t + 1) * 128, :], w=[xt[i]])
            K.dma(SP, mTl[i].t[:], mixT_d[:, :, t * 128:(t + 1) * 128], w=[mTl[i]])
            yield
            for half in range(2):
                for k in range(8):
                    K.P(lambda e: e.matmul(op_[i].t[:, half * 512:(half + 1) * 512], lhsT=mTl[i].t[:, k, :],
                                           rhs=wo.t[:, k, half * 512:(half + 1) * 512], start=(k == 0), stop=(k == 7)),
                        r=[mTl[i], wo], w=[op_[i]], mark=(k == 7 and half == 1))
            yield
            K.V(lambda e: e.tensor_tensor(out=tmpf[i].t[:], in0=op_[i].t[:], in1=g1b[ci].t[:], op=ALU.mult),
                r=[op_[i], g1b[ci]], w=[tmpf[i]])
            yield
            K.G(lambda e: e.tensor_tensor(out=x1t[i].t[:], in0=tmpf[i].t[:], in1=xt[i].t[:], op=ALU.add),
                r=[tmpf[i], xt[i]], w=[x1t[i]])
            K.dma(SP, x1_d[t * 128:(t + 1) * 128, :], x1t[i].t[:], r=[x1t[i]])
            yield
            K.A(lambda e: e.activation(out=junk[i].t[:], in_=x1t[i].t[:], func=AF.Square, accum_out=ssq_x[i].t[:]),
                r=[x1t[i]], w=[junk[i], ssq_x[i]])
            yield
            rs = rstd_of(S, ssq_x[i], 1024, "rsc%d" % i)
            yield
            K.V(lambda e: e.scalar_tensor_tensor(out=tmpf[i].t[:], in0=x1t[i].t[:], scalar=rs.t[:, 0:1],
                                                 in1=gm2b[ci].t[:], op0=ALU.mult, op1=ALU.mult),
                r=[x1t[i], rs, gm2b[ci]], w=[tmpf[i]])
            yield
            K.G(lambda e: e.tensor_tensor(out=h2b[i].t[:], in0=tmpf[i].t[:], in1=sh2b[ci].t[:], op=ALU.add),
                r=[tmpf[i], sh2b[ci]], w=[h2b[i]])
            K.dma(SP, h2_d[t * 128:(t + 1) * 128, :], h2b[i].t[:], r=[h2b[i]])
            yield
            for k in range(8):
                K.P(lambda e: e.transpose(out=tpc_[i].t[:, k, :], in_=h2b[i].t[:, k * 128:(k + 1) * 128],
                                          identity=idb.t[:]), r=[h2b[i], idb], w=[tpc_[i]], mark=(k == 7))
            K.A(lambda e: e.copy(out=h2T[i].t[:], in_=tpc_[i].t[:]), r=[tpc_[i]], w=[h2T[i]])
            yield
            rlp = sml[i].t[:, 0, :]
            for k in range(8):
                K.P(lambda e: e.matmul(rlp, lhsT=h2T[i].t[:, k, :], rhs=rwb.t[:, k, :], start=(k == 0),
                                       stop=(k == 7)), r=[h2T[i], rwb], w=[sml[i]], mark=(k == 7))
            yield
            K.A(lambda e: e.activation(out=scs[i].t[:], in_=rlp, func=AF.Sigmoid), r=[sml[i]], w=[scs[i]])
            yield
            selv = sel_all.t[:, t, :]
            K.V(lambda e: e.tensor_tensor(out=selv, in0=scs[i].t[:], in1=rbb.t[:], op=ALU.add),
                r=[scs[i], rbb], w=[sel_all])
            K.V(lambda e: e.max(out=m8_all.t[:, t, :], in_=selv), r=[sel_all], w=[m8_all])
            K.V(lambda e: e.tensor_single_scalar(out=msk[i].t[:], in_=selv, scalar=m8_all.t[:, t, 5:6], op=ALU.is_ge),
                r=[sel_all, m8_all], w=[msk[i]])
            yield
            K.V(lambda e: e.tensor_tensor(out=gmv[i].t[:], in0=msk[i].t[:], in1=scs[i].t[:], op=ALU.mult),
                r=[msk[i], scs[i]], w=[gmv[i]])
            K.V(lambda e: e.reduce_sum(out=den[i].t[:], in_=gmv[i].t[:], axis=AX.X), r=[gmv[i]], w=[den[i]])
            K.A(lambda e: e.copy(out=mskb[i].t[:], in_=msk[i].t[:]), r=[msk[i]], w=[mskb[i]])
            yield
            K.V(lambda e: e.reciprocal(out=rden[i].t[:], in_=den[i].t[:]), r=[den[i]], w=[rden[i]])
            K.P(lambda e: e.matmul(sml[i].t[:, 1, :], lhsT=ltb.t[:], rhs=mskb[i].t[:], start=True, stop=True),
                r=[ltb, mskb[i]], w=[sml[i]], mark=False)
            K.P(lambda e: e.matmul(sml[i].t[:, 2, :], lhsT=oneb.t[:], rhs=mskb[i].t[:], start=True, stop=True),
                r=[oneb, mskb[i]], w=[sml[i]])
            yield
            K.V(lambda e: e.tensor_scalar(out=gts_all.t[:, t, :], in0=gmv[i].t[:], scalar1=rden[i].t[:, 0:1],
                                          scalar2=2.5, op0=ALU.mult, op1=ALU.mult), r=[gmv[i], rden[i]], w=[gts_all])
            K.V(lambda e: e.tensor_tensor(out=rank_all.t[:, t, :], in0=sml[i].t[:, 1, :], in1=carry.t[:], op=ALU.add),
                r=[sml[i], carry], w=[rank_all])
            K.V(lambda e: e.tensor_tensor(out=carry.t[:], in0=sml[i].t[:, 2, :], in1=carry.t[:], op=ALU.add),
                r=[sml[i], carry], w=[carry])
            yield

        run_interleaved([(tileC, t) for t in range(NT)], NSL, stagger=0)

        ci32 = S.sb("ci32", [128, 64], I32)
        padf = S.sb("padf", [128, 64], F32)
        K.V(lambda e: e.tensor_single_scalar(out=padf.t[:], in_=carry.t[:], scalar=127.0, op=ALU.add), r=[carry], w=[padf])
        K.V(lambda e: e.tensor_copy(out=ci32.t[:], in_=padf.t[:]), r=[padf], w=[ci32])
        K.V(lambda e: e.tensor_single_scalar(out=ci32.t[:], in_=ci32.t[:], scalar=7, op=ALU.arith_shift_right),
            r=[ci32], w=[ci32])
        K.V(lambda e: e.tensor_single_scalar(out=ci32.t[:], in_=ci32.t[:], scalar=7, op=ALU.logical_shift_left),
            r=[ci32], w=[ci32])
        K.V(lambda e: e.tensor_copy(out=padf.t[:], in_=ci32.t[:]), r=[ci32], w=[padf])
        ptp = op_[0]
        K.P(lambda e: e.transpose(out=ptp.t[0:64, 0:128], in_=padf.t[:], identity=idf2.t[:]), r=[padf, idf2], w=[ptp])
        PTs = S.sb("PTs", [64, 128], F32)
        K.A(lambda e: e.copy(out=PTs.t[:], in_=ptp.t[0:64, 0:128]), r=[ptp], w=[PTs])
        offp = op_[1]
        K.P(lambda e: e.matmul(offp.t[:, 0:64], lhsT=PTs.t[:], rhs=ltf.t[0:64, 0:64], start=True, stop=True),
            r=[PTs, ltf], w=[offp])
        offb = S.sb("offb", [128, 64], F32)
        K.A(lambda e: e.copy(out=offb.t[:], in_=offp.t[:, 0:64]), r=[offp], w=[offb])

        zt = S.sb("zt", [128, NST * 2], I32)
        K.V(lambda e: e.memset(zt.t[:], 0), w=[zt])
        K.dma(SP, sinfo_d.rearrange("(p n) c -> p (n c)", p=128), zt.t[:], r=[zt], w=[sinfo_b])
        pos = S.sb("pos", [128, 64], F32)
        oh = S.sb("oh", [128, 6, 64], F32)
        ohp = S.sb("ohp", [128, 6, 64], F32)
        pos6f = S.sb("pos6f", [128, NT * 6], F32)
        e6f = S.sb("e6f", [128, 6], F32)
        g6a = S.sb("g6a", [128, NT * 6], F32)
        pos6i = S.sb("pos6i", [128, NT * 6], I32)
        tokf = S.sb("tokf", [128, 1], F32)
        infs = [S.sb("infs%d" % i, [128, 6, 2], I32) for i in range(2)]
        hi6i = S.sb("hi6i", [128, 6], I32)
        hi6f = S.sb("hi6f", [128, 6], F32)
        fl6f = S.sb("fl6f", [128, 6], F32)
        fl6i = [S.sb("fl6i%d" % i, [128, 6], I32) for i in range(2)]
        ecl = S.sb("ecl", [128, 4], F32)
        K.dma(SP, ecl.t[:], ecols[:, :], w=[ecl])
        for t in range(NT):
            i = t % 2
            K.V(lambda e: e.tensor_tensor(out=pos.t[:], in0=rank_all.t[:, t, :], in1=offb.t[:], op=ALU.add),
                r=[rank_all, offb], w=[pos])
            K.V(lambda e: e.tensor_tensor(out=oh.t[:], in0=bc(sel_all.t[:, t, :], [128, 6, 64], 1),
                                          in1=bc(m8_all.t[:, t, 0:6], [128, 6, 64], 2), op=ALU.is_equal),
                r=[sel_all, m8_all], w=[oh])
            for (src_ap, srcb, dst_ap, dstb) in (
                    (pos.t[:], pos, pos6f.t[:, t * 6:(t + 1) * 6], pos6f),
                    (gts_all.t[:, t, :], gts_all, g6a.t[:, t * 6:(t + 1) * 6], g6a),
                    (iof.t[:], iof, e6f.t[:], e6f)):
                K.V(lambda e: e.tensor_tensor(out=ohp.t[:], in0=oh.t[:], in1=bc(src_ap, [128, 6, 64], 1), op=ALU.mult),
                    r=[oh, srcb], w=[ohp])
                K.V(lambda e: e.reduce_sum(out=dst_ap, in_=ohp.t[:], axis=AX.X), r=[ohp], w=[dstb])
            K.V(lambda e: e.tensor_copy(out=pos6i.t[:, t * 6:(t + 1) * 6], in_=pos6f.t[:, t * 6:(t + 1) * 6]),
                r=[pos6f], w=[pos6i])
            K.V(lambda e: e.tensor_single_scalar(out=hi6i.t[:], in_=pos6i.t[:, t * 6:(t + 1) * 6], scalar=7,
                                                 op=ALU.arith_shift_right), r=[pos6i], w=[hi6i])
            K.V(lambda e: e.tensor_copy(out=hi6f.t[:], in_=hi6i.t[:]), r=[hi6i], w=[hi6f])
            K.V(lambda e: e.tensor_single_scalar(out=fl6f.t[:], in_=pos6f.t[:, t * 6:(t + 1) * 6], scalar=float(NST),
                                                 op=ALU.mult), r=[pos6f], w=[fl6f])
            K.V(lambda e: e.scalar_tensor_tensor(out=fl6f.t[:], in0=hi6f.t[:], scalar=-float(128 * NST - 1),
                                                 in1=fl6f.t[:], op0=ALU.mult, op1=ALU.add), r=[hi6f, fl6f], w=[fl6f])
            K.V(lambda e: e.tensor_copy(out=fl6i[i].t[:], in_=fl6f.t[:]), r=[fl6f], w=[fl6i[i]])
            K.V(lambda e: e.tensor_single_scalar(out=tokf.t[:], in_=ecl.t[:, 2:3], scalar=float(t * 128), op=ALU.add),
                r=[ecl], w=[tokf])
            K.V(lambda e: e.tensor_copy(out=infs[i].t[:, :, 0], in_=bc(tokf.t[:, 0:1], [128, 6, 1], 1)[:, :, 0]),
                r=[tokf], w=[infs[i]])
            K.V(lambda e: e.tensor_copy(out=infs[i].t[:, :, 1], in_=e6f.t[:]), r=[e6f], w=[infs[i]])
            for j in range(6):
                K.iscatter(sinfo_d[:, :], fl6i[i].t[:, j:j + 1], infs[i].t[:, j, :],
                           r=[infs[i], fl6i[i], sinfo_b])
        K.dma(SP, pos6_d[:, :], pos6i.t[:], r=[pos6i])
        K.dma(SP, g6_d[:, :], g6a.t[:], r=[g6a])
        K.wait_bg()

    with K.scope() as S:
        idb = make_ident(S)
        tpD = [S.ps("ftp%d" % i, [128, 8, 128], BF16) for i in range(2)]
        guD = [S.ps("fgu%d" % i, [128, 512], F32) for i in range(2)]
        htpD = S.ps("fhtp", [128, 2, 128], BF16)
        ypD = S.ps("fyp", [128, 1024], F32)
        xsTD = [S.sb("fxsT%d" % i, [128, 8, 128], BF16) for i in range(3)]
        sgD = [S.sb("fsg%d" % i, [128, 256], F32) for i in range(2)]
        HD = [S.sb("fH%d" % i, [128, 256], BF16) for i in range(2)]
        HTD = [S.sb("fHT%d" % i, [128, 2, 128], BF16) for i in range(2)]
        soD = [S.sb("fso%d" % i, [128, 1024], BF16) for i in range(2)]
        NB = 5
        NW = 4
        xs = [S.sb("xs%d" % i, [128, 1024], BF16) for i in range(NB)]
        Wt = [S.sb("Wt%d" % i, [128, 6144], BF16) for i in range(NW)]
        ecl = S.sb("ecl2", [128, 4], F32)
        K.dma(SP, ecl.t[:], ecols[:, :], w=[ecl])
        Wsh = S.sb("Wsh", [128, 6144], BF16)
        guv = Wsh.t[:, 0:4096].rearrange("p (k n) -> p k n", k=8)
        K.dma(POOL, guv[:, :, 0:256], sh_w_gate[0].rearrange("(k p) n -> p k n", p=128), w=[Wsh])
        K.dma(POOL, guv[:, :, 256:512], sh_w_up[0].rearrange("(k p) n -> p k n", p=128), w=[Wsh])
        K.dma(POOL, Wsh.t[:, 4096:6144].rearrange("p (c n) -> p c n", c=2),
              sh_w_down[0].rearrange("(c p) n -> p c n", p=128), w=[Wsh])
        wall_rows = wall_d.rearrange("e p n -> (e p) n")
        bnd_reg = nc.gpsimd.alloc_register("bnd")
        nc.gpsimd.reg_mov(bnd_reg, 64 * 128 - 1)

        sinf_all = S.sb("sinf_all", [128, NST, 2], I32)
        K.dma(SP, sinf_all.t[:].rearrange("p j c -> p (j c)"), sinfo_d.rearrange("(p j) c -> p (j c)", p=128),
              w=[sinf_all])
        rowi = S.sb("rowi", [1, NST * 2], I32)
        K.dma(SP, rowi.t[:], sinfo_d[0:NST, :].rearrange("(o j) c -> o (j c)", o=1), w=[rowi])
        rowf = S.sb("rowf", [1, NST * 2], F32)
        K.V(lambda e: e.tensor_copy(out=rowf.t[:], in_=rowi.t[:]), r=[rowi], w=[rowf])
        one1 = S.sb("one1", [1, 128], F32)
        K.V(lambda e: e.memset(one1.t[:], 1.0), w=[one1])
        for (c0, c1) in ((0, 512), (512, NST * 2)):
            K.P(lambda e: e.matmul(ypD.t[:, c0:c1], lhsT=one1.t[:], rhs=rowf.t[:, c0:c1], start=True, stop=True),
                r=[one1, rowf], w=[ypD], mark=(c0 == 512))
        e_all = S.sb("e_all", [128, NST], F32)
        K.V(lambda e: e.tensor_copy(out=e_all.t[:], in_=ypD.t[:, 0:NST * 2].rearrange("p (j c) -> p j c", c=2)[:, :, 1]),
            r=[ypD], w=[e_all])
        wf_all = S.sb("wf_all", [128, NST], F32)
        K.V(lambda e: e.scalar_tensor_tensor(out=wf_all.t[:], in0=e_all.t[:], scalar=128.0,
                                             in1=ecl.t[:, 2:3].to_broadcast([128, NST]), op0=ALU.mult, op1=ALU.add),
            r=[e_all, ecl], w=[wf_all])
        eq_all = S.sb("eq_all", [128, NST], F32)
        K.V(lambda e: e.tensor_tensor(out=eq_all.t[:, NW:NST], in0=e_all.t[:, NW:NST], in1=e_all.t[:, 0:NST - NW],
                                      op=ALU.is_equal), r=[e_all], w=[eq_all])
        K.V(lambda e: e.scalar_tensor_tensor(out=wf_all.t[:, NW:NST], in0=eq_all.t[:, NW:NST], scalar=1.0e6,
                                             in1=wf_all.t[:, NW:NST], op0=ALU.mult, op1=ALU.add),
            r=[eq_all, wf_all], w=[wf_all])
        widx_all = S.sb("widx_all", [128, NST], I32)
        K.V(lambda e: e.tensor_copy(out=widx_all.t[:], in_=wf_all.t[:]), r=[wf_all], w=[widx_all])

        NU = NST + NT

        def u_x(u):
            return xs[u % NB]

        def u_w(u):
            return Wt[u % NW] if u < NST else Wsh

        def u_wd(u):
            return u_w(u)

        def u_dst(u):
            return so_d[u * 128:(u + 1) * 128, :]

        def prep_idx(j):
            return

        def prep_x(u):
            if u >= NU:
                return
            i = u % NB
            if u < NST:
                K.igather(xs[i].t[:, :], h2_d[:, :], sinf_all.t[:, u, 0:1], r=[sinf_all], w=[xs[i]])
            else:
                t = u - NST
                K.dma(SP, xs[i].t[:], h2_d[t * 128:(t + 1) * 128, :], w=[xs[i]])

        def prep_w(j):
            if j >= NST:
                return
            K.igather(Wt[j % NW].t[:, :], wall_rows, widx_all.t[:, j:j + 1], r=[widx_all], w=[Wt[j % NW]],
                      bounds=bnd_reg)

        def ph_x(u):
            if u >= NU:
                return
            tp_ = tpD[u % 2]
            xT = xsTD[u % 3]
            x_ = u_x(u)
            for k in range(8):
                K.P(lambda e: e.transpose(out=tp_.t[:, k, :], in_=x_.t[:, k * 128:(k + 1) * 128], identity=idb.t[:]),
                    r=[x_, idb], w=[tp_], mark=(k == 7))
            K.A(lambda e: e.copy(out=xT.t[:], in_=tp_.t[:]), r=[tp_], w=[xT])

        def ph_y(u):
            if u >= NU:
                return
            gu = guD[u % 2]
            xT = xsTD[u % 3]
            Wb = u_w(u)
            for k in range(8):
                K.P(lambda e: e.matmul(gu.t[:], lhsT=xT.t[:, k, :], rhs=Wb.t[:, k * 512:(k + 1) * 512],
                                       start=(k == 0), stop=(k == 7)), r=[xT, Wb], w=[gu], mark=(k == 7))
            K.A(lambda e: e.activation(out=sgD[u % 2].t[:], in_=gu.t[:, 0:256], func=AF.Silu), r=[gu], w=[sgD[u % 2]])
            K.V(lambda e: e.tensor_tensor(out=HD[u % 2].t[:], in0=gu.t[:, 256:512], in1=sgD[u % 2].t[:], op=ALU.mult),
                r=[gu, sgD[u % 2]], w=[HD[u % 2]])

        def ph_z1(u):
            H_ = HD[u % 2]
            for c in range(2):
                K.P(lambda e: e.transpose(out=htpD.t[:, c, :], in_=H_.t[:, c * 128:(c + 1) * 128], identity=idb.t[:]),
                    r=[H_, idb], w=[htpD], mark=(c == 1))
            K.A(lambda e: e.copy(out=HTD[u % 2].t[:], in_=htpD.t[:]), r=[htpD], w=[HTD[u % 2]])

        def ph_z2(u):
            HT = HTD[u % 2]
            Wb = u_wd(u)
            so = soD[u % 2]
            for half in range(2):
                for c in range(2):
                    K.P(lambda e: e.matmul(ypD.t[:, half * 512:(half + 1) * 512], lhsT=HT.t[:, c, :],
                                           rhs=Wb.t[:, 4096 + c * 1024 + half * 512:4096 + c * 1024 + (half + 1) * 512],
                                           start=(c == 0), stop=(c == 1)), r=[HT, Wb], w=[ypD],
                        mark=(c == 1 and half == 1))
            K.V(lambda e: e.tensor_copy(out=so.t[:], in_=ypD.t[:]), r=[ypD], w=[so])
            K.dma(SP, u_dst(u), so.t[:], r=[so])

        for j in range(4):
            prep_idx(j)
        for u in range(3):
            prep_x(u)
        for j in range(3):
            prep_w(j)
        ph_x(0)
        ph_x(1)
        ph_y(0)
        for u in range(NU):
            prep_idx(u + 4)
            prep_x(u + 3)
            prep_w(u + 3)
            ph_x(u + 2)
            ph_z1(u)
            ph_y(u + 1)
            ph_z2(u)

    with K.scope() as S:
        g2b = [mod_bc(S, "g2b%d" % ci, ci, 5) for ci in range(2)]
        p6 = S.sb("p6", [128, NT * 6], I32)
        K.dma(SP, p6.t[:], pos6_d[:, :], w=[p6])
        g6 = S.sb("g6", [128, NT * 6], F32)
        K.dma(SP, g6.t[:], g6_d[:, :], w=[g6])
        gb = [[S.sb("gb%d_%d" % (i, j), [128, 1024], BF16) for j in range(6)] for i in range(2)]
        shd = [S.sb("shd%d" % i, [128, 1024], BF16) for i in range(2)]
        x1l = [S.sb("x1l%d" % i, [128, 1024], F32) for i in range(2)]
        acc = S.sb("acc", [128, 1024], F32)
        yo = [S.sb("yo%d" % i, [128, 1024], F32) for i in range(2)]
        def pre_e(t):
            i = t % 2
            K.dma(SP, shd[i].t[:], so_d[NSLOT + t * 128:NSLOT + (t + 1) * 128, :], w=[shd[i]])
            K.dma(SP, x1l[i].t[:], x1_d[t * 128:(t + 1) * 128, :], w=[x1l[i]])
            for j in range(6):
                K.igather(gb[i][j].t[:, :], so_d[:, :], p6.t[:, t * 6 + j:t * 6 + j + 1], r=[p6], w=[gb[i][j]])

        pre_e(0)
        for t in range(NT):
            i = t % 2
            ci = tile_ci(t)
            if t + 1 < NT:
                pre_e(t + 1)
            prev = shd[i]
            for j in range(6):
                K.V(lambda e: e.scalar_tensor_tensor(out=acc.t[:], in0=gb[i][j].t[:],
                                                     scalar=g6.t[:, t * 6 + j:t * 6 + j + 1], in1=prev.t[:],
                                                     op0=ALU.mult, op1=ALU.add), r=[gb[i][j], g6, prev], w=[acc])
                prev = acc
            K.V(lambda e: e.tensor_tensor(out=acc.t[:], in0=acc.t[:], in1=g2b[ci].t[:], op=ALU.mult),
                r=[acc, g2b[ci]], w=[acc])
            K.V(lambda e: e.tensor_tensor(out=yo[i].t[:], in0=acc.t[:], in1=x1l[i].t[:], op=ALU.add),
                r=[acc, x1l[i]], w=[yo[i]])
            K.dma(SP, y[t * 128:(t + 1) * 128, :], yo[i].t[:], r=[yo[i]])
    return nc


def _consts():
    c = {}
    c["c_ident"] = np.eye(128, dtype=np.float32)
    tok = np.arange(4096)
    row = (tok // 64).astype(np.float64)
    col = (tok % 64).astype(np.float64)

    def tab(p):
        inv = 1.0 / (10000.0 ** (np.arange(p, dtype=np.float64) / p))
        ar = row[:, None] * inv[None, :]
        ac = col[:, None] * inv[None, :]
        cos = np.concatenate([np.cos(ar), np.cos(ar), np.cos(ac), np.cos(ac)], axis=1)
        sin = np.concatenate([-np.sin(ar), np.sin(ar), -np.sin(ac), np.sin(ac)], axis=1)
        t = np.stack([cos, sin], axis=1).astype(np.float32)
        return np.ascontiguousarray(t.reshape(NTS, 128, 2, 4 * p))
    c["c_ropeq"] = tab(8)
    c["c_roper"] = tab(16)
    k = np.arange(128)[:, None]
    cc = np.arange(128)[None, :]
    df = cc - k
    c["c_dexp"] = np.stack([np.where(df >= 0, df, 0), np.where(df < 0, -df, 0)]).astype(np.float32)
    c["c_dmask"] = np.stack([(df >= 0), (df < 0)]).astype(np.float32)
    p = np.arange(128, dtype=np.float32)
    c["c_ecols"] = np.stack([127 - p, p + 1, p, 128 - p], axis=1).astype(np.float32)
    c["c_ltri"] = (np.arange(128)[:, None] < np.arange(128)[None, :]).astype(np.float32)
    c["c_iota"] = np.broadcast_to(np.arange(64, dtype=np.float32)[None, :], (128, 64)).copy()
    return c


_WNAMES = ["w_ada", "b_ada", "norm1_g", "w_in", "mla_q_norm_g", "w_uq", "mla_kv_norm_g", "w_ukv", "mla_q_gain",
           "mla_k_gain", "ret_decay_fwd", "ret_decay_bwd", "ret_norm_g", "w_o", "norm2_g", "router_w", "router_bias",
           "exp_w_gate", "exp_w_up", "exp_w_down", "sh_w_gate", "sh_w_up", "sh_w_down"]


def kernel(**inputs):
    f = lambda a: np.ascontiguousarray(np.asarray(a, dtype=np.float32))
    inp = {k: f(v) for k, v in inputs.items()}
    consts = _consts()
    nc = build_program()
    in_maps = []
    for b in range(8):
        m = {}
        m["x"] = np.ascontiguousarray(np.concatenate(
            [inp["x_sample"][b], inp["x_prompt"][4 * b:4 * b + 4].reshape(1024, 1024)], axis=0))
        m["cckv"] = np.ascontiguousarray(inp["cache_mla_ckv"][b, 0])
        m["ckr"] = np.ascontiguousarray(inp["cache_mla_krope"][b, 0])
        m["sf"] = np.ascontiguousarray(inp["state_ret_fwd"][b, 0])
        m["sb"] = np.ascontiguousarray(inp["state_ret_bwd"][b, 0])
        m["cond"] = np.ascontiguousarray(np.stack([inp["c"][b], inp["c_ctx"]], axis=0))
        for n in _WNAMES:
            m[n] = inp[n]
        m.update(consts)
        in_maps.append(m)
    res = run_bass_kernel_spmd(nc, in_maps, core_ids=list(range(8)))
    R = res.results
    y_sample = np.stack([R[b]["y"][0:4096] for b in range(8)], axis=0)
    y_prompt = np.concatenate([R[b]["y"][4096:].reshape(4, 256, 1024) for b in range(8)], axis=0)
    new_ckv = np.concatenate([R[b]["ockv"].reshape(4, 1, 256, 128) for b in range(8)], axis=0)
    new_kr = np.concatenate([R[b]["okr"].reshape(4, 1, 256, 32) for b in range(8)], axis=0)
    new_rf = np.concatenate([R[b]["orf"].reshape(4, 1, 8, 64, 64) for b in range(8)], axis=0)
    new_rb = np.concatenate([R[b]["orb"].reshape(4, 1, 8, 64, 64) for b in range(8)], axis=0)
    return (y_prompt.astype(np.float32), y_sample.astype(np.float32), new_ckv.astype(np.float32),
            new_kr.astype(np.float32), new_rf.astype(np.float32), new_rb.astype(np.float32))
```

```python
import contextlib
import numpy as np
import concourse.bass as bass
import concourse.mybir as mybir
from concourse.bass_utils import run_bass_kernel_spmd

F32 = mybir.dt.float32
BF16 = mybir.dt.bfloat16
I32 = mybir.dt.int32
AF = mybir.ActivationFunctionType
ALU = mybir.AluOpType
AX = mybir.AxisListType

NT = 40
NTS = 32
TOK = 5120
EPS = 1e-6
NKC = 44
NST = 304
NSLOT = NST * 128


class Buf:
    def __init__(self, t, name):
        self.t = t
        self.name = name
        self.wr = None
        self.rd = {}
        self.ds = {}


class Eng:
    def __init__(self, nc, name, e, self_sync=True):
        self.nc = nc
        self.name = name
        self.e = e
        self.self_sync = self_sync
        self.sem = nc.alloc_semaphore("s_" + name)
        self.cnt = 0
        self.seen = {}
        self.pend_r = []
        self.pend_w = []

    def wait(self, tk):
        if tk is None:
            return
        sem, val, key = tk
        if key == self.name and not self.self_sync:
            return
        if self.seen.get(key, 0) >= val:
            return
        self.e.wait_ge(sem, val)
        self.seen[key] = val


class KB:
    def __init__(self, nc):
        self.nc = nc
        self.PE = Eng(nc, "pe", nc.tensor, self_sync=False)
        self.ACT = Eng(nc, "act", nc.scalar)
        self.DVE = Eng(nc, "dve", nc.vector)
        self.POOL = Eng(nc, "pool", nc.gpsimd)
        self.SP = Eng(nc, "sp", nc.sync)
        self.engs = [self.PE, self.ACT, self.DVE, self.POOL, self.SP]
        self.dma_tk = {}
        self.bsem = nc.alloc_semaphore("s_bar")
        self.bcnt = 0
        self.nsem = 0
        self.uid = 0
        self.free_sems = {"hw": [], "sw": []}
        self.bg_tk = {}

    def op(self, E, fn, r=(), w=(), mark=True):
        for b in r:
            E.wait(b.wr)
        for b in w:
            E.wait(b.wr)
            for tk in list(b.rd.values()):
                E.wait(tk)
        inst = fn(E.e)
        if mark:
            E.cnt += 1
            inst.then_inc(E.sem, 1)
            tk = (E.sem, E.cnt, E.name)
            for b in E.pend_r:
                b.rd[E.name] = tk
            for b in E.pend_w:
                b.wr = tk
                b.rd = {}
            E.pend_r = []
            E.pend_w = []
            for b in r:
                b.rd[E.name] = tk
            for b in w:
                b.wr = tk
                b.rd = {}
        else:
            E.pend_r.extend(r)
            E.pend_w.extend(w)
        return inst

    def P(self, fn, r=(), w=(), mark=True):
        return self.op(self.PE, fn, r, w, mark)

    def A(self, fn, r=(), w=()):
        return self.op(self.ACT, fn, r, w)

    def V(self, fn, r=(), w=()):
        return self.op(self.DVE, fn, r, w)

    def G(self, fn, r=(), w=()):
        return self.op(self.POOL, fn, r, w)

    def _dma_common(self, Q, r, w, issue, bg=False):
        for b in r:
            Q.wait(b.wr)
        for b in w:
            Q.wait(b.wr)
            for tk in list(b.rd.values()):
                Q.wait(tk)
        prim = w[0] if len(w) else r[0]
        kind = "sw" if Q is self.POOL else "hw"
        if kind not in prim.ds:
            if kind == "hw" and self.free_sems[kind]:
                prim.ds[kind] = self.free_sems[kind].pop()
            else:
                prim.ds[kind] = [self.nc.alloc_semaphore("d%d" % self.nsem), "dsem_%d" % self.nsem, 0]
                self.nsem += 1
        ent = prim.ds[kind]
        inst = issue(Q.e)
        ent[2] += 16
        inst.then_inc(ent[0], 16)
        tk = (ent[0], ent[2], ent[1])
        (self.bg_tk if bg else self.dma_tk)[ent[1]] = tk
        for b in r:
            b.rd[ent[1]] = tk
        for b in w:
            b.wr = tk
            b.rd = {}
        return inst

    def dma(self, Q, out, in_, r=(), w=(), noncontig=False, bg=False):
        def issue(e):
            if noncontig:
                with self.nc.allow_non_contiguous_dma(reason="tiny transposed load"):
                    return e.dma_start(out=out, in_=in_)
            return e.dma_start(out=out, in_=in_)
        return self._dma_common(Q, r, w, issue, bg)

    def igather(self, out, src, idx, r=(), w=(), bounds=None):
        if bounds is None:
            return self._dma_common(self.POOL, r, w, lambda e: e.indirect_dma_start(
                out=out, out_offset=None, in_=src, in_offset=bass.IndirectOffsetOnAxis(ap=idx, axis=0)))
        return self._dma_common(self.POOL, r, w, lambda e: e.indirect_dma_start(
            out=out, out_offset=None, in_=src, in_offset=bass.IndirectOffsetOnAxis(ap=idx, axis=0),
            bounds_check=bounds, oob_is_err=False))

    def iscatter(self, dst, idx, in_, r=(), w=()):
        return self._dma_common(self.POOL, r, w, lambda e: e.indirect_dma_start(
            out=dst, out_offset=bass.IndirectOffsetOnAxis(ap=idx, axis=0), in_=in_, in_offset=None))

    def wait_bg(self):
        for tk in list(self.bg_tk.values()):
            self.SP.wait(tk)
        self.bg_tk = {}

    def barrier(self):
        SP = self.SP
        for E in self.engs:
            assert not E.pend_r and not E.pend_w, E.name
            if E is not SP and E.cnt > 0:
                SP.wait((E.sem, E.cnt, E.name))
        for tk in list(self.dma_tk.values()):
            SP.wait(tk)
        self.dma_tk = {}
        self.bcnt += 1
        SP.e.sem_inc(self.bsem, 1)
        for E in self.engs:
            if E is not SP:
                E.e.wait_ge(self.bsem, self.bcnt)

    @contextlib.contextmanager
    def scope(self):
        es = contextlib.ExitStack()
        kb = self
        bufs = []

        class S:
            def sb(self_, name, shape, dt):
                kb.uid += 1
                nm = "%s_%d" % (name, kb.uid)
                t = es.enter_context(kb.nc.sbuf_tensor(nm, shape, dt))
                b = Buf(t, nm)
                bufs.append(b)
                return b

            def ps(self_, name, shape, dt):
                kb.uid += 1
                nm = "%s_%d" % (name, kb.uid)
                t = es.enter_context(kb.nc.psum_tensor(nm, shape, dt))
                return Buf(t, nm)

        with es:
            yield S()
            self.barrier()
            for b in bufs:
                for kind, ent in b.ds.items():
                    self.free_sems[kind].append(ent)


def run_interleaved(jobs, nslots, stagger=0):
    pending = list(jobs)
    live = [None] * nslots
    rnd = 0
    while True:
        progressed = False
        rnd += 1
        for sl in range(nslots):
            if live[sl] is None and pending and rnd > sl * stagger:
                fn, a = pending.pop(0)
                live[sl] = fn(a, sl)
            if live[sl] is not None:
                progressed = True
                try:
                    next(live[sl])
                except StopIteration:
                    live[sl] = None
        if not progressed and not pending:
            break
        if not progressed and rnd > nslots * stagger + 2:
            break


def bc(ap, shape, axis):
    return ap.unsqueeze(axis).to_broadcast(shape)


def build_program():
    nc = bass.Bass("TRN2", target_bir_lowering=False)

    def din(name, shape, dt=F32):
        return nc.dram_tensor(name, list(shape), dt, kind="ExternalInput").ap()

    def dout(name, shape, dt=F32):
        return nc.dram_tensor(name, list(shape), dt, kind="ExternalOutput").ap()

    def dscr(name, shape, dt):
        return nc.dram_tensor(name, list(shape), dt, kind="Internal").ap()

    x = din("x", [TOK, 1024])
    cckv = din("cckv", [512, 128])
    ckr = din("ckr", [512, 32])
    sf_in = din("sf", [8, 64, 64])
    sb_in = din("sb", [8, 64, 64])
    cond = din("cond", [2, 1024])
    w_ada = din("w_ada", [1, 1024, 6144])
    b_ada = din("b_ada", [1, 6144])
    norm1_g = din("norm1_g", [1, 1024])
    w_in = din("w_in", [1, 1024, 2464])
    mla_q_norm_g = din("mla_q_norm_g", [1, 256])
    w_uq = din("w_uq", [1, 256, 768])
    mla_kv_norm_g = din("mla_kv_norm_g", [1, 128])
    w_ukv = din("w_ukv", [1, 128, 1024])
    mla_q_gain = din("mla_q_gain", [1, 96])
    mla_k_gain = din("mla_k_gain", [1, 96])
    ret_decay_fwd = din("ret_decay_fwd", [1, 8])
    ret_decay_bwd = din("ret_decay_bwd", [1, 8])
    ret_norm_g = din("ret_norm_g", [1, 64])
    w_o = din("w_o", [1, 1024, 1024])
    norm2_g = din("norm2_g", [1, 1024])
    router_w = din("router_w", [1, 1024, 64])
    router_bias = din("router_bias", [1, 64])
    exp_w_gate = din("exp_w_gate", [1, 64, 1024, 256])
    exp_w_up = din("exp_w_up", [1, 64, 1024, 256])
    exp_w_down = din("exp_w_down", [1, 64, 256, 1024])
    sh_w_gate = din("sh_w_gate", [1, 1024, 256])
    sh_w_up = din("sh_w_up", [1, 1024, 256])
    sh_w_down = din("sh_w_down", [1, 256, 1024])
    ident = din("c_ident", [128, 128])
    ropeq = din("c_ropeq", [NTS, 128, 2, 32])
    roper = din("c_roper", [NTS, 128, 2, 64])
    dexp = din("c_dexp", [2, 128, 128])
    dmask = din("c_dmask", [2, 128, 128])
    ecols = din("c_ecols", [128, 4])
    ltri_c = din("c_ltri", [128, 128])
    iota_c = din("c_iota", [128, 64])

    y = dout("y", [TOK, 1024])
    ockv = dout("ockv", [1024, 128])
    okr = dout("okr", [1024, 32])
    orf = dout("orf", [4, 8, 64, 64])
    orb = dout("orb", [4, 8, 64, 64])

    mod_d = dscr("mod_d", [2, 6144], F32)
    qT_d = dscr("qT_d", [8, 96, TOK], BF16)
    kT_d = dscr("kT_d", [8, 96, NKC * 128], BF16)
    v_d = dscr("v_d", [8, 128, NKC, 65], BF16)
    rqT_d = dscr("rqT_d", [NT, 64, 8, 128], BF16)
    rkT_d = dscr("rkT_d", [NT, 64, 8, 128], BF16)
    rk_d = dscr("rk_d", [NT, 128, 512], BF16)
    rv_d = dscr("rv_d", [NT, 128, 512], BF16)
    rg_d = dscr("rg_d", [NT, 128, 512], F32)
    mixT_d = dscr("mixT_d", [128, 8, TOK], BF16)
    x1_d = dscr("x1_d", [TOK, 1024], F32)
    h2T_d = dscr("h2T_d", [128, 8, TOK], BF16)
    gates_d = dscr("gates_d", [TOK, 64], F32)
    h2_d = dscr("h2_d", [TOK, 1024], BF16)
    wall_d = dscr("wall_d", [64, 128, 6144], BF16)
    sinfo_d = dscr("sinfo_d", [NSLOT, 2], I32)
    pos6_d = dscr("pos6_d", [128, NT * 6], I32)
    g6_d = dscr("g6_d", [128, NT * 6], F32)
    so_d = dscr("so_d", [NSLOT + TOK, 1024], BF16)

    K = KB(nc)
    SP, POOL = K.SP, K.POOL
    wall_b = Buf(None, "wall_b")
    sinfo_b = Buf(None, "sinfo_b")

    rs_cache = {}

    def rstd_of(S, ssq, n, name):
        shape = list(ssq.t[:].shape)
        ck = (id(S), name)
        if ck not in rs_cache:
            rs_cache[ck] = (S.sb(name + "_v", shape, F32), S.sb(name + "_s", shape, F32), S.sb(name + "_r", shape, F32))
        v, s, o = rs_cache[ck]
        K.V(lambda e: e.tensor_scalar(out=v.t[:], in0=ssq.t[:], scalar1=1.0 / n, scalar2=EPS,
                                      op0=ALU.mult, op1=ALU.add), r=[ssq], w=[v])
        K.A(lambda e: e.activation(out=s.t[:], in_=v.t[:], func=AF.Sqrt), r=[v], w=[s])
        K.V(lambda e: e.reciprocal(out=o.t[:], in_=s.t[:]), r=[s], w=[o])
        return o

    with K.scope() as S:
        cT = S.sb("cT", [128, 2, 8], F32)
        for c_ in range(2):
            K.dma(SP, cT.t[:, c_, :], cond[c_].rearrange("(k p) -> p k", p=128), w=[cT], noncontig=True)
        scT = S.sb("scT", [128, 2, 8], F32)
        K.A(lambda e: e.activation(out=scT.t[:], in_=cT.t[:], func=AF.Silu), r=[cT], w=[scT])
        bada = S.sb("bada", [2, 6144], F32)
        K.dma(SP, bada.t[:], b_ada[0:1, :].partition_broadcast(2), w=[bada])
        n1g = S.sb("n1g", [2, 1024], F32)
        K.dma(SP, n1g.t[:], norm1_g[0:1, :].partition_broadcast(2), w=[n1g])
        n2g = S.sb("n2g", [2, 1024], F32)
        K.dma(SP, n2g.t[:], norm2_g[0:1, :].partition_broadcast(2), w=[n2g])
        mod = S.sb("mod", [2, 6144], F32)
        wb = [S.sb("wa%d" % i, [128, 8, 512], F32) for i in range(3)]
        pp = [S.ps("pa%d" % i, [2, 512], F32) for i in range(2)]
        for g in range(12):
            wt = wb[g % 3]
            p = pp[g % 2]
            K.dma(SP, wt.t[:], w_ada[0, :, g * 512:(g + 1) * 512].rearrange("(k p) n -> p k n", p=128), w=[wt])
            for k in range(8):
                K.P(lambda e: e.matmul(p.t[:], lhsT=scT.t[:, :, k], rhs=wt.t[:, k, :],
                                       start=(k == 0), stop=(k == 7)), r=[scT, wt], w=[p], mark=(k == 7))
            K.V(lambda e: e.tensor_tensor(out=mod.t[:, g * 512:(g + 1) * 512], in0=p.t[:],
                                          in1=bada.t[:, g * 512:(g + 1) * 512], op=ALU.add), r=[p, bada], w=[mod])
        md = S.sb("md", [2, 6144], F32)

        def seg(b, i):
            return b.t[:, i * 1024:(i + 1) * 1024]
        K.V(lambda e: e.scalar_tensor_tensor(out=seg(md, 0), in0=seg(mod, 1), scalar=1.0, in1=n1g.t[:],
                                             op0=ALU.add, op1=ALU.mult), r=[mod, n1g], w=[md])
        K.V(lambda e: e.tensor_copy(out=seg(md, 1), in_=seg(mod, 0)), r=[mod], w=[md])
        K.V(lambda e: e.tensor_copy(out=seg(md, 2), in_=seg(mod, 2)), r=[mod], w=[md])
        K.V(lambda e: e.scalar_tensor_tensor(out=seg(md, 3), in0=seg(mod, 4), scalar=1.0, in1=n2g.t[:],
                                             op0=ALU.add, op1=ALU.mult), r=[mod, n2g], w=[md])
        K.V(lambda e: e.tensor_copy(out=seg(md, 4), in_=seg(mod, 3)), r=[mod], w=[md])
        K.V(lambda e: e.tensor_copy(out=seg(md, 5), in_=seg(mod, 5)), r=[mod], w=[md])
        K.dma(SP, mod_d[:, :], md.t[:], r=[md])

    def load_bc(S, name, src_row, n):
        b = S.sb(name, [128, n], F32)
        K.dma(SP, b.t[:], src_row.partition_broadcast(128), w=[b])
        return b

    def mod_bc(S, name, ci, i):
        return load_bc(S, name, mod_d[ci:ci + 1, i * 1024:(i + 1) * 1024], 1024)

    def make_ident(S):
        idf = S.sb("idf", [128, 128], F32)
        K.dma(SP, idf.t[:], ident[:, :], w=[idf])
        idb = S.sb("idb", [128, 128], BF16)
        K.V(lambda e: e.tensor_copy(out=idb.t[:], in_=idf.t[:]), r=[idf], w=[idb])
        return idb

    def tile_ci(t):
        return 0 if t < NTS else 1

    with K.scope() as S:
        idb = make_ident(S)
        win = S.sb("win", [128, 8, 2464], BF16)
        for k in range(8):
            K.dma(POOL, win.t[:, k, :], w_in[0, k * 128:(k + 1) * 128, :], w=[win])
        wuq = S.sb("wuq", [128, 2, 768], BF16)
        K.dma(POOL, wuq.t[:], w_uq[0].rearrange("(k p) n -> p k n", p=128), w=[wuq])
        wukv = S.sb("wukv", [128, 1024], BF16)
        K.dma(POOL, wukv.t[:], w_ukv[0], w=[wukv])
        precast = []
        for e_ in range(64):
            gu_v = wall_d[e_][:, 0:4096].rearrange("p (k n) -> p k n", k=8)
            precast.append((gu_v[:, :, 0:256], exp_w_gate[0, e_].rearrange("(k p) n -> p k n", p=128)))
            precast.append((gu_v[:, :, 256:512], exp_w_up[0, e_].rearrange("(k p) n -> p k n", p=128)))
            precast.append((wall_d[e_][:, 4096:6144].rearrange("p (c n) -> p c n", c=2),
                            exp_w_down[0, e_].rearrange("(c p) n -> p c n", p=128)))

        def issue_precast(n):
            for _ in range(n):
                if precast:
                    o_, i_ = precast.pop(0)
                    K.dma(POOL, o_, i_, r=[wall_b], bg=True)
        gm1b = [mod_bc(S, "gm1b%d" % ci, ci, 0) for ci in range(2)]
        sh1b = [mod_bc(S, "sh1b%d" % ci, ci, 1) for ci in range(2)]
        gqb = load_bc(S, "gqb", mla_q_norm_g[0:1, :], 256)
        gkvb = load_bc(S, "gkvb", mla_kv_norm_g[0:1, :], 128)
        qgb = load_bc(S, "qgb", mla_q_gain[0:1, :], 96)
        kgb = load_bc(S, "kgb", mla_k_gain[0:1, :], 96)

        NSL = 2

        def mk(name, shape, dt):
            return [S.sb("%s_s%d" % (name, i), shape, dt) for i in range(NSL)]
        junk = mk("junk", [128, 1024], BF16)
        xt = mk("xt", [128, 1024], F32)
        tmpf = mk("tmpf", [128, 1024], F32)
        hb = mk("hb", [128, 1024], BF16)
        hT = mk("hT", [128, 8, 128], BF16)
        ssq_x = mk("ssq_x", [128, 1], F32)
        ssq_q = mk("ssq_q", [128, 1], F32)
        ssq_kv = mk("ssq_kv", [128, 1], F32)
        cqn = mk("cqn", [128, 256], BF16)
        ckvn = mk("ckvn", [128, 128], F32)
        ckvnb = mk("ckvnb", [128, 128], BF16)
        krf = mk("krf", [128, 32], F32)
        cT3 = mk("cT3", [128, 3, 128], BF16)
        sqf = mk("sqf", [128, 768], F32)
        ssqh = mk("ssqh", [128, 8], F32)
        qn = mk("qn", [128, 8, 96], F32)
        kcat = mk("kcat", [128, 8, 96], F32)
        ra = mk("ra", [128, 8, 64], F32)
        rb = mk("rb", [128, 8, 64], F32)
        qb = mk("qb", [128, 8, 96], BF16)
        qT = mk("qT", [96, 8, 128], BF16)
        kT = mk("kT", [96, 8, 128], BF16)
        vaug = mk("vaug", [128, 8, 65], BF16)
        for i in range(NSL):
            K.V(lambda e: e.memset(vaug[i].t[:], 1.0), w=[vaug[i]])
        rf = mk("rf", [128, 8, 64], F32)
        rqb = mk("rqb", [128, 8, 64], BF16)
        rkb = mk("rkb", [128, 8, 64], BF16)
        rqT = mk("rqT", [64, 8, 128], BF16)
        rkT = mk("rkT", [64, 8, 128], BF16)
        rvb = mk("rvb", [128, 512], BF16)
        rgf = mk("rgf", [128, 512], F32)
        rpq = mk("rpq", [128, 2, 32], F32)
        rpr = mk("rpr", [128, 2, 64], F32)

        tp = [S.ps("tp%d" % i, [128, 8, 128], BF16) for i in range(NSL)]
        zp = [S.ps("zp%d" % i, [128, 512], F32) for i in range(NSL)]
        big = [S.ps("big%d" % i, [128, 1024], F32) for i in range(NSL)]

        ra2 = mk("ra2", [128, 8, 64], F32)
        rb2 = mk("rb2", [128, 8, 64], F32)

        def rope(i, xb, xv, R, tab, alt=False):
            hf = R // 4
            ra_ = ra2 if alt else ra
            rb_ = rb2 if alt else rb
            rav = ra_[i].t[:, :, 0:R]
            rbv = rb_[i].t[:, :, 0:R]
            K.V(lambda e: e.tensor_tensor(out=rav, in0=xv, in1=bc(tab.t[:, 0, :], [128, 8, R], 1), op=ALU.mult),
                r=[xb, tab], w=[ra_[i]])
            x5 = xv.rearrange("p h (a f j) -> p h a f j", a=2, f=2)
            r5 = rbv.rearrange("p h (a f j) -> p h a f j", a=2, f=2)
            s5 = tab.t[:, 1, :].rearrange("p (a f j) -> p a f j", a=2, f=2)
            for f in range(2):
                K.V(lambda e: e.tensor_tensor(out=r5[:, :, :, f, :], in0=x5[:, :, :, 1 - f, :],
                                              in1=bc(s5[:, :, f, :], [128, 8, 2, hf], 1), op=ALU.mult),
                    r=[xb, tab], w=[rb_[i]])
            K.V(lambda e: e.tensor_tensor(out=xv, in0=rav, in1=rbv, op=ALU.add), r=[ra_[i], rb_[i]], w=[xb])

        def qk_proc(i, srcb, srcv, gainb, tab, outT, dst_ap):
            K.A(lambda e: e.activation(out=sqf[i].t[:].rearrange("p (h d) -> p h d", h=8), in_=srcv, func=AF.Square),
                r=[srcb], w=[sqf[i]])
            K.V(lambda e: e.reduce_sum(out=ssqh[i].t[:], in_=sqf[i].t[:].rearrange("p (h d) -> p h d", h=8), axis=AX.X),
                r=[sqf[i]], w=[ssqh[i]])
            yield
            rs = rstd_of(S, ssqh[i], 96, "rsh%d" % i)
            yield
            K.V(lambda e: e.tensor_tensor(out=qn[i].t[:], in0=srcv, in1=bc(rs.t[:, :], [128, 8, 96], 2), op=ALU.mult),
                r=[srcb, rs], w=[qn[i]])
            K.V(lambda e: e.tensor_tensor(out=qn[i].t[:], in0=qn[i].t[:], in1=bc(gainb.t[:, :], [128, 8, 96], 1),
                                          op=ALU.mult), r=[qn[i], gainb], w=[qn[i]])
            yield
            if tab is not None:
                rope(i, qn[i], qn[i].t[:, :, 64:96], 32, tab)
                yield
            K.A(lambda e: e.copy(out=qb[i].t[:], in_=qn[i].t[:]), r=[qn[i]], w=[qb[i]])
            yield
            t_ = tp[i]
            for h in range(8):
                K.P(lambda e: e.transpose(out=t_.t[0:96, h, :], in_=qb[i].t[:, h, :], identity=idb.t[:]),
                    r=[qb[i], idb], w=[t_], mark=(h == 7))
            K.A(lambda e: e.copy(out=outT.t[:], in_=t_.t[0:96, :, :]), r=[t_], w=[outT])
            K.dma(SP, dst_ap, outT.t[:], r=[outT])
            yield

        def kv_part(i, tab, gc):
            kvp = big[i]
            for cg in range(2):
                K.P(lambda e: e.matmul(kvp.t[:, cg * 512:(cg + 1) * 512], lhsT=cT3[i].t[:, 2, :],
                                       rhs=wukv.t[:, cg * 512:(cg + 1) * 512], start=True, stop=True),
                    r=[cT3[i], wukv], w=[kvp], mark=(cg == 1))
            yield
            kv3 = kvp.t[:].rearrange("p (h d) -> p h d", h=8)
            K.A(lambda e: e.copy(out=kcat[i].t[:, :, 0:64], in_=kv3[:, :, 0:64]), r=[kvp], w=[kcat[i]])
            K.V(lambda e: e.tensor_copy(out=kcat[i].t[:, :, 64:96], in_=bc(krf[i].t[:, :], [128, 8, 32], 1)),
                r=[krf[i]], w=[kcat[i]])
            K.A(lambda e: e.copy(out=vaug[i].t[:, :, 0:64], in_=kv3[:, :, 64:128]), r=[kvp], w=[vaug[i]])
            K.dma(SP, v_d[:, :, gc, :].rearrange("h p d -> p h d"), vaug[i].t[:], r=[vaug[i]])
            yield
            yield from qk_proc(i, kcat[i], kcat[i].t[:], kgb, tab, kT[i],
                               kT_d[:, :, gc * 128:(gc + 1) * 128].rearrange("h d t -> d h t"))

        def tileA(t, i):
            ci = tile_ci(t)
            sample = t < NTS
            gc = t if sample else 36 + (t - NTS)
            issue_precast(5)
            K.dma(SP, xt[i].t[:], x[t * 128:(t + 1) * 128, :], w=[xt[i]])
            if sample:
                K.dma(SP, rpq[i].t[:], ropeq[t], w=[rpq[i]])
                K.dma(SP, rpr[i].t[:], roper[t], w=[rpr[i]])
            yield
            K.A(lambda e: e.activation(out=junk[i].t[:], in_=xt[i].t[:], func=AF.Square, accum_out=ssq_x[i].t[:]),
                r=[xt[i]], w=[junk[i], ssq_x[i]])
            yield
            rs = rstd_of(S, ssq_x[i], 1024, "rsx%d" % i)
            yield
            K.V(lambda e: e.scalar_tensor_tensor(out=tmpf[i].t[:], in0=xt[i].t[:], scalar=rs.t[:, 0:1],
                                                 in1=gm1b[ci].t[:], op0=ALU.mult, op1=ALU.mult),
                r=[xt[i], rs, gm1b[ci]], w=[tmpf[i]])
            K.G(lambda e: e.tensor_tensor(out=hb[i].t[:], in0=tmpf[i].t[:], in1=sh1b[ci].t[:], op=ALU.add),
                r=[tmpf[i], sh1b[ci]], w=[hb[i]])
            yield
            t_ = tp[i]
            for k in range(8):
                K.P(lambda e: e.transpose(out=t_.t[:, k, :], in_=hb[i].t[:, k * 128:(k + 1) * 128], identity=idb.t[:]),
                    r=[hb[i], idb], w=[t_], mark=(k == 7))
            K.A(lambda e: e.copy(out=hT[i].t[:], in_=t_.t[:]), r=[t_], w=[hT[i]])
            yield

            def zgroup(c0, c1):
                z = zp[i]
                for k in range(8):
                    K.P(lambda e: e.matmul(z.t[:, 0:c1 - c0], lhsT=hT[i].t[:, k, :], rhs=win.t[:, k, c0:c1],
                                           start=(k == 0), stop=(k == 7)), r=[hT[i], win], w=[z], mark=(k == 7))
                return z

            z0 = zgroup(0, 416)
            yield
            K.A(lambda e: e.activation(out=junk[i].t[:, 0:256], in_=z0.t[:, 0:256], func=AF.Square,
                                       accum_out=ssq_q[i].t[:]), r=[z0], w=[junk[i], ssq_q[i]])
            K.A(lambda e: e.activation(out=junk[i].t[:, 256:384], in_=z0.t[:, 256:384], func=AF.Square,
                                       accum_out=ssq_kv[i].t[:]), r=[z0], w=[junk[i], ssq_kv[i]])
            yield
            rq_ = rstd_of(S, ssq_q[i], 256, "rsq%d" % i)
            rkv_ = rstd_of(S, ssq_kv[i], 128, "rskv%d" % i)
            yield
            K.V(lambda e: e.scalar_tensor_tensor(out=cqn[i].t[:], in0=z0.t[:, 0:256], scalar=rq_.t[:, 0:1],
                                                 in1=gqb.t[:], op0=ALU.mult, op1=ALU.mult),
                r=[z0, rq_, gqb], w=[cqn[i]])
            K.V(lambda e: e.scalar_tensor_tensor(out=ckvn[i].t[:], in0=z0.t[:, 256:384], scalar=rkv_.t[:, 0:1],
                                                 in1=gkvb.t[:], op0=ALU.mult, op1=ALU.mult),
                r=[z0, rkv_, gkvb], w=[ckvn[i]])
            K.A(lambda e: e.copy(out=krf[i].t[:], in_=z0.t[:, 384:416]), r=[z0], w=[krf[i]])
            K.A(lambda e: e.copy(out=ckvnb[i].t[:], in_=ckvn[i].t[:]), r=[ckvn[i]], w=[ckvnb[i]])
            if not sample:
                pr = (t - NTS) * 128
                K.dma(SP, ockv[pr:pr + 128, :], ckvn[i].t[:], r=[ckvn[i]])
                K.dma(SP, okr[pr:pr + 128, :], krf[i].t[:], r=[krf[i]])
            yield
            K.P(lambda e: e.transpose(out=t_.t[:, 0, :], in_=cqn[i].t[:, 0:128], identity=idb.t[:]),
                r=[cqn[i], idb], w=[t_], mark=False)
            K.P(lambda e: e.transpose(out=t_.t[:, 1, :], in_=cqn[i].t[:, 128:256], identity=idb.t[:]),
                r=[cqn[i], idb], w=[t_], mark=False)
            K.P(lambda e: e.transpose(out=t_.t[:, 2, :], in_=ckvnb[i].t[:], identity=idb.t[:]),
                r=[ckvnb[i], idb], w=[t_])
            K.A(lambda e: e.copy(out=cT3[i].t[:], in_=t_.t[:, 0:3, :]), r=[t_], w=[cT3[i]])
            yield
            qp = big[i]
            for (c0, c1) in ((0, 512), (512, 768)):
                for k in range(2):
                    K.P(lambda e: e.matmul(qp.t[:, c0:c1], lhsT=cT3[i].t[:, k, :], rhs=wuq.t[:, k, c0:c1],
                                           start=(k == 0), stop=(k == 1)), r=[cT3[i], wuq], w=[qp],
                        mark=(k == 1 and c0 == 512))
            yield
            def chain_m():
                yield from qk_proc(i, qp, qp.t[:, 0:768].rearrange("p (h d) -> p h d", h=8), qgb,
                                   rpq[i] if sample else None, qT[i],
                                   qT_d[:, :, t * 128:(t + 1) * 128].rearrange("h d t -> d h t"))
                yield from kv_part(i, rpq[i] if sample else None, gc)

            def chain_r():
                for which, (c0, c1) in enumerate(((416, 928), (928, 1440))):
                    z = zgroup(c0, c1)
                    yield
                    sc = 1.0 if which == 0 else 0.125
                    K.A(lambda e: e.activation(out=rf[i].t[:].rearrange("p h d -> p (h d)"), in_=z.t[:], func=AF.Copy,
                                               scale=sc), r=[z], w=[rf[i]])
                    yield
                    if sample:
                        rope(i, rf[i], rf[i].t[:], 64, rpr[i], alt=True)
                        yield
                    ob = rqb[i] if which == 0 else rkb[i]
                    K.A(lambda e: e.copy(out=ob.t[:], in_=rf[i].t[:]), r=[rf[i]], w=[ob])
                    yield
                    t2_ = tp[i]
                    for h in range(8):
                        K.P(lambda e: e.transpose(out=t2_.t[0:64, h, :], in_=ob.t[:, h, :], identity=idb.t[:]),
                            r=[ob, idb], w=[t2_], mark=(h == 7))
                    oT = rqT[i] if which == 0 else rkT[i]
                    K.A(lambda e: e.copy(out=oT.t[:], in_=t2_.t[0:64, :, :]), r=[t2_], w=[oT])
                    K.dma(SP, (rqT_d if which == 0 else rkT_d)[t], oT.t[:], r=[oT])
                    if which == 1:
                        K.dma(SP, rk_d[t], rkb[i].t[:].rearrange("p h d -> p (h d)"), r=[rkb[i]])
                    yield
                z = zgroup(1440, 1952)
                yield
                K.A(lambda e: e.copy(out=rvb[i].t[:], in_=z.t[:]), r=[z], w=[rvb[i]])
                K.dma(SP, rv_d[t], rvb[i].t[:], r=[rvb[i]])
                yield
                z = zgroup(1952, 2464)
                yield
                K.A(lambda e: e.activation(out=rgf[i].t[:], in_=z.t[:], func=AF.Silu), r=[z], w=[rgf[i]])
                K.dma(SP, rg_d[t], rgf[i].t[:], r=[rgf[i]])
                yield

            subs = [chain_m(), chain_r()]
            while subs:
                for g_ in list(subs):
                    try:
                        next(g_)
                    except StopIteration:
                        subs.remove(g_)
                yield

        def tileCtx(j, i):
            K.dma(SP, ckvn[i].t[:], cckv[j * 128:(j + 1) * 128, :], w=[ckvn[i]])
            K.dma(SP, krf[i].t[:], ckr[j * 128:(j + 1) * 128, :], w=[krf[i]])
            yield
            K.A(lambda e: e.copy(out=ckvnb[i].t[:], in_=ckvn[i].t[:]), r=[ckvn[i]], w=[ckvnb[i]])
            yield
            t_ = tp[i]
            K.P(lambda e: e.transpose(out=t_.t[:, 2, :], in_=ckvnb[i].t[:], identity=idb.t[:]),
                r=[ckvnb[i], idb], w=[t_])
            K.A(lambda e: e.copy(out=cT3[i].t[:, 2, :], in_=t_.t[:, 2, :]), r=[t_], w=[cT3[i]])
            yield
            yield from kv_part(i, None, 32 + j)

        jobs = [(tileA, t) for t in range(NT)] + [(tileCtx, j) for j in range(4)]
        run_interleaved(jobs, NSL, stagger=0)
        issue_precast(1000)

    with K.scope() as S:
        idb = make_ident(S)
        lg = []
        for d, src in enumerate((ret_decay_fwd, ret_decay_bwd)):
            raw = load_bc(S, "lgraw%d" % d, src[0:1, :], 8)
            ex = S.sb("lgex%d" % d, [128, 8], F32)
            K.A(lambda e: e.activation(out=ex.t[:], in_=raw.t[:], func=AF.Exp), r=[raw], w=[ex])
            l_ = S.sb("lg%d" % d, [128, 8], F32)
            K.V(lambda e: e.tensor_single_scalar(out=l_.t[:], in_=ex.t[:], scalar=-1.0, op=ALU.mult), r=[ex], w=[l_])
            lg.append(l_)
        ec = S.sb("ec", [128, 4], F32)
        K.dma(SP, ec.t[:], ecols[:, :], w=[ec])
        rngb = load_bc(S, "rngb", ret_norm_g[0:1, :], 64)
        maskT, kdec, qdec, cdec = [], [], [], []
        for d in range(2):
            de = S.sb("de%d" % d, [128, 128], F32)
            K.dma(SP, de.t[:], dexp[d], w=[de])
            dm = S.sb("dm%d" % d, [128, 128], F32)
            K.dma(SP, dm.t[:], dmask[d], w=[dm])
            mt = S.sb("mt%d" % d, [128, 8, 128], F32)
            for h in range(8):
                K.A(lambda e: e.activation(out=mt.t[:, h, :], in_=de.t[:], func=AF.Exp, scale=lg[d].t[:, h:h + 1]),
                    r=[de, lg[d]], w=[mt])
            K.V(lambda e: e.tensor_tensor(out=mt.t[:], in0=mt.t[:], in1=bc(dm.t[:, :], [128, 8, 128], 1),
                                          op=ALU.mult), r=[mt, dm], w=[mt])
            maskT.append(mt)
            kd_ = S.sb("kdec%d" % d, [128, 8], F32)
            K.A(lambda e: e.activation(out=kd_.t[:], in_=lg[d].t[:], func=AF.Exp, scale=ec.t[:, 2 * d:2 * d + 1]),
                r=[lg[d], ec], w=[kd_])
            qd_ = S.sb("qdec%d" % d, [128, 8], F32)
            K.A(lambda e: e.activation(out=qd_.t[:], in_=lg[d].t[:], func=AF.Exp,
                                       scale=ec.t[:, 2 * d + 1:2 * d + 2]), r=[lg[d], ec], w=[qd_])
            cd_ = S.sb("cdec%d" % d, [128, 8], F32)
            K.A(lambda e: e.activation(out=cd_.t[:], in_=lg[d].t[:], func=AF.Exp, scale=128.0), r=[lg[d]], w=[cd_])
            kdec.append(kd_)
            qdec.append(qd_)
            cdec.append(cd_)

        OF = S.sb("OF", [128, NTS, 512], F32)
        NRB = 3
        rqTc = [S.sb("rqTc%d" % i, [64, 8, 128], BF16) for i in range(NRB)]
        rkTc = [S.sb("rkTc%d" % i, [64, 8, 128], BF16) for i in range(NRB)]
        rkc = [S.sb("rkc%d" % i, [128, 8, 64], BF16) for i in range(NRB)]
        rvc = [S.sb("rvc%d" % i, [128, 8, 64], BF16) for i in range(NRB)]
        rgc = [S.sb("rgc%d" % i, [128, 512], F32) for i in range(NRB)]
        STm = [S.sb("STm%d" % i, [128, 8, 128], BF16) for i in range(2)]
        kdb = [S.sb("kdb%d" % i, [128, 8, 64], BF16) for i in range(2)]
        t1 = S.sb("t1", [128, 8, 64], F32)
        ob_ = S.sb("ob_", [128, 8, 64], F32)
        Sst = S.sb("Sst", [64, 8, 64], F32)
        Sbf = S.sb("Sbf", [64, 8, 64], BF16)
        sq2 = S.sb("sq2", [128, 512], F32)
        ssq2 = S.sb("ssq2", [128, 8], F32)
        mixr = S.sb("mixr", [128, 512], BF16)
        mT = [S.sb("mTr%d" % i, [128, 4, 128], BF16) for i in range(2)]
        STp = [S.ps("STp%d" % i, [128, 8, 128], F32) for i in range(2)]
        INp = S.ps("INp", [128, 8, 64], F32)
        CRp = S.ps("CRp", [128, 8, 64], F32)
        KVp = S.ps("KVp", [64, 8, 64], F32)
        tpr = S.ps("tpr", [128, 4, 128], BF16)

        seqs = [(0, NTS, None)] + [(NTS + 2 * p, 2, p) for p in range(4)]
        units = []
        for (t0, n, pidx) in seqs:
            for d in range(2):
                order = list(range(n)) if d == 0 else list(range(n - 1, -1, -1))
                for k_, c in enumerate(order):
                    units.append(dict(t=t0 + c, c=c, d=d, pidx=pidx, first=(k_ == 0), last=(k_ == n - 1)))

        def pre_r(u):
            U = units[u]
            t, d = U["t"], U["d"]
            i = u % NRB
            K.dma(SP, rqTc[i].t[:], rqT_d[t], w=[rqTc[i]])
            K.dma(SP, rkTc[i].t[:], rkT_d[t], w=[rkTc[i]])
            K.dma(SP, rkc[i].t[:].rearrange("p h d -> p (h d)"), rk_d[t], w=[rkc[i]])
            K.dma(SP, rvc[i].t[:].rearrange("p h d -> p (h d)"), rv_d[t], w=[rvc[i]])
            if d == 1:
                K.dma(SP, rgc[i].t[:], rg_d[t], w=[rgc[i]])
            sp_ = STp[u % 2]
            for h in range(8):
                K.P(lambda e: e.matmul(sp_.t[:, h, :], lhsT=rkTc[i].t[:, h, :], rhs=rqTc[i].t[:, h, :],
                                       start=True, stop=True), r=[rkTc[i], rqTc[i]], w=[sp_], mark=(h == 7))
            K.V(lambda e: e.tensor_tensor(out=STm[u % 2].t[:], in0=sp_.t[:], in1=maskT[d].t[:], op=ALU.mult),
                r=[sp_, maskT[d]], w=[STm[u % 2]])
            K.V(lambda e: e.tensor_tensor(out=kdb[u % 2].t[:], in0=rkc[i].t[:], in1=bc(kdec[d].t[:, :], [128, 8, 64], 2),
                                          op=ALU.mult), r=[rkc[i], kdec[d]], w=[kdb[u % 2]])

        def main_r(u):
            U = units[u]
            t, c, d, pidx = U["t"], U["c"], U["d"], U["pidx"]
            i = u % NRB
            sm = STm[u % 2]
            kd_ = kdb[u % 2]
            if U["first"]:
                if pidx is None:
                    K.dma(SP, Sst.t[:], (sf_in if d == 0 else sb_in).rearrange("h d e -> d h e"), w=[Sst])
                else:
                    K.V(lambda e: e.memset(Sst.t[:], 0.0), w=[Sst])
                K.A(lambda e: e.copy(out=Sbf.t[:], in_=Sst.t[:]), r=[Sst], w=[Sbf])
            for h in range(8):
                K.P(lambda e: e.matmul(INp.t[:, h, :], lhsT=sm.t[:, h, :], rhs=rvc[i].t[:, h, :],
                                       start=True, stop=True), r=[sm, rvc[i]], w=[INp], mark=(h == 7))
            for h in range(8):
                K.P(lambda e: e.matmul(KVp.t[:, h, :], lhsT=kd_.t[:, h, :], rhs=rvc[i].t[:, h, :],
                                       start=True, stop=True), r=[kd_, rvc[i]], w=[KVp], mark=(h == 7))
            for h in range(8):
                K.P(lambda e: e.matmul(CRp.t[:, h, :], lhsT=rqTc[i].t[:, h, :], rhs=Sbf.t[:, h, :],
                                       start=True, stop=True), r=[rqTc[i], Sbf], w=[CRp], mark=(h == 7))
            K.V(lambda e: e.tensor_tensor(out=Sst.t[:], in0=Sst.t[:], in1=bc(cdec[d].t[0:64, :], [64, 8, 64], 2),
                                          op=ALU.mult), r=[Sst, cdec[d]], w=[Sst])
            K.V(lambda e: e.tensor_tensor(out=t1.t[:], in0=CRp.t[:], in1=bc(qdec[d].t[:, :], [128, 8, 64], 2),
                                          op=ALU.mult), r=[CRp, qdec[d]], w=[t1])
            K.V(lambda e: e.tensor_tensor(out=Sst.t[:], in0=Sst.t[:], in1=KVp.t[:], op=ALU.add),
                r=[Sst, KVp], w=[Sst])
            K.A(lambda e: e.copy(out=Sbf.t[:], in_=Sst.t[:]), r=[Sst], w=[Sbf])
            ofv = OF.t[:, c, :].rearrange("p (h d) -> p h d", h=8)
            if d == 0:
                K.V(lambda e: e.tensor_tensor(out=ofv, in0=t1.t[:], in1=INp.t[:], op=ALU.add),
                    r=[t1, INp], w=[OF])
            else:
                K.V(lambda e: e.tensor_tensor(out=ob_.t[:], in0=t1.t[:], in1=INp.t[:], op=ALU.add),
                    r=[t1, INp], w=[ob_])
                K.V(lambda e: e.tensor_tensor(out=ob_.t[:], in0=ob_.t[:], in1=ofv, op=ALU.add),
                    r=[ob_, OF], w=[ob_])
                o2 = ob_.t[:].rearrange("p h d -> p (h d)")
                K.A(lambda e: e.activation(out=sq2.t[:], in_=o2, func=AF.Square), r=[ob_], w=[sq2])
                K.V(lambda e: e.reduce_sum(out=ssq2.t[:], in_=sq2.t[:].rearrange("p (h d) -> p h d", h=8),
                                           axis=AX.X), r=[sq2], w=[ssq2])
                rs = rstd_of(S, ssq2, 64, "rso")
                K.V(lambda e: e.tensor_tensor(out=ob_.t[:], in0=ob_.t[:], in1=bc(rs.t[:, :], [128, 8, 64], 2),
                                              op=ALU.mult), r=[ob_, rs], w=[ob_])
                K.V(lambda e: e.tensor_tensor(out=ob_.t[:], in0=ob_.t[:], in1=bc(rngb.t[:, :], [128, 8, 64], 1),
                                              op=ALU.mult), r=[ob_, rngb], w=[ob_])
                K.V(lambda e: e.tensor_tensor(out=mixr.t[:], in0=o2, in1=rgc[i].t[:], op=ALU.mult),
                    r=[ob_, rgc[i]], w=[mixr])
                for j in range(4):
                    K.P(lambda e: e.transpose(out=tpr.t[:, j, :], in_=mixr.t[:, j * 128:(j + 1) * 128],
                                              identity=idb.t[:]), r=[mixr, idb], w=[tpr], mark=(j == 3))
                K.A(lambda e: e.copy(out=mT[u % 2].t[:], in_=tpr.t[:]), r=[tpr], w=[mT[u % 2]])
                K.dma(SP, mixT_d[:, 4:8, t * 128:(t + 1) * 128], mT[u % 2].t[:], r=[mT[u % 2]])
            if U["last"] and pidx is not None:
                K.dma(SP, (orf if d == 0 else orb)[pidx].rearrange("h d e -> d h e"), Sst.t[:], r=[Sst])

        pre_r(0)
        for u in range(len(units)):
            if u + 1 < len(units):
                pre_r(u + 1)
            main_r(u)

    with K.scope() as S:
        onesf = S.sb("onesf", [128, 64], F32)
        K.V(lambda e: e.memset(onesf.t[:], 1.0), w=[onesf])
        kTh = [S.sb("kTh%d" % i, [96, 36 * 128], BF16) for i in range(2)]
        vh = [S.sb("vh%d" % i, [128, 36, 65], BF16) for i in range(2)]
        qTh = [S.sb("qTh%d" % i, [96, 4096], BF16) for i in range(2)]
        pt = [S.sb("pt%d" % i, [128, 2, 512], BF16) for i in range(3)]
        rcb = S.sb("rcb", [128, 512], F32)
        bcs = S.sb("bcs", [64, 512], F32)
        bcr = S.sb("bcr", [64, 512], F32)
        ot = [S.sb("ot%d" % i, [64, 512], BF16) for i in range(2)]
        spp = [S.ps("spp%d" % i, [128, 2, 512], F32) for i in range(3)]
        accp = [S.ps("accp%d" % i, [65, 512], F32) for i in range(2)]
        scale = float(96 ** -0.5)
        seqs = [(0, 4096, 0, 36, 512)] + [(4096 + 256 * p, 256, 36 + 2 * p, 2, 256) for p in range(4)]
        hc = 0
        gcnt = 0
        slot_ctr = [0]
        pending_tail = [None]

        def alloc_slot():
            slot_ctr[0] += 1
            return slot_ctr[0] % 3

        for (c0, L, kc0, nkc, QG) in seqs:
            for h in range(8):
                i = hc % 2
                hc += 1
                K.dma(SP, kTh[i].t[:, 0:nkc * 128], kT_d[h, :, kc0 * 128:(kc0 + nkc) * 128], w=[kTh[i]])
                K.dma(SP, vh[i].t[:, 0:nkc, :], v_d[h, :, kc0:kc0 + nkc, :], w=[vh[i]])
                K.dma(SP, qTh[i].t[:, 0:L], qT_d[h, :, c0:c0 + L], w=[qTh[i]])
                nu = nkc // 2
                for qg in range(L // QG):
                    acc = accp[gcnt % 2]
                    oo = ot[gcnt % 2]
                    gcnt += 1
                    uslot = {}

                    def emit_s(u):
                        j = alloc_slot()
                        uslot[u] = j
                        for c in range(2):
                            kc = 2 * u + c
                            K.P(lambda e: e.matmul(spp[j].t[:, c, 0:QG], lhsT=kTh[i].t[:, kc * 128:(kc + 1) * 128],
                                                   rhs=qTh[i].t[:, qg * QG:(qg + 1) * QG], start=True, stop=True),
                                r=[kTh[i], qTh[i]], w=[spp[j]], mark=(c == 1))
                        K.A(lambda e: e.activation(out=pt[j].t[:, :, 0:QG], in_=spp[j].t[:, :, 0:QG], func=AF.Exp,
                                                   scale=scale), r=[spp[j]], w=[pt[j]])

                    def emit_pv(u):
                        j = uslot[u]
                        for c in range(2):
                            kc = 2 * u + c
                            K.P(lambda e: e.matmul(acc.t[:, 0:QG], lhsT=vh[i].t[:, kc, :], rhs=pt[j].t[:, c, 0:QG],
                                                   start=(kc == 0), stop=(kc == nkc - 1)),
                                r=[vh[i], pt[j]], w=[acc], mark=(kc == nkc - 1))

                    emit_s(0)
                    if nu > 1:
                        emit_s(1)
                    for u in range(nu):
                        if u + 2 < nu:
                            emit_s(u + 2)
                        emit_pv(u)
                        if u == min(1, nu - 1) and pending_tail[0] is not None:
                            pending_tail[0](uslot[u])
                            pending_tail[0] = None

                    def tail(j, acc=acc, oo=oo, QG=QG, h=h, col=c0 + qg * QG):
                        bcp = spp[j]
                        K.A(lambda e: e.copy(out=rcb.t[64:65, 0:QG], in_=acc.t[64:65, 0:QG]), r=[acc], w=[rcb])
                        K.P(lambda e: e.matmul(bcp.t[0:64, 0, 0:QG], lhsT=onesf.t[64:65, 0:64], rhs=rcb.t[64:65, 0:QG],
                                               start=True, stop=True), r=[onesf, rcb], w=[bcp])
                        K.A(lambda e: e.copy(out=bcs.t[:, 0:QG], in_=bcp.t[0:64, 0, 0:QG]), r=[bcp], w=[bcs])
                        K.V(lambda e: e.reciprocal(out=bcr.t[:, 0:QG], in_=bcs.t[:, 0:QG]), r=[bcs], w=[bcr])
                        K.V(lambda e: e.tensor_tensor(out=oo.t[:, 0:QG], in0=acc.t[0:64, 0:QG], in1=bcr.t[:, 0:QG],
                                                      op=ALU.mult), r=[acc, bcr], w=[oo])
                        K.dma(SP, mixT_d[(h % 2) * 64:(h % 2) * 64 + 64, h // 2, col:col + QG], oo.t[:, 0:QG], r=[oo])
                    assert pending_tail[0] is None
                    pending_tail[0] = tail
                    last_slot = uslot[nu - 1]
        if pending_tail[0] is not None:
            pending_tail[0](last_slot)
            pending_tail[0] = None

    with K.scope() as S:
        idb = make_ident(S)
        idf2 = S.sb("idf2", [128, 128], F32)
        K.dma(SP, idf2.t[:], ident[:, :], w=[idf2])
        wo = S.sb("wo", [128, 8, 1024], BF16)
        for k in range(8):
            K.dma(POOL, wo.t[:, k, :], w_o[0, k * 128:(k + 1) * 128, :], w=[wo])
        rwb = S.sb("rwb", [128, 8, 64], BF16)
        K.dma(POOL, rwb.t[:], router_w[0].rearrange("(k p) n -> p k n", p=128), w=[rwb])
        rbb = load_bc(S, "rbb", router_bias[0:1, :], 64)
        g1b = [mod_bc(S, "g1b%d" % ci, ci, 2) for ci in range(2)]
        gm2b = [mod_bc(S, "gm2b%d" % ci, ci, 3) for ci in range(2)]
        sh2b = [mod_bc(S, "sh2b%d" % ci, ci, 4) for ci in range(2)]
        ltf = S.sb("ltf", [128, 128], F32)
        K.dma(SP, ltf.t[:], ltri_c[:, :], w=[ltf])
        ltb = S.sb("ltb", [128, 128], BF16)
        K.V(lambda e: e.tensor_copy(out=ltb.t[:], in_=ltf.t[:]), r=[ltf], w=[ltb])
        oneb = S.sb("oneb", [128, 128], BF16)
        K.V(lambda e: e.memset(oneb.t[:], 1.0), w=[oneb])
        iof = S.sb("iof", [128, 64], F32)
        K.dma(SP, iof.t[:], iota_c[:, :], w=[iof])
        NSL = 2

        def mk(name, shape, dt):
            return [S.sb("%s_c%d" % (name, i), shape, dt) for i in range(NSL)]
        xt = mk("xc", [128, 1024], F32)
        mTl = mk("mTl", [128, 8, 128], BF16)
        x1t = mk("x1t", [128, 1024], F32)
        tmpf = mk("tmpc", [128, 1024], F32)
        junk = mk("junkc", [128, 1024], BF16)
        ssq_x = mk("ssqc", [128, 1], F32)
        h2b = mk("h2b", [128, 1024], BF16)
        h2T = mk("h2T", [128, 8, 128], BF16)
        scs = mk("scs", [128, 64], F32)
        msk = mk("msk", [128, 64], F32)
        mskb = mk("mskb", [128, 64], BF16)
        gmv = mk("gmv", [128, 64], F32)
        den = mk("den", [128, 1], F32)
        rden = mk("rden", [128, 1], F32)
        sel_all = S.sb("sel_all", [128, NT, 64], F32)
        m8_all = S.sb("m8_all", [128, NT, 8], F32)
        gts_all = S.sb("gts_all", [128, NT, 64], F32)
        rank_all = S.sb("rank_all", [128, NT, 64], F32)
        carry = S.sb("carry", [128, 64], F32)
        K.V(lambda e: e.memset(carry.t[:], 0.0), w=[carry])
        op_ = [S.ps("op_%d" % i, [128, 1024], F32) for i in range(NSL)]
        tpc_ = [S.ps("tpc_%d" % i, [128, 8, 128], BF16) for i in range(NSL)]
        sml = [S.ps("sml%d" % i, [128, 3, 64], F32) for i in range(NSL)]

        def tileC(t, i):
            ci = tile_ci(t)
            K.dma(SP, xt[i].t[:], x[t * 128:(t + 1) * 128, :], w=[xt[i]])
            K.dma(SP, mTl[i].t[:], mixT_d[:, :, t * 128:(t + 1) * 128], w=[mTl[i]])
            yield
            for half in range(2):
                for k in range(8):
                    K.P(lambda e: e.matmul(op_[i].t[:, half * 512:(half + 1) * 512], lhsT=mTl[i].t[:, k, :],
                                           rhs=wo.t[:, k, half * 512:(half + 1) * 512], start=(k == 0), stop=(k == 7)),
                        r=[mTl[i], wo], w=[op_[i]], mark=(k == 7 and half == 1))
            yield
            K.V(lambda e: e.tensor_tensor(out=tmpf[i].t[:], in0=op_[i].t[:], in1=g1b[ci].t[:], op=ALU.mult),
                r=[op_[i], g1b[ci]], w=[tmpf[i]])
            yield
            K.G(lambda e: e.tensor_tensor(out=x1t[i].t[:], in0=tmpf[i].t[:], in1=xt[i].t[:], op=ALU.add),
                r=[tmpf[i], xt[i]], w=[x1t[i]])
            K.dma(SP, x1_d[t * 128:(t + 1) * 128, :], x1t[i].t[:], r=[x1t[i]])
            yield
            K.A(lambda e: e.activation(out=junk[i].t[:], in_=x1t[i].t[:], func=AF.Square, accum_out=ssq_x[i].t[:]),
                r=[x1t[i]], w=[junk[i], ssq_x[i]])
            yield
            rs = rstd_of(S, ssq_x[i], 1024, "rsc%d" % i)
            yield
            K.V(lambda e: e.scalar_tensor_tensor(out=tmpf[i].t[:], in0=x1t[i].t[:], scalar=rs.t[:, 0:1],
                                                 in1=gm2b[ci].t[:], op0=ALU.mult, op1=ALU.mult),
                r=[x1t[i], rs, gm2b[ci]], w=[tmpf[i]])
            yield
            K.G(lambda e: e.tensor_tensor(out=h2b[i].t[:], in0=tmpf[i].t[:], in1=sh2b[ci].t[:], op=ALU.add),
                r=[tmpf[i], sh2b[ci]], w=[h2b[i]])
            K.dma(SP, h2_d[t * 128:(t + 1) * 128, :], h2b[i].t[:], r=[h2b[i]])
            yield
            for k in range(8):
                K.P(lambda e: e.transpose(out=tpc_[i].t[:, k, :], in_=h2b[i].t[:, k * 128:(k + 1) * 128],
                                          identity=idb.t[:]), r=[h2b[i], idb], w=[tpc_[i]], mark=(k == 7))
            K.A(lambda e: e.copy(out=h2T[i].t[:], in_=tpc_[i].t[:]), r=[tpc_[i]], w=[h2T[i]])
            yield
            rlp = sml[i].t[:, 0, :]
            for k in range(8):
                K.P(lambda e: e.matmul(rlp, lhsT=h2T[i].t[:, k, :], rhs=rwb.t[:, k, :], start=(k == 0),
                                       stop=(k == 7)), r=[h2T[i], rwb], w=[sml[i]], mark=(k == 7))
            yield
            K.A(lambda e: e.activation(out=scs[i].t[:], in_=rlp, func=AF.Sigmoid), r=[sml[i]], w=[scs[i]])
            yield
            selv = sel_all.t[:, t, :]
            K.V(lambda e: e.tensor_tensor(out=selv, in0=scs[i].t[:], in1=rbb.t[:], op=ALU.add),
                r=[scs[i], rbb], w=[sel_all])
            K.V(lambda e: e.max(out=m8_all.t[:, t, :], in_=selv), r=[sel_all], w=[m8_all])
            K.V(lambda e: e.tensor_single_scalar(out=msk[i].t[:], in_=selv, scalar=m8_all.t[:, t, 5:6], op=ALU.is_ge),
                r=[sel_all, m8_all], w=[msk[i]])
            yield
            K.V(lambda e: e.tensor_tensor(out=gmv[i].t[:], in0=msk[i].t[:], in1=scs[i].t[:], op=ALU.mult),
                r=[msk[i], scs[i]], w=[gmv[i]])
            K.V(lambda e: e.reduce_sum(out=den[i].t[:], in_=gmv[i].t[:], axis=AX.X), r=[gmv[i]], w=[den[i]])
            K.A(lambda e: e.copy(out=mskb[i].t[:], in_=msk[i].t[:]), r=[msk[i]], w=[mskb[i]])
            yield
            K.V(lambda e: e.reciprocal(out=rden[i].t[:], in_=den[i].t[:]), r=[den[i]], w=[rden[i]])
            K.P(lambda e: e.matmul(sml[i].t[:, 1, :], lhsT=ltb.t[:], rhs=mskb[i].t[:], start=True, stop=True),
                r=[ltb, mskb[i]], w=[sml[i]], mark=False)
            K.P(lambda e: e.matmul(sml[i].t[:, 2, :], lhsT=oneb.t[:], rhs=mskb[i].t[:], start=True, stop=True),
                r=[oneb, mskb[i]], w=[sml[i]])
            yield
            K.V(lambda e: e.tensor_scalar(out=gts_all.t[:, t, :], in0=gmv[i].t[:], scalar1=rden[i].t[:, 0:1],
                                          scalar2=2.5, op0=ALU.mult, op1=ALU.mult), r=[gmv[i], rden[i]], w=[gts_all])
            K.V(lambda e: e.tensor_tensor(out=rank_all.t[:, t, :], in0=sml[i].t[:, 1, :], in1=carry.t[:], op=ALU.add),
                r=[sml[i], carry], w=[rank_all])
            K.V(lambda e: e.tensor_tensor(out=carry.t[:], in0=sml[i].t[:, 2, :], in1=carry.t[:], op=ALU.add),
                r=[sml[i], carry], w=[carry])
            yield

        run_interleaved([(tileC, t) for t in range(NT)], NSL, stagger=0)

        ci32 = S.sb("ci32", [128, 64], I32)
        padf = S.sb("padf", [128, 64], F32)
        K.V(lambda e: e.tensor_single_scalar(out=padf.t[:], in_=carry.t[:], scalar=127.0, op=ALU.add), r=[carry], w=[padf])
        K.V(lambda e: e.tensor_copy(out=ci32.t[:], in_=padf.t[:]), r=[padf], w=[ci32])
        K.V(lambda e: e.tensor_single_scalar(out=ci32.t[:], in_=ci32.t[:], scalar=7, op=ALU.arith_shift_right),
            r=[ci32], w=[ci32])
        K.V(lambda e: e.tensor_single_scalar(out=ci32.t[:], in_=ci32.t[:], scalar=7, op=ALU.logical_shift_left),
            r=[ci32], w=[ci32])
        K.V(lambda e: e.tensor_copy(out=padf.t[:], in_=ci32.t[:]), r=[ci32], w=[padf])
        ptp = op_[0]
        K.P(lambda e: e.transpose(out=ptp.t[0:64, 0:128], in_=padf.t[:], identity=idf2.t[:]), r=[padf, idf2], w=[ptp])
        PTs = S.sb("PTs", [64, 128], F32)
        K.A(lambda e: e.copy(out=PTs.t[:], in_=ptp.t[0:64, 0:128]), r=[ptp], w=[PTs])
        offp = op_[1]
        K.P(lambda e: e.matmul(offp.t[:, 0:64], lhsT=PTs.t[:], rhs=ltf.t[0:64, 0:64], start=True, stop=True),
            r=[PTs, ltf], w=[offp])
        offb = S.sb("offb", [128, 64], F32)
        K.A(lambda e: e.copy(out=offb.t[:], in_=offp.t[:, 0:64]), r=[offp], w=[offb])

        zt = S.sb("zt", [128, NST * 2], I32)
        K.V(lambda e: e.memset(zt.t[:], 0), w=[zt])
        K.dma(SP, sinfo_d.rearrange("(p n) c -> p (n c)", p=128), zt.t[:], r=[zt], w=[sinfo_b])
        pos = S.sb("pos", [128, 64], F32)
        oh = S.sb("oh", [128, 6, 64], F32)
        ohp = S.sb("ohp", [128, 6, 64], F32)
        pos6f = S.sb("pos6f", [128, NT * 6], F32)
        e6f = S.sb("e6f", [128, 6], F32)
        g6a = S.sb("g6a", [128, NT * 6], F32)
        pos6i = S.sb("pos6i", [128, NT * 6], I32)
        tokf = S.sb("tokf", [128, 1], F32)
        infs = [S.sb("infs%d" % i, [128, 6, 2], I32) for i in range(2)]
        hi6i = S.sb("hi6i", [128, 6], I32)
        hi6f = S.sb("hi6f", [128, 6], F32)
        fl6f = S.sb("fl6f", [128, 6], F32)
        fl6i = [S.sb("fl6i%d" % i, [128, 6], I32) for i in range(2)]
        ecl = S.sb("ecl", [128, 4], F32)
        K.dma(SP, ecl.t[:], ecols[:, :], w=[ecl])
        for t in range(NT):
            i = t % 2
            K.V(lambda e: e.tensor_tensor(out=pos.t[:], in0=rank_all.t[:, t, :], in1=offb.t[:], op=ALU.add),
                r=[rank_all, offb], w=[pos])
            K.V(lambda e: e.tensor_tensor(out=oh.t[:], in0=bc(sel_all.t[:, t, :], [128, 6, 64], 1),
                                          in1=bc(m8_all.t[:, t, 0:6], [128, 6, 64], 2), op=ALU.is_equal),
                r=[sel_all, m8_all], w=[oh])
            for (src_ap, srcb, dst_ap, dstb) in (
                    (pos.t[:], pos, pos6f.t[:, t * 6:(t + 1) * 6], pos6f),
                    (gts_all.t[:, t, :], gts_all, g6a.t[:, t * 6:(t + 1) * 6], g6a),
                    (iof.t[:], iof, e6f.t[:], e6f)):
                K.V(lambda e: e.tensor_tensor(out=ohp.t[:], in0=oh.t[:], in1=bc(src_ap, [128, 6, 64], 1), op=ALU.mult),
                    r=[oh, srcb], w=[ohp])
                K.V(lambda e: e.reduce_sum(out=dst_ap, in_=ohp.t[:], axis=AX.X), r=[ohp], w=[dstb])
            K.V(lambda e: e.tensor_copy(out=pos6i.t[:, t * 6:(t + 1) * 6], in_=pos6f.t[:, t * 6:(t + 1) * 6]),
                r=[pos6f], w=[pos6i])
            K.V(lambda e: e.tensor_single_scalar(out=hi6i.t[:], in_=pos6i.t[:, t * 6:(t + 1) * 6], scalar=7,
                                                 op=ALU.arith_shift_right), r=[pos6i], w=[hi6i])
            K.V(lambda e: e.tensor_copy(out=hi6f.t[:], in_=hi6i.t[:]), r=[hi6i], w=[hi6f])
            K.V(lambda e: e.tensor_single_scalar(out=fl6f.t[:], in_=pos6f.t[:, t * 6:(t + 1) * 6], scalar=float(NST),
                                                 op=ALU.mult), r=[pos6f], w=[fl6f])
            K.V(lambda e: e.scalar_tensor_tensor(out=fl6f.t[:], in0=hi6f.t[:], scalar=-float(128 * NST - 1),
                                                 in1=fl6f.t[:], op0=ALU.mult, op1=ALU.add), r=[hi6f, fl6f], w=[fl6f])
            K.V(lambda e: e.tensor_copy(out=fl6i[i].t[:], in_=fl6f.t[:]), r=[fl6f], w=[fl6i[i]])
            K.V(lambda e: e.tensor_single_scalar(out=tokf.t[:], in_=ecl.t[:, 2:3], scalar=float(t * 128), op=ALU.add),
                r=[ecl], w=[tokf])
            K.V(lambda e: e.tensor_copy(out=infs[i].t[:, :, 0], in_=bc(tokf.t[:, 0:1], [128, 6, 1], 1)[:, :, 0]),
                r=[tokf], w=[infs[i]])
            K.V(lambda e: e.tensor_copy(out=infs[i].t[:, :, 1], in_=e6f.t[:]), r=[e6f], w=[infs[i]])
            for j in range(6):
                K.iscatter(sinfo_d[:, :], fl6i[i].t[:, j:j + 1], infs[i].t[:, j, :],
                           r=[infs[i], fl6i[i], sinfo_b])
        K.dma(SP, pos6_d[:, :], pos6i.t[:], r=[pos6i])
        K.dma(SP, g6_d[:, :], g6a.t[:], r=[g6a])
        K.wait_bg()

    with K.scope() as S:
        idb = make_ident(S)
        tpD = [S.ps("ftp%d" % i, [128, 8, 128], BF16) for i in range(2)]
        guD = [S.ps("fgu%d" % i, [128, 512], F32) for i in range(2)]
        htpD = S.ps("fhtp", [128, 2, 128], BF16)
        ypD = S.ps("fyp", [128, 1024], F32)
        xsTD = [S.sb("fxsT%d" % i, [128, 8, 128], BF16) for i in range(3)]
        sgD = [S.sb("fsg%d" % i, [128, 256], F32) for i in range(2)]
        HD = [S.sb("fH%d" % i, [128, 256], BF16) for i in range(2)]
        HTD = [S.sb("fHT%d" % i, [128, 2, 128], BF16) for i in range(2)]
        soD = [S.sb("fso%d" % i, [128, 1024], BF16) for i in range(2)]
        NB = 5
        NW = 4
        xs = [S.sb("xs%d" % i, [128, 1024], BF16) for i in range(NB)]
        Wt = [S.sb("Wt%d" % i, [128, 6144], BF16) for i in range(NW)]
        ecl = S.sb("ecl2", [128, 4], F32)
        K.dma(SP, ecl.t[:], ecols[:, :], w=[ecl])
        Wsh = S.sb("Wsh", [128, 6144], BF16)
        guv = Wsh.t[:, 0:4096].rearrange("p (k n) -> p k n", k=8)
        K.dma(POOL, guv[:, :, 0:256], sh_w_gate[0].rearrange("(k p) n -> p k n", p=128), w=[Wsh])
        K.dma(POOL, guv[:, :, 256:512], sh_w_up[0].rearrange("(k p) n -> p k n", p=128), w=[Wsh])
        K.dma(POOL, Wsh.t[:, 4096:6144].rearrange("p (c n) -> p c n", c=2),
              sh_w_down[0].rearrange("(c p) n -> p c n", p=128), w=[Wsh])
        wall_rows = wall_d.rearrange("e p n -> (e p) n")
        bnd_reg = nc.gpsimd.alloc_register("bnd")
        nc.gpsimd.reg_mov(bnd_reg, 64 * 128 - 1)

        sinf_all = S.sb("sinf_all", [128, NST, 2], I32)
        K.dma(SP, sinf_all.t[:].rearrange("p j c -> p (j c)"), sinfo_d.rearrange("(p j) c -> p (j c)", p=128),
              w=[sinf_all])
        rowi = S.sb("rowi", [1, NST * 2], I32)
        K.dma(SP, rowi.t[:], sinfo_d[0:NST, :].rearrange("(o j) c -> o (j c)", o=1), w=[rowi])
        rowf = S.sb("rowf", [1, NST * 2], F32)
        K.V(lambda e: e.tensor_copy(out=rowf.t[:], in_=rowi.t[:]), r=[rowi], w=[rowf])
        one1 = S.sb("one1", [1, 128], F32)
        K.V(lambda e: e.memset(one1.t[:], 1.0), w=[one1])
        for (c0, c1) in ((0, 512), (512, NST * 2)):
            K.P(lambda e: e.matmul(ypD.t[:, c0:c1], lhsT=one1.t[:], rhs=rowf.t[:, c0:c1], start=True, stop=True),
                r=[one1, rowf], w=[ypD], mark=(c0 == 512))
        e_all = S.sb("e_all", [128, NST], F32)
        K.V(lambda e: e.tensor_copy(out=e_all.t[:], in_=ypD.t[:, 0:NST * 2].rearrange("p (j c) -> p j c", c=2)[:, :, 1]),
            r=[ypD], w=[e_all])
        wf_all = S.sb("wf_all", [128, NST], F32)
        K.V(lambda e: e.scalar_tensor_tensor(out=wf_all.t[:], in0=e_all.t[:], scalar=128.0,
                                             in1=ecl.t[:, 2:3].to_broadcast([128, NST]), op0=ALU.mult, op1=ALU.add),
            r=[e_all, ecl], w=[wf_all])
        eq_all = S.sb("eq_all", [128, NST], F32)
        K.V(lambda e: e.tensor_tensor(out=eq_all.t[:, NW:NST], in0=e_all.t[:, NW:NST], in1=e_all.t[:, 0:NST - NW],
                                      op=ALU.is_equal), r=[e_all], w=[eq_all])
        K.V(lambda e: e.scalar_tensor_tensor(out=wf_all.t[:, NW:NST], in0=eq_all.t[:, NW:NST], scalar=1.0e6,
                                             in1=wf_all.t[:, NW:NST], op0=ALU.mult, op1=ALU.add),
            r=[eq_all, wf_all], w=[wf_all])
        widx_all = S.sb("widx_all", [128, NST], I32)
        K.V(lambda e: e.tensor_copy(out=widx_all.t[:], in_=wf_all.t[:]), r=[wf_all], w=[widx_all])

        NU = NST + NT

        def u_x(u):
            return xs[u % NB]

        def u_w(u):
            return Wt[u % NW] if u < NST else Wsh

        def u_wd(u):
            return u_w(u)

        def u_dst(u):
            return so_d[u * 128:(u + 1) * 128, :]

        def prep_idx(j):
            return

        def prep_x(u):
            if u >= NU:
                return
            i = u % NB
            if u < NST:
                K.igather(xs[i].t[:, :], h2_d[:, :], sinf_all.t[:, u, 0:1], r=[sinf_all], w=[xs[i]])
            else:
                t = u - NST
                K.dma(SP, xs[i].t[:], h2_d[t * 128:(t + 1) * 128, :], w=[xs[i]])

        def prep_w(j):
            if j >= NST:
                return
            K.igather(Wt[j % NW].t[:, :], wall_rows, widx_all.t[:, j:j + 1], r=[widx_all], w=[Wt[j % NW]],
                      bounds=bnd_reg)

        def ph_x(u):
            if u >= NU:
                return
            tp_ = tpD[u % 2]
            xT = xsTD[u % 3]
            x_ = u_x(u)
            for k in range(8):
                K.P(lambda e: e.transpose(out=tp_.t[:, k, :], in_=x_.t[:, k * 128:(k + 1) * 128], identity=idb.t[:]),
                    r=[x_, idb], w=[tp_], mark=(k == 7))
            K.A(lambda e: e.copy(out=xT.t[:], in_=tp_.t[:]), r=[tp_], w=[xT])

        def ph_y(u):
            if u >= NU:
                return
            gu = guD[u % 2]
            xT = xsTD[u % 3]
            Wb = u_w(u)
            for k in range(8):
                K.P(lambda e: e.matmul(gu.t[:], lhsT=xT.t[:, k, :], rhs=Wb.t[:, k * 512:(k + 1) * 512],
                                       start=(k == 0), stop=(k == 7)), r=[xT, Wb], w=[gu], mark=(k == 7))
            K.A(lambda e: e.activation(out=sgD[u % 2].t[:], in_=gu.t[:, 0:256], func=AF.Silu), r=[gu], w=[sgD[u % 2]])
            K.V(lambda e: e.tensor_tensor(out=HD[u % 2].t[:], in0=gu.t[:, 256:512], in1=sgD[u % 2].t[:], op=ALU.mult),
                r=[gu, sgD[u % 2]], w=[HD[u % 2]])

        def ph_z1(u):
            H_ = HD[u % 2]
            for c in range(2):
                K.P(lambda e: e.transpose(out=htpD.t[:, c, :], in_=H_.t[:, c * 128:(c + 1) * 128], identity=idb.t[:]),
                    r=[H_, idb], w=[htpD], mark=(c == 1))
            K.A(lambda e: e.copy(out=HTD[u % 2].t[:], in_=htpD.t[:]), r=[htpD], w=[HTD[u % 2]])

        def ph_z2(u):
            HT = HTD[u % 2]
            Wb = u_wd(u)
            so = soD[u % 2]
            for half in range(2):
                for c in range(2):
                    K.P(lambda e: e.matmul(ypD.t[:, half * 512:(half + 1) * 512], lhsT=HT.t[:, c, :],
                                           rhs=Wb.t[:, 4096 + c * 1024 + half * 512:4096 + c * 1024 + (half + 1) * 512],
                                           start=(c == 0), stop=(c == 1)), r=[HT, Wb], w=[ypD],
                        mark=(c == 1 and half == 1))
            K.V(lambda e: e.tensor_copy(out=so.t[:], in_=ypD.t[:]), r=[ypD], w=[so])
            K.dma(SP, u_dst(u), so.t[:], r=[so])

        for j in range(4):
            prep_idx(j)
        for u in range(3):
            prep_x(u)
        for j in range(3):
            prep_w(j)
        ph_x(0)
        ph_x(1)
        ph_y(0)
        for u in range(NU):
            prep_idx(u + 4)
            prep_x(u + 3)
            prep_w(u + 3)
            ph_x(u + 2)
            ph_z1(u)
            ph_y(u + 1)
            ph_z2(u)

    with K.scope() as S:
        g2b = [mod_bc(S, "g2b%d" % ci, ci, 5) for ci in range(2)]
        p6 = S.sb("p6", [128, NT * 6], I32)
        K.dma(SP, p6.t[:], pos6_d[:, :], w=[p6])
        g6 = S.sb("g6", [128, NT * 6], F32)
        K.dma(SP, g6.t[:], g6_d[:, :], w=[g6])
        gb = [[S.sb("gb%d_%d" % (i, j), [128, 1024], BF16) for j in range(6)] for i in range(2)]
        shd = [S.sb("shd%d" % i, [128, 1024], BF16) for i in range(2)]
        x1l = [S.sb("x1l%d" % i, [128, 1024], F32) for i in range(2)]
        acc = S.sb("acc", [128, 1024], F32)
        yo = [S.sb("yo%d" % i, [128, 1024], F32) for i in range(2)]
        def pre_e(t):
            i = t % 2
            K.dma(SP, shd[i].t[:], so_d[NSLOT + t * 128:NSLOT + (t + 1) * 128, :], w=[shd[i]])
            K.dma(SP, x1l[i].t[:], x1_d[t * 128:(t + 1) * 128, :], w=[x1l[i]])
            for j in range(6):
                K.igather(gb[i][j].t[:, :], so_d[:, :], p6.t[:, t * 6 + j:t * 6 + j + 1], r=[p6], w=[gb[i][j]])

        pre_e(0)
        for t in range(NT):
            i = t % 2
            ci = tile_ci(t)
            if t + 1 < NT:
                pre_e(t + 1)
            prev = shd[i]
            for j in range(6):
                K.V(lambda e: e.scalar_tensor_tensor(out=acc.t[:], in0=gb[i][j].t[:],
                                                     scalar=g6.t[:, t * 6 + j:t * 6 + j + 1], in1=prev.t[:],
                                                     op0=ALU.mult, op1=ALU.add), r=[gb[i][j], g6, prev], w=[acc])
                prev = acc
            K.V(lambda e: e.tensor_tensor(out=acc.t[:], in0=acc.t[:], in1=g2b[ci].t[:], op=ALU.mult),
                r=[acc, g2b[ci]], w=[acc])
            K.V(lambda e: e.tensor_tensor(out=yo[i].t[:], in0=acc.t[:], in1=x1l[i].t[:], op=ALU.add),
                r=[acc, x1l[i]], w=[yo[i]])
            K.dma(SP, y[t * 128:(t + 1) * 128, :], yo[i].t[:], r=[yo[i]])
    return nc


def _consts():
    c = {}
    c["c_ident"] = np.eye(128, dtype=np.float32)
    tok = np.arange(4096)
    row = (tok // 64).astype(np.float64)
    col = (tok % 64).astype(np.float64)

    def tab(p):
        inv = 1.0 / (10000.0 ** (np.arange(p, dtype=np.float64) / p))
        ar = row[:, None] * inv[None, :]
        ac = col[:, None] * inv[None, :]
        cos = np.concatenate([np.cos(ar), np.cos(ar), np.cos(ac), np.cos(ac)], axis=1)
        sin = np.concatenate([-np.sin(ar), np.sin(ar), -np.sin(ac), np.sin(ac)], axis=1)
        t = np.stack([cos, sin], axis=1).astype(np.float32)
        return np.ascontiguousarray(t.reshape(NTS, 128, 2, 4 * p))
    c["c_ropeq"] = tab(8)
    c["c_roper"] = tab(16)
    k = np.arange(128)[:, None]
    cc = np.arange(128)[None, :]
    df = cc - k
    c["c_dexp"] = np.stack([np.where(df >= 0, df, 0), np.where(df < 0, -df, 0)]).astype(np.float32)
    c["c_dmask"] = np.stack([(df >= 0), (df < 0)]).astype(np.float32)
    p = np.arange(128, dtype=np.float32)
    c["c_ecols"] = np.stack([127 - p, p + 1, p, 128 - p], axis=1).astype(np.float32)
    c["c_ltri"] = (np.arange(128)[:, None] < np.arange(128)[None, :]).astype(np.float32)
    c["c_iota"] = np.broadcast_to(np.arange(64, dtype=np.float32)[None, :], (128, 64)).copy()
    return c


_WNAMES = ["w_ada", "b_ada", "norm1_g", "w_in", "mla_q_norm_g", "w_uq", "mla_kv_norm_g", "w_ukv", "mla_q_gain",
           "mla_k_gain", "ret_decay_fwd", "ret_decay_bwd", "ret_norm_g", "w_o", "norm2_g", "router_w", "router_bias",
           "exp_w_gate", "exp_w_up", "exp_w_down", "sh_w_gate", "sh_w_up", "sh_w_down"]


def kernel(**inputs):
    f = lambda a: np.ascontiguousarray(np.asarray(a, dtype=np.float32))
    inp = {k: f(v) for k, v in inputs.items()}
    consts = _consts()
    nc = build_program()
    in_maps = []
    for b in range(8):
        m = {}
        m["x"] = np.ascontiguousarray(np.concatenate(
            [inp["x_sample"][b], inp["x_prompt"][4 * b:4 * b + 4].reshape(1024, 1024)], axis=0))
        m["cckv"] = np.ascontiguousarray(inp["cache_mla_ckv"][b, 0])
        m["ckr"] = np.ascontiguousarray(inp["cache_mla_krope"][b, 0])
        m["sf"] = np.ascontiguousarray(inp["state_ret_fwd"][b, 0])
        m["sb"] = np.ascontiguousarray(inp["state_ret_bwd"][b, 0])
        m["cond"] = np.ascontiguousarray(np.stack([inp["c"][b], inp["c_ctx"]], axis=0))
        for n in _WNAMES:
            m[n] = inp[n]
        m.update(consts)
        in_maps.append(m)
    res = run_bass_kernel_spmd(nc, in_maps, core_ids=list(range(8)))
    R = res.results
    y_sample = np.stack([R[b]["y"][0:4096] for b in range(8)], axis=0)
    y_prompt = np.concatenate([R[b]["y"][4096:].reshape(4, 256, 1024) for b in range(8)], axis=0)
    new_ckv = np.concatenate([R[b]["ockv"].reshape(4, 1, 256, 128) for b in range(8)], axis=0)
    new_kr = np.concatenate([R[b]["okr"].reshape(4, 1, 256, 32) for b in range(8)], axis=0)
    new_rf = np.concatenate([R[b]["orf"].reshape(4, 1, 8, 64, 64) for b in range(8)], axis=0)
    new_rb = np.concatenate([R[b]["orb"].reshape(4, 1, 8, 64, 64) for b in range(8)], axis=0)
    return (y_prompt.astype(np.float32), y_sample.astype(np.float32), new_ckv.astype(np.float32),
            new_kr.astype(np.float32), new_rf.astype(np.float32), new_rb.astype(np.float32))
```

```python
import contextlib
import numpy as np
import concourse.bass as bass
import concourse.mybir as mybir
from concourse.bass_utils import run_bass_kernel_spmd

F32 = mybir.dt.float32
BF16 = mybir.dt.bfloat16
I32 = mybir.dt.int32
AF = mybir.ActivationFunctionType
ALU = mybir.AluOpType
AX = mybir.AxisListType

NT = 40
NTS = 32
TOK = 5120
EPS = 1e-6
NKC = 44
NST = 304
NSLOT = NST * 128


class Buf:
    def __init__(self, t, name):
        self.t = t
        self.name = name
        self.wr = None
        self.rd = {}
        self.ds = {}


class Eng:
    def __init__(self, nc, name, e, self_sync=True):
        self.nc = nc
        self.name = name
        self.e = e
        self.self_sync = self_sync
        self.sem = nc.alloc_semaphore("s_" + name)
        self.cnt = 0
        self.seen = {}
        self.pend_r = []
        self.pend_w = []

    def wait(self, tk):
        if tk is None:
            return
        sem, val, key = tk
        if key == self.name and not self.self_sync:
            return
        if self.seen.get(key, 0) >= val:
            return
        self.e.wait_ge(sem, val)
        self.seen[key] = val


class KB:
    def __init__(self, nc):
        self.nc = nc
        self.PE = Eng(nc, "pe", nc.tensor, self_sync=False)
        self.ACT = Eng(nc, "act", nc.scalar)
        self.DVE = Eng(nc, "dve", nc.vector)
        self.POOL = Eng(nc, "pool", nc.gpsimd)
        self.SP = Eng(nc, "sp", nc.sync)
        self.engs = [self.PE, self.ACT, self.DVE, self.POOL, self.SP]
        self.dma_tk = {}
        self.bsem = nc.alloc_semaphore("s_bar")
        self.bcnt = 0
        self.nsem = 0
        self.uid = 0
        self.free_sems = {"hw": [], "sw": []}
        self.bg_tk = {}

    def op(self, E, fn, r=(), w=(), mark=True):
        for b in r:
            E.wait(b.wr)
        for b in w:
            E.wait(b.wr)
            for tk in list(b.rd.values()):
                E.wait(tk)
        inst = fn(E.e)
        if mark:
            E.cnt += 1
            inst.then_inc(E.sem, 1)
            tk = (E.sem, E.cnt, E.name)
            for b in E.pend_r:
                b.rd[E.name] = tk
            for b in E.pend_w:
                b.wr = tk
                b.rd = {}
            E.pend_r = []
            E.pend_w = []
            for b in r:
                b.rd[E.name] = tk
            for b in w:
                b.wr = tk
                b.rd = {}
        else:
            E.pend_r.extend(r)
            E.pend_w.extend(w)
        return inst

    def P(self, fn, r=(), w=(), mark=True):
        return self.op(self.PE, fn, r, w, mark)

    def A(self, fn, r=(), w=()):
        return self.op(self.ACT, fn, r, w)

    def V(self, fn, r=(), w=()):
        return self.op(self.DVE, fn, r, w)

    def G(self, fn, r=(), w=()):
        return self.op(self.POOL, fn, r, w)

    def _dma_common(self, Q, r, w, issue, bg=False):
        for b in r:
            Q.wait(b.wr)
        for b in w:
            Q.wait(b.wr)
            for tk in list(b.rd.values()):
                Q.wait(tk)
        prim = w[0] if len(w) else r[0]
        kind = "sw" if Q is self.POOL else "hw"
        if kind not in prim.ds:
            if kind == "hw" and self.free_sems[kind]:
                prim.ds[kind] = self.free_sems[kind].pop()
            else:
                prim.ds[kind] = [self.nc.alloc_semaphore("d%d" % self.nsem), "dsem_%d" % self.nsem, 0]
                self.nsem += 1
        ent = prim.ds[kind]
        inst = issue(Q.e)
        ent[2] += 16
        inst.then_inc(ent[0], 16)
        tk = (ent[0], ent[2], ent[1])
        (self.bg_tk if bg else self.dma_tk)[ent[1]] = tk
        for b in r:
            b.rd[ent[1]] = tk
        for b in w:
            b.wr = tk
            b.rd = {}
        return inst

    def dma(self, Q, out, in_, r=(), w=(), noncontig=False, bg=False):
        def issue(e):
            if noncontig:
                with self.nc.allow_non_contiguous_dma(reason="tiny transposed load"):
                    return e.dma_start(out=out, in_=in_)
            return e.dma_start(out=out, in_=in_)
        return self._dma_common(Q, r, w, issue, bg)

    def igather(self, out, src, idx, r=(), w=(), bounds=None):
        if bounds is None:
            return self._dma_common(self.POOL, r, w, lambda e: e.indirect_dma_start(
                out=out, out_offset=None, in_=src, in_offset=bass.IndirectOffsetOnAxis(ap=idx, axis=0)))
        return self._dma_common(self.POOL, r, w, lambda e: e.indirect_dma_start(
            out=out, out_offset=None, in_=src, in_offset=bass.IndirectOffsetOnAxis(ap=idx, axis=0),
            bounds_check=bounds, oob_is_err=False))

    def iscatter(self, dst, idx, in_, r=(), w=()):
        return self._dma_common(self.POOL, r, w, lambda e: e.indirect_dma_start(
            out=dst, out_offset=bass.IndirectOffsetOnAxis(ap=idx, axis=0), in_=in_, in_offset=None))

    def wait_bg(self):
        for tk in list(self.bg_tk.values()):
            self.SP.wait(tk)
        self.bg_tk = {}

    def barrier(self):
        SP = self.SP
        for E in self.engs:
            assert not E.pend_r and not E.pend_w, E.name
            if E is not SP and E.cnt > 0:
                SP.wait((E.sem, E.cnt, E.name))
        for tk in list(self.dma_tk.values()):
            SP.wait(tk)
        self.dma_tk = {}
        self.bcnt += 1
        SP.e.sem_inc(self.bsem, 1)
        for E in self.engs:
            if E is not SP:
                E.e.wait_ge(self.bsem, self.bcnt)

    @contextlib.contextmanager
    def scope(self):
        es = contextlib.ExitStack()
        kb = self
        bufs = []

        class S:
            def sb(self_, name, shape, dt):
                kb.uid += 1
                nm = "%s_%d" % (name, kb.uid)
                t = es.enter_context(kb.nc.sbuf_tensor(nm, shape, dt))
                b = Buf(t, nm)
                bufs.append(b)
                return b

            def ps(self_, name, shape, dt):
                kb.uid += 1
                nm = "%s_%d" % (name, kb.uid)
                t = es.enter_context(kb.nc.psum_tensor(nm, shape, dt))
                return Buf(t, nm)

        with es:
            yield S()
            self.barrier()
            for b in bufs:
                for kind, ent in b.ds.items():
                    self.free_sems[kind].append(ent)


def run_interleaved(jobs, nslots, stagger=0):
    pending = list(jobs)
    live = [None] * nslots
    rnd = 0
    while True:
        progressed = False
        rnd += 1
        for sl in range(nslots):
            if live[sl] is None and pending and rnd > sl * stagger:
                fn, a = pending.pop(0)
                live[sl] = fn(a, sl)
            if live[sl] is not None:
                progressed = True
                try:
                    next(live[sl])
                except StopIteration:
                    live[sl] = None
        if not progressed and not pending:
            break
        if not progressed and rnd > nslots * stagger + 2:
            break


def bc(ap, shape, axis):
    return ap.unsqueeze(axis).to_broadcast(shape)


def build_program():
    nc = bass.Bass("TRN2", target_bir_lowering=False)

    def din(name, shape, dt=F32):
        return nc.dram_tensor(name, list(shape), dt, kind="ExternalInput").ap()

    def dout(name, shape, dt=F32):
        return nc.dram_tensor(name, list(shape), dt, kind="ExternalOutput").ap()

    def dscr(name, shape, dt):
        return nc.dram_tensor(name, list(shape), dt, kind="Internal").ap()

    x = din("x", [TOK, 1024])
    cckv = din("cckv", [512, 128])
    ckr = din("ckr", [512, 32])
    sf_in = din("sf", [8, 64, 64])
    sb_in = din("sb", [8, 64, 64])
    cond = din("cond", [2, 1024])
    w_ada = din("w_ada", [1, 1024, 6144])
    b_ada = din("b_ada", [1, 6144])
    norm1_g = din("norm1_g", [1, 1024])
    w_in = din("w_in", [1, 1024, 2464])
    mla_q_norm_g = din("mla_q_norm_g", [1, 256])
    w_uq = din("w_uq", [1, 256, 768])
    mla_kv_norm_g = din("mla_kv_norm_g", [1, 128])
    w_ukv = din("w_ukv", [1, 128, 1024])
    mla_q_gain = din("mla_q_gain", [1, 96])
    mla_k_gain = din("mla_k_gain", [1, 96])
    ret_decay_fwd = din("ret_decay_fwd", [1, 8])
    ret_decay_bwd = din("ret_decay_bwd", [1, 8])
    ret_norm_g = din("ret_norm_g", [1, 64])
    w_o = din("w_o", [1, 1024, 1024])
    norm2_g = din("norm2_g", [1, 1024])
    router_w = din("router_w", [1, 1024, 64])
    router_bias = din("router_bias", [1, 64])
    exp_w_gate = din("exp_w_gate", [1, 64, 1024, 256])
    exp_w_up = din("exp_w_up", [1, 64, 1024, 256])
    exp_w_down = din("exp_w_down", [1, 64, 256, 1024])
    sh_w_gate = din("sh_w_gate", [1, 1024, 256])
    sh_w_up = din("sh_w_up", [1, 1024, 256])
    sh_w_down = din("sh_w_down", [1, 256, 1024])
    ident = din("c_ident", [128, 128])
    ropeq = din("c_ropeq", [NTS, 128, 2, 32])
    roper = din("c_roper", [NTS, 128, 2, 64])
    dexp = din("c_dexp", [2, 128, 128])
    dmask = din("c_dmask", [2, 128, 128])
    ecols = din("c_ecols", [128, 4])
    ltri_c = din("c_ltri", [128, 128])
    iota_c = din("c_iota", [128, 64])

    y = dout("y", [TOK, 1024])
    ockv = dout("ockv", [1024, 128])
    okr = dout("okr", [1024, 32])
    orf = dout("orf", [4, 8, 64, 64])
    orb = dout("orb", [4, 8, 64, 64])

    mod_d = dscr("mod_d", [2, 6144], F32)
    qT_d = dscr("qT_d", [8, 96, TOK], BF16)
    kT_d = dscr("kT_d", [8, 96, NKC * 128], BF16)
    v_d = dscr("v_d", [8, 128, NKC, 65], BF16)
    rqT_d = dscr("rqT_d", [NT, 64, 8, 128], BF16)
    rkT_d = dscr("rkT_d", [NT, 64, 8, 128], BF16)
    rk_d = dscr("rk_d", [NT, 128, 512], BF16)
    rv_d = dscr("rv_d", [NT, 128, 512], BF16)
    rg_d = dscr("rg_d", [NT, 128, 512], F32)
    mixT_d = dscr("mixT_d", [128, 8, TOK], BF16)
    x1_d = dscr("x1_d", [TOK, 1024], F32)
    h2T_d = dscr("h2T_d", [128, 8, TOK], BF16)
    gates_d = dscr("gates_d", [TOK, 64], F32)
    h2_d = dscr("h2_d", [TOK, 1024], BF16)
    wall_d = dscr("wall_d", [64, 128, 6144], BF16)
    sinfo_d = dscr("sinfo_d", [NSLOT, 2], I32)
    pos6_d = dscr("pos6_d", [128, NT * 6], I32)
    g6_d = dscr("g6_d", [128, NT * 6], F32)
    so_d = dscr("so_d", [NSLOT + TOK, 1024], BF16)

    K = KB(nc)
    SP, POOL = K.SP, K.POOL
    wall_b = Buf(None, "wall_b")
    sinfo_b = Buf(None, "sinfo_b")

    rs_cache = {}

    def rstd_of(S, ssq, n, name):
        shape = list(ssq.t[:].shape)
        ck = (id(S), name)
        if ck not in rs_cache:
            rs_cache[ck] = (S.sb(name + "_v", shape, F32), S.sb(name + "_s", shape, F32), S.sb(name + "_r", shape, F32))
        v, s, o = rs_cache[ck]
        K.V(lambda e: e.tensor_scalar(out=v.t[:], in0=ssq.t[:], scalar1=1.0 / n, scalar2=EPS,
                                      op0=ALU.mult, op1=ALU.add), r=[ssq], w=[v])
        K.A(lambda e: e.activation(out=s.t[:], in_=v.t[:], func=AF.Sqrt), r=[v], w=[s])
        K.V(lambda e: e.reciprocal(out=o.t[:], in_=s.t[:]), r=[s], w=[o])
        return o

    with K.scope() as S:
        cT = S.sb("cT", [128, 2, 8], F32)
        for c_ in range(2):
            K.dma(SP, cT.t[:, c_, :], cond[c_].rearrange("(k p) -> p k", p=128), w=[cT], noncontig=True)
        scT = S.sb("scT", [128, 2, 8], F32)
        K.A(lambda e: e.activation(out=scT.t[:], in_=cT.t[:], func=AF.Silu), r=[cT], w=[scT])
        bada = S.sb("bada", [2, 6144], F32)
        K.dma(SP, bada.t[:], b_ada[0:1, :].partition_broadcast(2), w=[bada])
        n1g = S.sb("n1g", [2, 1024], F32)
        K.dma(SP, n1g.t[:], norm1_g[0:1, :].partition_broadcast(2), w=[n1g])
        n2g = S.sb("n2g", [2, 1024], F32)
        K.dma(SP, n2g.t[:], norm2_g[0:1, :].partition_broadcast(2), w=[n2g])
        mod = S.sb("mod", [2, 6144], F32)
        wb = [S.sb("wa%d" % i, [128, 8, 512], F32) for i in range(3)]
        pp = [S.ps("pa%d" % i, [2, 512], F32) for i in range(2)]
        for g in range(12):
            wt = wb[g % 3]
            p = pp[g % 2]
            K.dma(SP, wt.t[:], w_ada[0, :, g * 512:(g + 1) * 512].rearrange("(k p) n -> p k n", p=128), w=[wt])
            for k in range(8):
                K.P(lambda e: e.matmul(p.t[:], lhsT=scT.t[:, :, k], rhs=wt.t[:, k, :],
                                       start=(k == 0), stop=(k == 7)), r=[scT, wt], w=[p], mark=(k == 7))
            K.V(lambda e: e.tensor_tensor(out=mod.t[:, g * 512:(g + 1) * 512], in0=p.t[:],
                                          in1=bada.t[:, g * 512:(g + 1) * 512], op=ALU.add), r=[p, bada], w=[mod])
        md = S.sb("md", [2, 6144], F32)

        def seg(b, i):
            return b.t[:, i * 1024:(i + 1) * 1024]
        K.V(lambda e: e.scalar_tensor_tensor(out=seg(md, 0), in0=seg(mod, 1), scalar=1.0, in1=n1g.t[:],
                                             op0=ALU.add, op1=ALU.mult), r=[mod, n1g], w=[md])
        K.V(lambda e: e.tensor_copy(out=seg(md, 1), in_=seg(mod, 0)), r=[mod], w=[md])
        K.V(lambda e: e.tensor_copy(out=seg(md, 2), in_=seg(mod, 2)), r=[mod], w=[md])
        K.V(lambda e: e.scalar_tensor_tensor(out=seg(md, 3), in0=seg(mod, 4), scalar=1.0, in1=n2g.t[:],
                                             op0=ALU.add, op1=ALU.mult), r=[mod, n2g], w=[md])
        K.V(lambda e: e.tensor_copy(out=seg(md, 4), in_=seg(mod, 3)), r=[mod], w=[md])
        K.V(lambda e: e.tensor_copy(out=seg(md, 5), in_=seg(mod, 5)), r=[mod], w=[md])
        K.dma(SP, mod_d[:, :], md.t[:], r=[md])

    def load_bc(S, name, src_row, n):
        b = S.sb(name, [128, n], F32)
        K.dma(SP, b.t[:], src_row.partition_broadcast(128), w=[b])
        return b

    def mod_bc(S, name, ci, i):
        return load_bc(S, name, mod_d[ci:ci + 1, i * 1024:(i + 1) * 1024], 1024)

    def make_ident(S):
        idf = S.sb("idf", [128, 128], F32)
        K.dma(SP, idf.t[:], ident[:, :], w=[idf])
        idb = S.sb("idb", [128, 128], BF16)
        K.V(lambda e: e.tensor_copy(out=idb.t[:], in_=idf.t[:]), r=[idf], w=[idb])
        return idb

    def tile_ci(t):
        return 0 if t < NTS else 1

    with K.scope() as S:
        idb = make_ident(S)
        win = S.sb("win", [128, 8, 2464], BF16)
        for k in range(8):
            K.dma(POOL, win.t[:, k, :], w_in[0, k * 128:(k + 1) * 128, :], w=[win])
        wuq = S.sb("wuq", [128, 2, 768], BF16)
        K.dma(POOL, wuq.t[:], w_uq[0].rearrange("(k p) n -> p k n", p=128), w=[wuq])
        wukv = S.sb("wukv", [128, 1024], BF16)
        K.dma(POOL, wukv.t[:], w_ukv[0], w=[wukv])
        precast = []
        for e_ in range(64):
            gu_v = wall_d[e_][:, 0:4096].rearrange("p (k n) -> p k n", k=8)
            precast.append((gu_v[:, :, 0:256], exp_w_gate[0, e_].rearrange("(k p) n -> p k n", p=128)))
            precast.append((gu_v[:, :, 256:512], exp_w_up[0, e_].rearrange("(k p) n -> p k n", p=128)))
            precast.append((wall_d[e_][:, 4096:6144].rearrange("p (c n) -> p c n", c=2),
                            exp_w_down[0, e_].rearrange("(c p) n -> p c n", p=128)))

        def issue_precast(n):
            for _ in range(n):
                if precast:
                    o_, i_ = precast.pop(0)
                    K.dma(POOL, o_, i_, r=[wall_b], bg=True)
        gm1b = [mod_bc(S, "gm1b%d" % ci, ci, 0) for ci in range(2)]
        sh1b = [mod_bc(S, "sh1b%d" % ci, ci, 1) for ci in range(2)]
        gqb = load_bc(S, "gqb", mla_q_norm_g[0:1, :], 256)
        gkvb = load_bc(S, "gkvb", mla_kv_norm_g[0:1, :], 128)
        qgb = load_bc(S, "qgb", mla_q_gain[0:1, :], 96)
        kgb = load_bc(S, "kgb", mla_k_gain[0:1, :], 96)

        NSL = 2

        def mk(name, shape, dt):
            return [S.sb("%s_s%d" % (name, i), shape, dt) for i in range(NSL)]
        junk = mk("junk", [128, 1024], BF16)
        xt = mk("xt", [128, 1024], F32)
        tmpf = mk("tmpf", [128, 1024], F32)
        hb = mk("hb", [128, 1024], BF16)
        hT = mk("hT", [128, 8, 128], BF16)
        ssq_x = mk("ssq_x", [128, 1], F32)
        ssq_q = mk("ssq_q", [128, 1], F32)
        ssq_kv = mk("ssq_kv", [128, 1], F32)
        cqn = mk("cqn", [128, 256], BF16)
        ckvn = mk("ckvn", [128, 128], F32)
        ckvnb = mk("ckvnb", [128, 128], BF16)
        krf = mk("krf", [128, 32], F32)
        cT3 = mk("cT3", [128, 3, 128], BF16)
        sqf = mk("sqf", [128, 768], F32)
        ssqh = mk("ssqh", [128, 8], F32)
        qn = mk("qn", [128, 8, 96], F32)
        kcat = mk("kcat", [128, 8, 96], F32)
        ra = mk("ra", [128, 8, 64], F32)
        rb = mk("rb", [128, 8, 64], F32)
        qb = mk("qb", [128, 8, 96], BF16)
        qT = mk("qT", [96, 8, 128], BF16)
        kT = mk("kT", [96, 8, 128], BF16)
        vaug = mk("vaug", [128, 8, 65], BF16)
        for i in range(NSL):
            K.V(lambda e: e.memset(vaug[i].t[:], 1.0), w=[vaug[i]])
        rf = mk("rf", [128, 8, 64], F32)
        rqb = mk("rqb", [128, 8, 64], BF16)
        rkb = mk("rkb", [128, 8, 64], BF16)
        rqT = mk("rqT", [64, 8, 128], BF16)
        rkT = mk("rkT", [64, 8, 128], BF16)
        rvb = mk("rvb", [128, 512], BF16)
        rgf = mk("rgf", [128, 512], F32)
        rpq = mk("rpq", [128, 2, 32], F32)
        rpr = mk("rpr", [128, 2, 64], F32)

        tp = [S.ps("tp%d" % i, [128, 8, 128], BF16) for i in range(NSL)]
        zp = [S.ps("zp%d" % i, [128, 512], F32) for i in range(NSL)]
        big = [S.ps("big%d" % i, [128, 1024], F32) for i in range(NSL)]

        ra2 = mk("ra2", [128, 8, 64], F32)
        rb2 = mk("rb2", [128, 8, 64], F32)
        ra3 = mk("ra3", [128, 8, 32], F32)
        rb3 = mk("rb3", [128, 8, 32], F32)
        sqfk = mk("sqfk", [128, 768], F32)
        ssqhk = mk("ssqhk", [128, 8], F32)
        qnk = mk("qnk", [128, 8, 96], F32)
        qbk = mk("qbk", [128, 8, 96], BF16)

        def rope(i, xb, xv, R, tab, alt=False):
            hf = R // 4
            ra_ = {False: ra, True: ra2, 3: ra3}[alt]
            rb_ = {False: rb, True: rb2, 3: rb3}[alt]
            rav = ra_[i].t[:, :, 0:R]
            rbv = rb_[i].t[:, :, 0:R]
            K.V(lambda e: e.tensor_tensor(out=rav, in0=xv, in1=bc(tab.t[:, 0, :], [128, 8, R], 1), op=ALU.mult),
                r=[xb, tab], w=[ra_[i]])
            x5 = xv.rearrange("p h (a f j) -> p h a f j", a=2, f=2)
            r5 = rbv.rearrange("p h (a f j) -> p h a f j", a=2, f=2)
            s5 = tab.t[:, 1, :].rearrange("p (a f j) -> p a f j", a=2, f=2)
            for f in range(2):
                K.V(lambda e: e.tensor_tensor(out=r5[:, :, :, f, :], in0=x5[:, :, :, 1 - f, :],
                                              in1=bc(s5[:, :, f, :], [128, 8, 2, hf], 1), op=ALU.mult),
                    r=[xb, tab], w=[rb_[i]])
            K.V(lambda e: e.tensor_tensor(out=xv, in0=rav, in1=rbv, op=ALU.add), r=[ra_[i], rb_[i]], w=[xb])

        def qk_proc(i, srcb, srcv, gainb, tab, outT, dst_ap, kset=False):
            sqf_ = sqfk if kset else sqf
            ssqh_ = ssqhk if kset else ssqh
            qn_ = qnk if kset else qn
            qb_ = qbk if kset else qb
            K.A(lambda e: e.activation(out=sqf_[i].t[:].rearrange("p (h d) -> p h d", h=8), in_=srcv, func=AF.Square),
                r=[srcb], w=[sqf_[i]])
            K.V(lambda e: e.reduce_sum(out=ssqh_[i].t[:], in_=sqf_[i].t[:].rearrange("p (h d) -> p h d", h=8), axis=AX.X),
                r=[sqf_[i]], w=[ssqh_[i]])
            yield
            rs = rstd_of(S, ssqh_[i], 96, ("rshk%d" if kset else "rsh%d") % i)
            yield
            K.V(lambda e: e.tensor_tensor(out=qn_[i].t[:], in0=srcv, in1=bc(rs.t[:, :], [128, 8, 96], 2), op=ALU.mult),
                r=[srcb, rs], w=[qn_[i]])
            K.V(lambda e: e.tensor_tensor(out=qn_[i].t[:], in0=qn_[i].t[:], in1=bc(gainb.t[:, :], [128, 8, 96], 1),
                                          op=ALU.mult), r=[qn_[i], gainb], w=[qn_[i]])
            yield
            if tab is not None:
                rope(i, qn_[i], qn_[i].t[:, :, 64:96], 32, tab, alt=(3 if kset else False))
                yield
            K.A(lambda e: e.copy(out=qb_[i].t[:], in_=qn_[i].t[:]), r=[qn_[i]], w=[qb_[i]])
            yield
            t_ = tp[i]
            for h in range(8):
                K.P(lambda e: e.transpose(out=t_.t[0:96, h, :], in_=qb_[i].t[:, h, :], identity=idb.t[:]),
                    r=[qb_[i], idb], w=[t_], mark=(h == 7))
            K.A(lambda e: e.copy(out=outT.t[:], in_=t_.t[0:96, :, :]), r=[t_], w=[outT])
            K.dma(SP, dst_ap, outT.t[:], r=[outT])
            yield

        def kv_part(i, tab, gc):
            kvp = big[i]
            for cg in range(2):
                K.P(lambda e: e.matmul(kvp.t[:, cg * 512:(cg + 1) * 512], lhsT=cT3[i].t[:, 2, :],
                                       rhs=wukv.t[:, cg * 512:(cg + 1) * 512], start=True, stop=True),
                    r=[cT3[i], wukv], w=[kvp], mark=(cg == 1))
            yield
            kv3 = kvp.t[:].rearrange("p (h d) -> p h d", h=8)
            K.A(lambda e: e.copy(out=kcat[i].t[:, :, 0:64], in_=kv3[:, :, 0:64]), r=[kvp], w=[kcat[i]])
            K.V(lambda e: e.tensor_copy(out=kcat[i].t[:, :, 64:96], in_=bc(krf[i].t[:, :], [128, 8, 32], 1)),
                r=[krf[i]], w=[kcat[i]])
            K.A(lambda e: e.copy(out=vaug[i].t[:, :, 0:64], in_=kv3[:, :, 64:128]), r=[kvp], w=[vaug[i]])
            K.dma(SP, v_d[:, :, gc, :].rearrange("h p d -> p h d"), vaug[i].t[:], r=[vaug[i]])
            yield
            yield from qk_proc(i, kcat[i], kcat[i].t[:], kgb, tab, kT[i],
                               kT_d[:, :, gc * 128:(gc + 1) * 128].rearrange("h d t -> d h t"), kset=True)

        def tileA(t, i):
            ci = tile_ci(t)
            sample = t < NTS
            gc = t if sample else 36 + (t - NTS)
            issue_precast(5)
            K.dma(SP, xt[i].t[:], x[t * 128:(t + 1) * 128, :], w=[xt[i]])
            if sample:
                K.dma(SP, rpq[i].t[:], ropeq[t], w=[rpq[i]])
                K.dma(SP, rpr[i].t[:], roper[t], w=[rpr[i]])
            yield
            K.A(lambda e: e.activation(out=junk[i].t[:], in_=xt[i].t[:], func=AF.Square, accum_out=ssq_x[i].t[:]),
                r=[xt[i]], w=[junk[i], ssq_x[i]])
            yield
            rs = rstd_of(S, ssq_x[i], 1024, "rsx%d" % i)
            yield
            K.V(lambda e: e.scalar_tensor_tensor(out=tmpf[i].t[:], in0=xt[i].t[:], scalar=rs.t[:, 0:1],
                                                 in1=gm1b[ci].t[:], op0=ALU.mult, op1=ALU.mult),
                r=[xt[i], rs, gm1b[ci]], w=[tmpf[i]])
            K.G(lambda e: e.tensor_tensor(out=hb[i].t[:], in0=tmpf[i].t[:], in1=sh1b[ci].t[:], op=ALU.add),
                r=[tmpf[i], sh1b[ci]], w=[hb[i]])
            yield
            t_ = tp[i]
            for k in range(8):
                K.P(lambda e: e.transpose(out=t_.t[:, k, :], in_=hb[i].t[:, k * 128:(k + 1) * 128], identity=idb.t[:]),
                    r=[hb[i], idb], w=[t_], mark=(k == 7))
            K.A(lambda e: e.copy(out=hT[i].t[:], in_=t_.t[:]), r=[t_], w=[hT[i]])
            yield

            def zgroup(c0, c1):
                z = zp[i]
                for k in range(8):
                    K.P(lambda e: e.matmul(z.t[:, 0:c1 - c0], lhsT=hT[i].t[:, k, :], rhs=win.t[:, k, c0:c1],
                                           start=(k == 0), stop=(k == 7)), r=[hT[i], win], w=[z], mark=(k == 7))
                return z

            z0 = zgroup(0, 416)
            yield
            K.A(lambda e: e.activation(out=junk[i].t[:, 0:256], in_=z0.t[:, 0:256], func=AF.Square,
                                       accum_out=ssq_q[i].t[:]), r=[z0], w=[junk[i], ssq_q[i]])
            K.A(lambda e: e.activation(out=junk[i].t[:, 256:384], in_=z0.t[:, 256:384], func=AF.Square,
                                       accum_out=ssq_kv[i].t[:]), r=[z0], w=[junk[i], ssq_kv[i]])
            yield
            rq_ = rstd_of(S, ssq_q[i], 256, "rsq%d" % i)
            rkv_ = rstd_of(S, ssq_kv[i], 128, "rskv%d" % i)
            yield
            K.V(lambda e: e.scalar_tensor_tensor(out=cqn[i].t[:], in0=z0.t[:, 0:256], scalar=rq_.t[:, 0:1],
                                                 in1=gqb.t[:], op0=ALU.mult, op1=ALU.mult),
                r=[z0, rq_, gqb], w=[cqn[i]])
            K.V(lambda e: e.scalar_tensor_tensor(out=ckvn[i].t[:], in0=z0.t[:, 256:384], scalar=rkv_.t[:, 0:1],
                                                 in1=gkvb.t[:], op0=ALU.mult, op1=ALU.mult),
                r=[z0, rkv_, gkvb], w=[ckvn[i]])
            K.A(lambda e: e.copy(out=krf[i].t[:], in_=z0.t[:, 384:416]), r=[z0], w=[krf[i]])
            K.A(lambda e: e.copy(out=ckvnb[i].t[:], in_=ckvn[i].t[:]), r=[ckvn[i]], w=[ckvnb[i]])
            if not sample:
                pr = (t - NTS) * 128
                K.dma(SP, ockv[pr:pr + 128, :], ckvn[i].t[:], r=[ckvn[i]])
                K.dma(SP, okr[pr:pr + 128, :], krf[i].t[:], r=[krf[i]])
            yield
            K.P(lambda e: e.transpose(out=t_.t[:, 0, :], in_=cqn[i].t[:, 0:128], identity=idb.t[:]),
                r=[cqn[i], idb], w=[t_], mark=False)
            K.P(lambda e: e.transpose(out=t_.t[:, 1, :], in_=cqn[i].t[:, 128:256], identity=idb.t[:]),
                r=[cqn[i], idb], w=[t_], mark=False)
            K.P(lambda e: e.transpose(out=t_.t[:, 2, :], in_=ckvnb[i].t[:], identity=idb.t[:]),
                r=[ckvnb[i], idb], w=[t_])
            K.A(lambda e: e.copy(out=cT3[i].t[:], in_=t_.t[:, 0:3, :]), r=[t_], w=[cT3[i]])
            yield
            qp = big[i]
            for (c0, c1) in ((0, 512), (512, 768)):
                for k in range(2):
                    K.P(lambda e: e.matmul(qp.t[:, c0:c1], lhsT=cT3[i].t[:, k, :], rhs=wuq.t[:, k, c0:c1],
                                           start=(k == 0), stop=(k == 1)), r=[cT3[i], wuq], w=[qp],
                        mark=(k == 1 and c0 == 512))
            yield
            def chain_m():
                yield from qk_proc(i, qp, qp.t[:, 0:768].rearrange("p (h d) -> p h d", h=8), qgb,
                                   rpq[i] if sample else None, qT[i],
                                   qT_d[:, :, t * 128:(t + 1) * 128].rearrange("h d t -> d h t"))

            def chain_k():
                yield
                yield
                yield from kv_part(i, rpq[i] if sample else None, gc)

            def chain_r():
                for which, (c0, c1) in enumerate(((416, 928), (928, 1440))):
                    z = zgroup(c0, c1)
                    yield
                    sc = 1.0 if which == 0 else 0.125
                    K.A(lambda e: e.activation(out=rf[i].t[:].rearrange("p h d -> p (h d)"), in_=z.t[:], func=AF.Copy,
                                               scale=sc), r=[z], w=[rf[i]])
                    yield
                    if sample:
                        rope(i, rf[i], rf[i].t[:], 64, rpr[i], alt=True)
                        yield
                    ob = rqb[i] if which == 0 else rkb[i]
                    K.A(lambda e: e.copy(out=ob.t[:], in_=rf[i].t[:]), r=[rf[i]], w=[ob])
                    yield
                    t2_ = tp[i]
                    for h in range(8):
                        K.P(lambda e: e.transpose(out=t2_.t[0:64, h, :], in_=ob.t[:, h, :], identity=idb.t[:]),
                            r=[ob, idb], w=[t2_], mark=(h == 7))
                    oT = rqT[i] if which == 0 else rkT[i]
                    K.A(lambda e: e.copy(out=oT.t[:], in_=t2_.t[0:64, :, :]), r=[t2_], w=[oT])
                    K.dma(SP, (rqT_d if which == 0 else rkT_d)[t], oT.t[:], r=[oT])
                    if which == 1:
                        K.dma(SP, rk_d[t], rkb[i].t[:].rearrange("p h d -> p (h d)"), r=[rkb[i]])
                    yield
                z = zgroup(1440, 1952)
                yield
                K.A(lambda e: e.copy(out=rvb[i].t[:], in_=z.t[:]), r=[z], w=[rvb[i]])
                K.dma(SP, rv_d[t], rvb[i].t[:], r=[rvb[i]])
                yield
                z = zgroup(1952, 2464)
                yield
                K.A(lambda e: e.activation(out=rgf[i].t[:], in_=z.t[:], func=AF.Silu), r=[z], w=[rgf[i]])
                K.dma(SP, rg_d[t], rgf[i].t[:], r=[rgf[i]])
                yield

            subs = [chain_m(), chain_k(), chain_r()]
            while subs:
                for g_ in list(subs):
                    try:
                        next(g_)
                    except StopIteration:
                        subs.remove(g_)
                yield

        def tileCtx(j, i):
            K.dma(SP, ckvn[i].t[:], cckv[j * 128:(j + 1) * 128, :], w=[ckvn[i]])
            K.dma(SP, krf[i].t[:], ckr[j * 128:(j + 1) * 128, :], w=[krf[i]])
            yield
            K.A(lambda e: e.copy(out=ckvnb[i].t[:], in_=ckvn[i].t[:]), r=[ckvn[i]], w=[ckvnb[i]])
            yield
            t_ = tp[i]
            K.P(lambda e: e.transpose(out=t_.t[:, 2, :], in_=ckvnb[i].t[:], identity=idb.t[:]),
                r=[ckvnb[i], idb], w=[t_])
            K.A(lambda e: e.copy(out=cT3[i].t[:, 2, :], in_=t_.t[:, 2, :]), r=[t_], w=[cT3[i]])
            yield
            yield from kv_part(i, None, 32 + j)

        jobs = [(tileA, t) for t in range(NT)] + [(tileCtx, j) for j in range(4)]
        run_interleaved(jobs, NSL, stagger=0)
        issue_precast(1000)

    with K.scope() as S:
        idb = make_ident(S)
        lg = []
        for d, src in enumerate((ret_decay_fwd, ret_decay_bwd)):
            raw = load_bc(S, "lgraw%d" % d, src[0:1, :], 8)
            ex = S.sb("lgex%d" % d, [128, 8], F32)
            K.A(lambda e: e.activation(out=ex.t[:], in_=raw.t[:], func=AF.Exp), r=[raw], w=[ex])
            l_ = S.sb("lg%d" % d, [128, 8], F32)
            K.V(lambda e: e.tensor_single_scalar(out=l_.t[:], in_=ex.t[:], scalar=-1.0, op=ALU.mult), r=[ex], w=[l_])
            lg.append(l_)
        ec = S.sb("ec", [128, 4], F32)
        K.dma(SP, ec.t[:], ecols[:, :], w=[ec])
        rngb = load_bc(S, "rngb", ret_norm_g[0:1, :], 64)
        maskT, kdec, qdec, cdec = [], [], [], []
        for d in range(2):
            de = S.sb("de%d" % d, [128, 128], F32)
            K.dma(SP, de.t[:], dexp[d], w=[de])
            dm = S.sb("dm%d" % d, [128, 128], F32)
            K.dma(SP, dm.t[:], dmask[d], w=[dm])
            mt = S.sb("mt%d" % d, [128, 8, 128], F32)
            for h in range(8):
                K.A(lambda e: e.activation(out=mt.t[:, h, :], in_=de.t[:], func=AF.Exp, scale=lg[d].t[:, h:h + 1]),
                    r=[de, lg[d]], w=[mt])
            K.V(lambda e: e.tensor_tensor(out=mt.t[:], in0=mt.t[:], in1=bc(dm.t[:, :], [128, 8, 128], 1),
                                          op=ALU.mult), r=[mt, dm], w=[mt])
            maskT.append(mt)
            kd_ = S.sb("kdec%d" % d, [128, 8], F32)
            K.A(lambda e: e.activation(out=kd_.t[:], in_=lg[d].t[:], func=AF.Exp, scale=ec.t[:, 2 * d:2 * d + 1]),
                r=[lg[d], ec], w=[kd_])
            qd_ = S.sb("qdec%d" % d, [128, 8], F32)
            K.A(lambda e: e.activation(out=qd_.t[:], in_=lg[d].t[:], func=AF.Exp,
                                       scale=ec.t[:, 2 * d + 1:2 * d + 2]), r=[lg[d], ec], w=[qd_])
            cd_ = S.sb("cdec%d" % d, [128, 8], F32)
            K.A(lambda e: e.activation(out=cd_.t[:], in_=lg[d].t[:], func=AF.Exp, scale=128.0), r=[lg[d]], w=[cd_])
            kdec.append(kd_)
            qdec.append(qd_)
            cdec.append(cd_)

        OF = S.sb("OF", [128, NTS, 512], F32)
        NRB = 3
        rqTc = [S.sb("rqTc%d" % i, [64, 8, 128], BF16) for i in range(NRB)]
        rkTc = [S.sb("rkTc%d" % i, [64, 8, 128], BF16) for i in range(NRB)]
        rkc = [S.sb("rkc%d" % i, [128, 8, 64], BF16) for i in range(NRB)]
        rvc = [S.sb("rvc%d" % i, [128, 8, 64], BF16) for i in range(NRB)]
        rgc = [S.sb("rgc%d" % i, [128, 512], F32) for i in range(NRB)]
        STm = [S.sb("STm%d" % i, [128, 8, 128], BF16) for i in range(2)]
        kdb = [S.sb("kdb%d" % i, [128, 8, 64], BF16) for i in range(2)]
        t1 = S.sb("t1", [128, 8, 64], F32)
        ob_ = S.sb("ob_", [128, 8, 64], F32)
        Sst = S.sb("Sst", [64, 8, 64], F32)
        Sbf = S.sb("Sbf", [64, 8, 64], BF16)
        sq2 = S.sb("sq2", [128, 512], F32)
        ssq2 = S.sb("ssq2", [128, 8], F32)
        mixr = S.sb("mixr", [128, 512], BF16)
        mT = [S.sb("mTr%d" % i, [128, 4, 128], BF16) for i in range(2)]
        STp = [S.ps("STp%d" % i, [128, 8, 128], F32) for i in range(2)]
        INp = S.ps("INp", [128, 8, 64], F32)
        CRp = S.ps("CRp", [128, 8, 64], F32)
        KVp = S.ps("KVp", [64, 8, 64], F32)
        tpr = S.ps("tpr", [128, 4, 128], BF16)

        seqs = [(0, NTS, None)] + [(NTS + 2 * p, 2, p) for p in range(4)]
        units = []
        for (t0, n, pidx) in seqs:
            for d in range(2):
                order = list(range(n)) if d == 0 else list(range(n - 1, -1, -1))
                for k_, c in enumerate(order):
                    units.append(dict(t=t0 + c, c=c, d=d, pidx=pidx, first=(k_ == 0), last=(k_ == n - 1)))

        def pre_r(u):
            U = units[u]
            t, d = U["t"], U["d"]
            i = u % NRB
            K.dma(SP, rqTc[i].t[:], rqT_d[t], w=[rqTc[i]])
            K.dma(SP, rkTc[i].t[:], rkT_d[t], w=[rkTc[i]])
            K.dma(SP, rkc[i].t[:].rearrange("p h d -> p (h d)"), rk_d[t], w=[rkc[i]])
            K.dma(SP, rvc[i].t[:].rearrange("p h d -> p (h d)"), rv_d[t], w=[rvc[i]])
            if d == 1:
                K.dma(SP, rgc[i].t[:], rg_d[t], w=[rgc[i]])
            sp_ = STp[u % 2]
            for h in range(8):
                K.P(lambda e: e.matmul(sp_.t[:, h, :], lhsT=rkTc[i].t[:, h, :], rhs=rqTc[i].t[:, h, :],
                                       start=True, stop=True), r=[rkTc[i], rqTc[i]], w=[sp_], mark=(h == 7))
            K.V(lambda e: e.tensor_tensor(out=STm[u % 2].t[:], in0=sp_.t[:], in1=maskT[d].t[:], op=ALU.mult),
                r=[sp_, maskT[d]], w=[STm[u % 2]])
            K.V(lambda e: e.tensor_tensor(out=kdb[u % 2].t[:], in0=rkc[i].t[:], in1=bc(kdec[d].t[:, :], [128, 8, 64], 2),
                                          op=ALU.mult), r=[rkc[i], kdec[d]], w=[kdb[u % 2]])

        def main_r(u):
            U = units[u]
            t, c, d, pidx = U["t"], U["c"], U["d"], U["pidx"]
            i = u % NRB
            sm = STm[u % 2]
            kd_ = kdb[u % 2]
            if U["first"]:
                if pidx is None:
                    K.dma(SP, Sst.t[:], (sf_in if d == 0 else sb_in).rearrange("h d e -> d h e"), w=[Sst])
                else:
                    K.V(lambda e: e.memset(Sst.t[:], 0.0), w=[Sst])
                K.A(lambda e: e.copy(out=Sbf.t[:], in_=Sst.t[:]), r=[Sst], w=[Sbf])
            for h in range(8):
                K.P(lambda e: e.matmul(INp.t[:, h, :], lhsT=sm.t[:, h, :], rhs=rvc[i].t[:, h, :],
                                       start=True, stop=True), r=[sm, rvc[i]], w=[INp], mark=(h == 7))
            for h in range(8):
                K.P(lambda e: e.matmul(KVp.t[:, h, :], lhsT=kd_.t[:, h, :], rhs=rvc[i].t[:, h, :],
                                       start=True, stop=True), r=[kd_, rvc[i]], w=[KVp], mark=(h == 7))
            for h in range(8):
                K.P(lambda e: e.matmul(CRp.t[:, h, :], lhsT=rqTc[i].t[:, h, :], rhs=Sbf.t[:, h, :],
                                       start=True, stop=True), r=[rqTc[i], Sbf], w=[CRp], mark=(h == 7))
            K.V(lambda e: e.tensor_tensor(out=Sst.t[:], in0=Sst.t[:], in1=bc(cdec[d].t[0:64, :], [64, 8, 64], 2),
                                          op=ALU.mult), r=[Sst, cdec[d]], w=[Sst])
            K.V(lambda e: e.tensor_tensor(out=t1.t[:], in0=CRp.t[:], in1=bc(qdec[d].t[:, :], [128, 8, 64], 2),
                                          op=ALU.mult), r=[CRp, qdec[d]], w=[t1])
            K.V(lambda e: e.tensor_tensor(out=Sst.t[:], in0=Sst.t[:], in1=KVp.t[:], op=ALU.add),
                r=[Sst, KVp], w=[Sst])
            K.A(lambda e: e.copy(out=Sbf.t[:], in_=Sst.t[:]), r=[Sst], w=[Sbf])
            ofv = OF.t[:, c, :].rearrange("p (h d) -> p h d", h=8)
            if d == 0:
                K.V(lambda e: e.tensor_tensor(out=ofv, in0=t1.t[:], in1=INp.t[:], op=ALU.add),
                    r=[t1, INp], w=[OF])
            else:
                K.V(lambda e: e.tensor_tensor(out=ob_.t[:], in0=t1.t[:], in1=INp.t[:], op=ALU.add),
                    r=[t1, INp], w=[ob_])
                K.V(lambda e: e.tensor_tensor(out=ob_.t[:], in0=ob_.t[:], in1=ofv, op=ALU.add),
                    r=[ob_, OF], w=[ob_])
                o2 = ob_.t[:].rearrange("p h d -> p (h d)")
                K.A(lambda e: e.activation(out=sq2.t[:], in_=o2, func=AF.Square), r=[ob_], w=[sq2])
                K.V(lambda e: e.reduce_sum(out=ssq2.t[:], in_=sq2.t[:].rearrange("p (h d) -> p h d", h=8),
                                           axis=AX.X), r=[sq2], w=[ssq2])
                rs = rstd_of(S, ssq2, 64, "rso")
                K.V(lambda e: e.tensor_tensor(out=ob_.t[:], in0=ob_.t[:], in1=bc(rs.t[:, :], [128, 8, 64], 2),
                                              op=ALU.mult), r=[ob_, rs], w=[ob_])
                K.V(lambda e: e.tensor_tensor(out=ob_.t[:], in0=ob_.t[:], in1=bc(rngb.t[:, :], [128, 8, 64], 1),
                                              op=ALU.mult), r=[ob_, rngb], w=[ob_])
                K.V(lambda e: e.tensor_tensor(out=mixr.t[:], in0=o2, in1=rgc[i].t[:], op=ALU.mult),
                    r=[ob_, rgc[i]], w=[mixr])
                for j in range(4):
                    K.P(lambda e: e.transpose(out=tpr.t[:, j, :], in_=mixr.t[:, j * 128:(j + 1) * 128],
                                              identity=idb.t[:]), r=[mixr, idb], w=[tpr], mark=(j == 3))
                K.A(lambda e: e.copy(out=mT[u % 2].t[:], in_=tpr.t[:]), r=[tpr], w=[mT[u % 2]])
                K.dma(SP, mixT_d[:, 4:8, t * 128:(t + 1) * 128], mT[u % 2].t[:], r=[mT[u % 2]])
            if U["last"] and pidx is not None:
                K.dma(SP, (orf if d == 0 else orb)[pidx].rearrange("h d e -> d h e"), Sst.t[:], r=[Sst])

        pre_r(0)
        for u in range(len(units)):
            if u + 1 < len(units):
                pre_r(u + 1)
            main_r(u)

    with K.scope() as S:
        onesf = S.sb("onesf", [128, 64], F32)
        K.V(lambda e: e.memset(onesf.t[:], 1.0), w=[onesf])
        kTh = [S.sb("kTh%d" % i, [96, 36 * 128], BF16) for i in range(2)]
        vh = [S.sb("vh%d" % i, [128, 36, 65], BF16) for i in range(2)]
        qTh = [S.sb("qTh%d" % i, [96, 4096], BF16) for i in range(2)]
        pt = [S.sb("pt%d" % i, [128, 2, 512], BF16) for i in range(3)]
        rcb = S.sb("rcb", [128, 512], F32)
        bcs = S.sb("bcs", [64, 512], F32)
        bcr = S.sb("bcr", [64, 512], F32)
        ot = [S.sb("ot%d" % i, [64, 512], BF16) for i in range(2)]
        spp = [S.ps("spp%d" % i, [128, 2, 512], F32) for i in range(3)]
        accp = [S.ps("accp%d" % i, [65, 512], F32) for i in range(2)]
        scale = float(96 ** -0.5)
        seqs = [(0, 4096, 0, 36, 512)] + [(4096 + 256 * p, 256, 36 + 2 * p, 2, 256) for p in range(4)]
        hc = 0
        gcnt = 0
        slot_ctr = [0]
        pending_tail = [None]

        def alloc_slot():
            slot_ctr[0] += 1
            return slot_ctr[0] % 3

        for (c0, L, kc0, nkc, QG) in seqs:
            for h in range(8):
                i = hc % 2
                hc += 1
                K.dma(SP, kTh[i].t[:, 0:nkc * 128], kT_d[h, :, kc0 * 128:(kc0 + nkc) * 128], w=[kTh[i]])
                K.dma(SP, vh[i].t[:, 0:nkc, :], v_d[h, :, kc0:kc0 + nkc, :], w=[vh[i]])
                K.dma(SP, qTh[i].t[:, 0:L], qT_d[h, :, c0:c0 + L], w=[qTh[i]])
                nu = nkc // 2
                for qg in range(L // QG):
                    acc = accp[gcnt % 2]
                    oo = ot[gcnt % 2]
                    gcnt += 1
                    uslot = {}

                    def emit_s(u):
                        j = alloc_slot()
                        uslot[u] = j
                        for c in range(2):
                            kc = 2 * u + c
                            K.P(lambda e: e.matmul(spp[j].t[:, c, 0:QG], lhsT=kTh[i].t[:, kc * 128:(kc + 1) * 128],
                                                   rhs=qTh[i].t[:, qg * QG:(qg + 1) * QG], start=True, stop=True),
                                r=[kTh[i], qTh[i]], w=[spp[j]], mark=(c == 1))
                        K.A(lambda e: e.activation(out=pt[j].t[:, :, 0:QG], in_=spp[j].t[:, :, 0:QG], func=AF.Exp,
                                                   scale=scale), r=[spp[j]], w=[pt[j]])

                    def emit_pv(u):
                        j = uslot[u]
                        for c in range(2):
                            kc = 2 * u + c
                            K.P(lambda e: e.matmul(acc.t[:, 0:QG], lhsT=vh[i].t[:, kc, :], rhs=pt[j].t[:, c, 0:QG],
                                                   start=(kc == 0), stop=(kc == nkc - 1)),
                                r=[vh[i], pt[j]], w=[acc], mark=(kc == nkc - 1))

                    emit_s(0)
                    if nu > 1:
                        emit_s(1)
                    for u in range(nu):
                        if u + 2 < nu:
                            emit_s(u + 2)
                        emit_pv(u)
                        if u == min(1, nu - 1) and pending_tail[0] is not None:
                            pending_tail[0](uslot[u])
                            pending_tail[0] = None

                    def tail(j, acc=acc, oo=oo, QG=QG, h=h, col=c0 + qg * QG):
                        bcp = spp[j]
                        K.A(lambda e: e.copy(out=rcb.t[64:65, 0:QG], in_=acc.t[64:65, 0:QG]), r=[acc], w=[rcb])
                        K.P(lambda e: e.matmul(bcp.t[0:64, 0, 0:QG], lhsT=onesf.t[64:65, 0:64], rhs=rcb.t[64:65, 0:QG],
                                               start=True, stop=True), r=[onesf, rcb], w=[bcp])
                        K.A(lambda e: e.copy(out=bcs.t[:, 0:QG], in_=bcp.t[0:64, 0, 0:QG]), r=[bcp], w=[bcs])
                        K.V(lambda e: e.reciprocal(out=bcr.t[:, 0:QG], in_=bcs.t[:, 0:QG]), r=[bcs], w=[bcr])
                        K.V(lambda e: e.tensor_tensor(out=oo.t[:, 0:QG], in0=acc.t[0:64, 0:QG], in1=bcr.t[:, 0:QG],
                                                      op=ALU.mult), r=[acc, bcr], w=[oo])
                        K.dma(SP, mixT_d[(h % 2) * 64:(h % 2) * 64 + 64, h // 2, col:col + QG], oo.t[:, 0:QG], r=[oo])
                    assert pending_tail[0] is None
                    pending_tail[0] = tail
                    last_slot = uslot[nu - 1]
        if pending_tail[0] is not None:
            pending_tail[0](last_slot)
            pending_tail[0] = None

    with K.scope() as S:
        idb = make_ident(S)
        idf2 = S.sb("idf2", [128, 128], F32)
        K.dma(SP, idf2.t[:], ident[:, :], w=[idf2])
        wo = S.sb("wo", [128, 8, 1024], BF16)
        for k in range(8):
            K.dma(POOL, wo.t[:, k, :], w_o[0, k * 128:(k + 1) * 128, :], w=[wo])
        rwb = S.sb("rwb", [128, 8, 64], BF16)
        K.dma(POOL, rwb.t[:], router_w[0].rearrange("(k p) n -> p k n", p=128), w=[rwb])
        rbb = load_bc(S, "rbb", router_bias[0:1, :], 64)
        g1b = [mod_bc(S, "g1b%d" % ci, ci, 2) for ci in range(2)]
        gm2b = [mod_bc(S, "gm2b%d" % ci, ci, 3) for ci in range(2)]
        sh2b = [mod_bc(S, "sh2b%d" % ci, ci, 4) for ci in range(2)]
        ltf = S.sb("ltf", [128, 128], F32)
        K.dma(SP, ltf.t[:], ltri_c[:, :], w=[ltf])
        ltb = S.sb("ltb", [128, 128], BF16)
        K.V(lambda e: e.tensor_copy(out=ltb.t[:], in_=ltf.t[:]), r=[ltf], w=[ltb])
        oneb = S.sb("oneb", [128, 128], BF16)
        K.V(lambda e: e.memset(oneb.t[:], 1.0), w=[oneb])
        iof = S.sb("iof", [128, 64], F32)
        K.dma(SP, iof.t[:], iota_c[:, :], w=[iof])
        NSL = 2

        def mk(name, shape, dt):
            return [S.sb("%s_c%d" % (name, i), shape, dt) for i in range(NSL)]
        xt = mk("xc", [128, 1024], F32)
        mTl = mk("mTl", [128, 8, 128], BF16)
        x1t = mk("x1t", [128, 1024], F32)
        tmpf = mk("tmpc", [128, 1024], F32)
        junk = mk("junkc", [128, 1024], BF16)
        ssq_x = mk("ssqc", [128, 1], F32)
        h2b = mk("h2b", [128, 1024], BF16)
        h2T = mk("h2T", [128, 8, 128], BF16)
        scs = mk("scs", [128, 64], F32)
        msk = mk("msk", [128, 64], F32)
        mskb = mk("mskb", [128, 64], BF16)
        gmv = mk("gmv", [128, 64], F32)
        den = mk("den", [128, 1], F32)
        rden = mk("rden", [128, 1], F32)
        sel_all = S.sb("sel_all", [128, NT, 64], F32)
        m8_all = S.sb("m8_all", [128, NT, 8], F32)
        gts_all = S.sb("gts_all", [128, NT, 64], F32)
        rank_all = S.sb("rank_all", [128, NT, 64], F32)
        carry = S.sb("carry", [128, 64], F32)
        K.V(lambda e: e.memset(carry.t[:], 0.0), w=[carry])
        op_ = [S.ps("op_%d" % i, [128, 1024], F32) for i in range(NSL)]
        tpc_ = [S.ps("tpc_%d" % i, [128, 8, 128], BF16) for i in range(NSL)]
        sml = [S.ps("sml%d" % i, [128, 3, 64], F32) for i in range(NSL)]

        def tileC(t, i):
            ci = tile_ci(t)
            K.dma(SP, xt[i].t[:], x[t * 128:(t + 1) * 128, :], w=[xt[i]])
            K.dma(SP, mTl[i].t[:], mixT_d[:, :, t * 128:(t + 1) * 128], w=[mTl[i]])
            yield
            for half in range(2):
                for k in range(8):
                    K.P(lambda e: e.matmul(op_[i].t[:, half * 512:(half + 1) * 512], lhsT=mTl[i].t[:, k, :],
                                           rhs=wo.t[:, k, half * 512:(half + 1) * 512], start=(k == 0), stop=(k == 7)),
                        r=[mTl[i], wo], w=[op_[i]], mark=(k == 7 and half == 1))
            yield
            K.V(lambda e: e.tensor_tensor(out=tmpf[i].t[:], in0=op_[i].t[:], in1=g1b[ci].t[:], op=ALU.mult),
                r=[op_[i], g1b[ci]], w=[tmpf[i]])
            yield
            K.G(lambda e: e.tensor_tensor(out=x1t[i].t[:], in0=tmpf[i].t[:], in1=xt[i].t[:], op=ALU.add),
                r=[tmpf[i], xt[i]], w=[x1t[i]])
            K.dma(SP, x1_d[t * 128:(t + 1) * 128, :], x1t[i].t[:], r=[x1t[i]])
            yield
            K.A(lambda e: e.activation(out=junk[i].t[:], in_=x1t[i].t[:], func=AF.Square, accum_out=ssq_x[i].t[:]),
                r=[x1t[i]], w=[junk[i], ssq_x[i]])
            yield
            rs = rstd_of(S, ssq_x[i], 1024, "rsc%d" % i)
            yield
            K.V(lambda e: e.scalar_tensor_tensor(out=tmpf[i].t[:], in0=x1t[i].t[:], scalar=rs.t[:, 0:1],
                                                 in1=gm2b[ci].t[:], op0=ALU.mult, op1=ALU.mult),
                r=[x1t[i], rs, gm2b[ci]], w=[tmpf[i]])
            yield
            K.G(lambda e: e.tensor_tensor(out=h2b[i].t[:], in0=tmpf[i].t[:], in1=sh2b[ci].t[:], op=ALU.add),
                r=[tmpf[i], sh2b[ci]], w=[h2b[i]])
            K.dma(SP, h2_d[t * 128:(t + 1) * 128, :], h2b[i].t[:], r=[h2b[i]])
            yield
            for k in range(8):
                K.P(lambda e: e.transpose(out=tpc_[i].t[:, k, :], in_=h2b[i].t[:, k * 128:(k + 1) * 128],
                                          identity=idb.t[:]), r=[h2b[i], idb], w=[tpc_[i]], mark=(k == 7))
            K.A(lambda e: e.copy(out=h2T[i].t[:], in_=tpc_[i].t[:]), r=[tpc_[i]], w=[h2T[i]])
            yield
            rlp = sml[i].t[:, 0, :]
            for k in range(8):
                K.P(lambda e: e.matmul(rlp, lhsT=h2T[i].t[:, k, :], rhs=rwb.t[:, k, :], start=(k == 0),
                                       stop=(k == 7)), r=[h2T[i], rwb], w=[sml[i]], mark=(k == 7))
            yield
            K.A(lambda e: e.activation(out=scs[i].t[:], in_=rlp, func=AF.Sigmoid), r=[sml[i]], w=[scs[i]])
            yield
            selv = sel_all.t[:, t, :]
            K.V(lambda e: e.tensor_tensor(out=selv, in0=scs[i].t[:], in1=rbb.t[:], op=ALU.add),
                r=[scs[i], rbb], w=[sel_all])
            K.V(lambda e: e.max(out=m8_all.t[:, t, :], in_=selv), r=[sel_all], w=[m8_all])
            K.V(lambda e: e.tensor_single_scalar(out=msk[i].t[:], in_=selv, scalar=m8_all.t[:, t, 5:6], op=ALU.is_ge),
                r=[sel_all, m8_all], w=[msk[i]])
            yield
            K.V(lambda e: e.tensor_tensor(out=gmv[i].t[:], in0=msk[i].t[:], in1=scs[i].t[:], op=ALU.mult),
                r=[msk[i], scs[i]], w=[gmv[i]])
            K.V(lambda e: e.reduce_sum(out=den[i].t[:], in_=gmv[i].t[:], axis=AX.X), r=[gmv[i]], w=[den[i]])
            K.A(lambda e: e.copy(out=mskb[i].t[:], in_=msk[i].t[:]), r=[msk[i]], w=[mskb[i]])
            yield
            K.V(lambda e: e.reciprocal(out=rden[i].t[:], in_=den[i].t[:]), r=[den[i]], w=[rden[i]])
            K.P(lambda e: e.matmul(sml[i].t[:, 1, :], lhsT=ltb.t[:], rhs=mskb[i].t[:], start=True, stop=True),
                r=[ltb, mskb[i]], w=[sml[i]], mark=False)
            K.P(lambda e: e.matmul(sml[i].t[:, 2, :], lhsT=oneb.t[:], rhs=mskb[i].t[:], start=True, stop=True),
                r=[oneb, mskb[i]], w=[sml[i]])
            yield
            K.V(lambda e: e.tensor_scalar(out=gts_all.t[:, t, :], in0=gmv[i].t[:], scalar1=rden[i].t[:, 0:1],
                                          scalar2=2.5, op0=ALU.mult, op1=ALU.mult), r=[gmv[i], rden[i]], w=[gts_all])
            K.V(lambda e: e.tensor_tensor(out=rank_all.t[:, t, :], in0=sml[i].t[:, 1, :], in1=carry.t[:], op=ALU.add),
                r=[sml[i], carry], w=[rank_all])
            K.V(lambda e: e.tensor_tensor(out=carry.t[:], in0=sml[i].t[:, 2, :], in1=carry.t[:], op=ALU.add),
                r=[sml[i], carry], w=[carry])
            yield

        run_interleaved([(tileC, t) for t in range(NT)], NSL, stagger=0)

        ci32 = S.sb("ci32", [128, 64], I32)
        padf = S.sb("padf", [128, 64], F32)
        K.V(lambda e: e.tensor_single_scalar(out=padf.t[:], in_=carry.t[:], scalar=127.0, op=ALU.add), r=[carry], w=[padf])
        K.V(lambda e: e.tensor_copy(out=ci32.t[:], in_=padf.t[:]), r=[padf], w=[ci32])
        K.V(lambda e: e.tensor_single_scalar(out=ci32.t[:], in_=ci32.t[:], scalar=7, op=ALU.arith_shift_right),
            r=[ci32], w=[ci32])
        K.V(lambda e: e.tensor_single_scalar(out=ci32.t[:], in_=ci32.t[:], scalar=7, op=ALU.logical_shift_left),
            r=[ci32], w=[ci32])
        K.V(lambda e: e.tensor_copy(out=padf.t[:], in_=ci32.t[:]), r=[ci32], w=[padf])
        ptp = op_[0]
        K.P(lambda e: e.transpose(out=ptp.t[0:64, 0:128], in_=padf.t[:], identity=idf2.t[:]), r=[padf, idf2], w=[ptp])
        PTs = S.sb("PTs", [64, 128], F32)
        K.A(lambda e: e.copy(out=PTs.t[:], in_=ptp.t[0:64, 0:128]), r=[ptp], w=[PTs])
        offp = op_[1]
        K.P(lambda e: e.matmul(offp.t[:, 0:64], lhsT=PTs.t[:], rhs=ltf.t[0:64, 0:64], start=True, stop=True),
            r=[PTs, ltf], w=[offp])
        offb = S.sb("offb", [128, 64], F32)
        K.A(lambda e: e.copy(out=offb.t[:], in_=offp.t[:, 0:64]), r=[offp], w=[offb])

        zt = S.sb("zt", [128, NST * 2], I32)
        K.V(lambda e: e.memset(zt.t[:], 0), w=[zt])
        K.dma(SP, sinfo_d.rearrange("(p n) c -> p (n c)", p=128), zt.t[:], r=[zt], w=[sinfo_b])
        pos = S.sb("pos", [128, 64], F32)
        oh = S.sb("oh", [128, 6, 64], F32)
        ohp = S.sb("ohp", [128, 6, 64], F32)
        pos6f = S.sb("pos6f", [128, NT * 6], F32)
        e6f = S.sb("e6f", [128, 6], F32)
        g6a = S.sb("g6a", [128, NT * 6], F32)
        pos6i = S.sb("pos6i", [128, NT * 6], I32)
        tokf = S.sb("tokf", [128, 1], F32)
        infs = [S.sb("infs%d" % i, [128, 6, 2], I32) for i in range(2)]
        hi6i = S.sb("hi6i", [128, 6], I32)
        hi6f = S.sb("hi6f", [128, 6], F32)
        fl6f = S.sb("fl6f", [128, 6], F32)
        fl6i = [S.sb("fl6i%d" % i, [128, 6], I32) for i in range(2)]
        ecl = S.sb("ecl", [128, 4], F32)
        K.dma(SP, ecl.t[:], ecols[:, :], w=[ecl])
        for t in range(NT):
            i = t % 2
            K.V(lambda e: e.tensor_tensor(out=pos.t[:], in0=rank_all.t[:, t, :], in1=offb.t[:], op=ALU.add),
                r=[rank_all, offb], w=[pos])
            K.V(lambda e: e.tensor_tensor(out=oh.t[:], in0=bc(sel_all.t[:, t, :], [128, 6, 64], 1),
                                          in1=bc(m8_all.t[:, t, 0:6], [128, 6, 64], 2), op=ALU.is_equal),
                r=[sel_all, m8_all], w=[oh])
            for (src_ap, srcb, dst_ap, dstb) in (
                    (pos.t[:], pos, pos6f.t[:, t * 6:(t + 1) * 6], pos6f),
                    (gts_all.t[:, t, :], gts_all, g6a.t[:, t * 6:(t + 1) * 6], g6a),
                    (iof.t[:], iof, e6f.t[:], e6f)):
                K.V(lambda e: e.tensor_tensor(out=ohp.t[:], in0=oh.t[:], in1=bc(src_ap, [128, 6, 64], 1), op=ALU.mult),
                    r=[oh, srcb], w=[ohp])
                K.V(lambda e: e.reduce_sum(out=dst_ap, in_=ohp.t[:], axis=AX.X), r=[ohp], w=[dstb])
            K.V(lambda e: e.tensor_copy(out=pos6i.t[:, t * 6:(t + 1) * 6], in_=pos6f.t[:, t * 6:(t + 1) * 6]),
                r=[pos6f], w=[pos6i])
            K.V(lambda e: e.tensor_single_scalar(out=hi6i.t[:], in_=pos6i.t[:, t * 6:(t + 1) * 6], scalar=7,
                                                 op=ALU.arith_shift_right), r=[pos6i], w=[hi6i])
            K.V(lambda e: e.tensor_copy(out=hi6f.t[:], in_=hi6i.t[:]), r=[hi6i], w=[hi6f])
            K.V(lambda e: e.tensor_single_scalar(out=fl6f.t[:], in_=pos6f.t[:, t * 6:(t + 1) * 6], scalar=float(NST),
                                                 op=ALU.mult), r=[pos6f], w=[fl6f])
            K.V(lambda e: e.scalar_tensor_tensor(out=fl6f.t[:], in0=hi6f.t[:], scalar=-float(128 * NST - 1),
                                                 in1=fl6f.t[:], op0=ALU.mult, op1=ALU.add), r=[hi6f, fl6f], w=[fl6f])
            K.V(lambda e: e.tensor_copy(out=fl6i[i].t[:], in_=fl6f.t[:]), r=[fl6f], w=[fl6i[i]])
            K.V(lambda e: e.tensor_single_scalar(out=tokf.t[:], in_=ecl.t[:, 2:3], scalar=float(t * 128), op=ALU.add),
                r=[ecl], w=[tokf])
            K.V(lambda e: e.tensor_copy(out=infs[i].t[:, :, 0], in_=bc(tokf.t[:, 0:1], [128, 6, 1], 1)[:, :, 0]),
                r=[tokf], w=[infs[i]])
            K.V(lambda e: e.tensor_copy(out=infs[i].t[:, :, 1], in_=e6f.t[:]), r=[e6f], w=[infs[i]])
            for j in range(6):
                K.iscatter(sinfo_d[:, :], fl6i[i].t[:, j:j + 1], infs[i].t[:, j, :],
                           r=[infs[i], fl6i[i], sinfo_b])
        K.dma(SP, pos6_d[:, :], pos6i.t[:], r=[pos6i])
        K.dma(SP, g6_d[:, :], g6a.t[:], r=[g6a])
        K.wait_bg()

    with K.scope() as S:
        idb = make_ident(S)
        tpD = [S.ps("ftp%d" % i, [128, 8, 128], BF16) for i in range(2)]
        guD = [S.ps("fgu%d" % i, [128, 512], F32) for i in range(2)]
        htpD = S.ps("fhtp", [128, 2, 128], BF16)
        ypD = S.ps("fyp", [128, 1024], F32)
        xsTD = [S.sb("fxsT%d" % i, [128, 8, 128], BF16) for i in range(3)]
        sgD = [S.sb("fsg%d" % i, [128, 256], F32) for i in range(2)]
        HD = [S.sb("fH%d" % i, [128, 256], BF16) for i in range(2)]
        HTD = [S.sb("fHT%d" % i, [128, 2, 128], BF16) for i in range(2)]
        soD = [S.sb("fso%d" % i, [128, 1024], BF16) for i in range(2)]
        NB = 5
        NW = 4
        xs = [S.sb("xs%d" % i, [128, 1024], BF16) for i in range(NB)]
        Wt = [S.sb("Wt%d" % i, [128, 6144], BF16) for i in range(NW)]
        ecl = S.sb("ecl2", [128, 4], F32)
        K.dma(SP, ecl.t[:], ecols[:, :], w=[ecl])
        Wsh = S.sb("Wsh", [128, 6144], BF16)
        guv = Wsh.t[:, 0:4096].rearrange("p (k n) -> p k n", k=8)
        K.dma(POOL, guv[:, :, 0:256], sh_w_gate[0].rearrange("(k p) n -> p k n", p=128), w=[Wsh])
        K.dma(POOL, guv[:, :, 256:512], sh_w_up[0].rearrange("(k p) n -> p k n", p=128), w=[Wsh])
        K.dma(POOL, Wsh.t[:, 4096:6144].rearrange("p (c n) -> p c n", c=2),
              sh_w_down[0].rearrange("(c p) n -> p c n", p=128), w=[Wsh])
        wall_rows = wall_d.rearrange("e p n -> (e p) n")
        bnd_reg = nc.gpsimd.alloc_register("bnd")
        nc.gpsimd.reg_mov(bnd_reg, 64 * 128 - 1)

        sinf_all = S.sb("sinf_all", [128, NST, 2], I32)
        K.dma(SP, sinf_all.t[:].rearrange("p j c -> p (j c)"), sinfo_d.rearrange("(p j) c -> p (j c)", p=128),
              w=[sinf_all])
        rowi = S.sb("rowi", [1, NST * 2], I32)
        K.dma(SP, rowi.t[:], sinfo_d[0:NST, :].rearrange("(o j) c -> o (j c)", o=1), w=[rowi])
        rowf = S.sb("rowf", [1, NST * 2], F32)
        K.V(lambda e: e.tensor_copy(out=rowf.t[:], in_=rowi.t[:]), r=[rowi], w=[rowf])
        one1 = S.sb("one1", [1, 128], F32)
        K.V(lambda e: e.memset(one1.t[:], 1.0), w=[one1])
        for (c0, c1) in ((0, 512), (512, NST * 2)):
            K.P(lambda e: e.matmul(ypD.t[:, c0:c1], lhsT=one1.t[:], rhs=rowf.t[:, c0:c1], start=True, stop=True),
                r=[one1, rowf], w=[ypD], mark=(c0 == 512))
        e_all = S.sb("e_all", [128, NST], F32)
        K.V(lambda e: e.tensor_copy(out=e_all.t[:], in_=ypD.t[:, 0:NST * 2].rearrange("p (j c) -> p j c", c=2)[:, :, 1]),
            r=[ypD], w=[e_all])
        wf_all = S.sb("wf_all", [128, NST], F32)
        K.V(lambda e: e.scalar_tensor_tensor(out=wf_all.t[:], in0=e_all.t[:], scalar=128.0,
                                             in1=ecl.t[:, 2:3].to_broadcast([128, NST]), op0=ALU.mult, op1=ALU.add),
            r=[e_all, ecl], w=[wf_all])
        eq_all = S.sb("eq_all", [128, NST], F32)
        K.V(lambda e: e.tensor_tensor(out=eq_all.t[:, NW:NST], in0=e_all.t[:, NW:NST], in1=e_all.t[:, 0:NST - NW],
                                      op=ALU.is_equal), r=[e_all], w=[eq_all])
        K.V(lambda e: e.scalar_tensor_tensor(out=wf_all.t[:, NW:NST], in0=eq_all.t[:, NW:NST], scalar=1.0e6,
                                             in1=wf_all.t[:, NW:NST], op0=ALU.mult, op1=ALU.add),
            r=[eq_all, wf_all], w=[wf_all])
        widx_all = S.sb("widx_all", [128, NST], I32)
        K.V(lambda e: e.tensor_copy(out=widx_all.t[:], in_=wf_all.t[:]), r=[wf_all], w=[widx_all])

        NU = NST + NT

        def u_x(u):
            return xs[u % NB]

        def u_w(u):
            return Wt[u % NW] if u < NST else Wsh

        def u_wd(u):
            return u_w(u)

        def u_dst(u):
            return so_d[u * 128:(u + 1) * 128, :]

        def prep_idx(j):
            return

        def prep_x(u):
            if u >= NU:
                return
            i = u % NB
            if u < NST:
                K.igather(xs[i].t[:, :], h2_d[:, :], sinf_all.t[:, u, 0:1], r=[sinf_all], w=[xs[i]])
            else:
                t = u - NST
                K.dma(SP, xs[i].t[:], h2_d[t * 128:(t + 1) * 128, :], w=[xs[i]])

        def prep_w(j):
            if j >= NST:
                return
            K.igather(Wt[j % NW].t[:, :], wall_rows, widx_all.t[:, j:j + 1], r=[widx_all], w=[Wt[j % NW]],
                      bounds=bnd_reg)

        def ph_x(u):
            if u >= NU:
                return
            tp_ = tpD[u % 2]
            xT = xsTD[u % 3]
            x_ = u_x(u)
            for k in range(8):
                K.P(lambda e: e.transpose(out=tp_.t[:, k, :], in_=x_.t[:, k * 128:(k + 1) * 128], identity=idb.t[:]),
                    r=[x_, idb], w=[tp_], mark=(k == 7))
            K.A(lambda e: e.copy(out=xT.t[:], in_=tp_.t[:]), r=[tp_], w=[xT])

        def ph_y(u):
            if u >= NU:
                return
            gu = guD[u % 2]
            xT = xsTD[u % 3]
            Wb = u_w(u)
            for k in range(8):
                K.P(lambda e: e.matmul(gu.t[:], lhsT=xT.t[:, k, :], rhs=Wb.t[:, k * 512:(k + 1) * 512],
                                       start=(k == 0), stop=(k == 7)), r=[xT, Wb], w=[gu], mark=(k == 7))
            K.A(lambda e: e.activation(out=sgD[u % 2].t[:], in_=gu.t[:, 0:256], func=AF.Silu), r=[gu], w=[sgD[u % 2]])
            K.V(lambda e: e.tensor_tensor(out=HD[u % 2].t[:], in0=gu.t[:, 256:512], in1=sgD[u % 2].t[:], op=ALU.mult),
                r=[gu, sgD[u % 2]], w=[HD[u % 2]])

        def ph_z1(u):
            H_ = HD[u % 2]
            for c in range(2):
                K.P(lambda e: e.transpose(out=htpD.t[:, c, :], in_=H_.t[:, c * 128:(c + 1) * 128], identity=idb.t[:]),
                    r=[H_, idb], w=[htpD], mark=(c == 1))
            K.A(lambda e: e.copy(out=HTD[u % 2].t[:], in_=htpD.t[:]), r=[htpD], w=[HTD[u % 2]])

        def ph_z2(u):
            HT = HTD[u % 2]
            Wb = u_wd(u)
            so = soD[u % 2]
            for half in range(2):
                for c in range(2):
                    K.P(lambda e: e.matmul(ypD.t[:, half * 512:(half + 1) * 512], lhsT=HT.t[:, c, :],
                                           rhs=Wb.t[:, 4096 + c * 1024 + half * 512:4096 + c * 1024 + (half + 1) * 512],
                                           start=(c == 0), stop=(c == 1)), r=[HT, Wb], w=[ypD],
                        mark=(c == 1 and half == 1))
            K.V(lambda e: e.tensor_copy(out=so.t[:], in_=ypD.t[:]), r=[ypD], w=[so])
            K.dma(SP, u_dst(u), so.t[:], r=[so])

        for j in range(4):
            prep_idx(j)
        for u in range(3):
            prep_x(u)
        for j in range(3):
            prep_w(j)
        ph_x(0)
        ph_x(1)
        ph_y(0)
        for u in range(NU):
            prep_idx(u + 4)
            prep_x(u + 3)
            prep_w(u + 3)
            ph_x(u + 2)
            ph_z1(u)
            ph_y(u + 1)
            ph_z2(u)

    with K.scope() as S:
        g2b = [mod_bc(S, "g2b%d" % ci, ci, 5) for ci in range(2)]
        p6 = S.sb("p6", [128, NT * 6], I32)
        K.dma(SP, p6.t[:], pos6_d[:, :], w=[p6])
        g6 = S.sb("g6", [128, NT * 6], F32)
        K.dma(SP, g6.t[:], g6_d[:, :], w=[g6])
        gb = [[S.sb("gb%d_%d" % (i, j), [128, 1024], BF16) for j in range(6)] for i in range(2)]
        shd = [S.sb("shd%d" % i, [128, 1024], BF16) for i in range(2)]
        x1l = [S.sb("x1l%d" % i, [128, 1024], F32) for i in range(2)]
        acc = S.sb("acc", [128, 1024], F32)
        yo = [S.sb("yo%d" % i, [128, 1024], F32) for i in range(2)]
        def pre_e(t):
            i = t % 2
            K.dma(SP, shd[i].t[:], so_d[NSLOT + t * 128:NSLOT + (t + 1) * 128, :], w=[shd[i]])
            K.dma(SP, x1l[i].t[:], x1_d[t * 128:(t + 1) * 128, :], w=[x1l[i]])
            for j in range(6):
                K.igather(gb[i][j].t[:, :], so_d[:, :], p6.t[:, t * 6 + j:t * 6 + j + 1], r=[p6], w=[gb[i][j]])

        pre_e(0)
        for t in range(NT):
            i = t % 2
            ci = tile_ci(t)
            if t + 1 < NT:
                pre_e(t + 1)
            prev = shd[i]
            for j in range(6):
                K.V(lambda e: e.scalar_tensor_tensor(out=acc.t[:], in0=gb[i][j].t[:],
                                                     scalar=g6.t[:, t * 6 + j:t * 6 + j + 1], in1=prev.t[:],
                                                     op0=ALU.mult, op1=ALU.add), r=[gb[i][j], g6, prev], w=[acc])
                prev = acc
            K.V(lambda e: e.tensor_tensor(out=acc.t[:], in0=acc.t[:], in1=g2b[ci].t[:], op=ALU.mult),
                r=[acc, g2b[ci]], w=[acc])
            K.V(lambda e: e.tensor_tensor(out=yo[i].t[:], in0=acc.t[:], in1=x1l[i].t[:], op=ALU.add),
                r=[acc, x1l[i]], w=[yo[i]])
            K.dma(SP, y[t * 128:(t + 1) * 128, :], yo[i].t[:], r=[yo[i]])
    return nc


def _consts():
    c = {}
    c["c_ident"] = np.eye(128, dtype=np.float32)
    tok = np.arange(4096)
    row = (tok // 64).astype(np.float64)
    col = (tok % 64).astype(np.float64)

    def tab(p):
        inv = 1.0 / (10000.0 ** (np.arange(p, dtype=np.float64) / p))
        ar = row[:, None] * inv[None, :]
        ac = col[:, None] * inv[None, :]
        cos = np.concatenate([np.cos(ar), np.cos(ar), np.cos(ac), np.cos(ac)], axis=1)
        sin = np.concatenate([-np.sin(ar), np.sin(ar), -np.sin(ac), np.sin(ac)], axis=1)
        t = np.stack([cos, sin], axis=1).astype(np.float32)
        return np.ascontiguousarray(t.reshape(NTS, 128, 2, 4 * p))
    c["c_ropeq"] = tab(8)
    c["c_roper"] = tab(16)
    k = np.arange(128)[:, None]
    cc = np.arange(128)[None, :]
    df = cc - k
    c["c_dexp"] = np.stack([np.where(df >= 0, df, 0), np.where(df < 0, -df, 0)]).astype(np.float32)
    c["c_dmask"] = np.stack([(df >= 0), (df < 0)]).astype(np.float32)
    p = np.arange(128, dtype=np.float32)
    c["c_ecols"] = np.stack([127 - p, p + 1, p, 128 - p], axis=1).astype(np.float32)
    c["c_ltri"] = (np.arange(128)[:, None] < np.arange(128)[None, :]).astype(np.float32)
    c["c_iota"] = np.broadcast_to(np.arange(64, dtype=np.float32)[None, :], (128, 64)).copy()
    return c


_WNAMES = ["w_ada", "b_ada", "norm1_g", "w_in", "mla_q_norm_g", "w_uq", "mla_kv_norm_g", "w_ukv", "mla_q_gain",
           "mla_k_gain", "ret_decay_fwd", "ret_decay_bwd", "ret_norm_g", "w_o", "norm2_g", "router_w", "router_bias",
           "exp_w_gate", "exp_w_up", "exp_w_down", "sh_w_gate", "sh_w_up", "sh_w_down"]


def kernel(**inputs):
    f = lambda a: np.ascontiguousarray(np.asarray(a, dtype=np.float32))
    inp = {k: f(v) for k, v in inputs.items()}
    consts = _consts()
    nc = build_program()
    in_maps = []
    for b in range(8):
        m = {}
        m["x"] = np.ascontiguousarray(np.concatenate(
            [inp["x_sample"][b], inp["x_prompt"][4 * b:4 * b + 4].reshape(1024, 1024)], axis=0))
        m["cckv"] = np.ascontiguousarray(inp["cache_mla_ckv"][b, 0])
        m["ckr"] = np.ascontiguousarray(inp["cache_mla_krope"][b, 0])
        m["sf"] = np.ascontiguousarray(inp["state_ret_fwd"][b, 0])
        m["sb"] = np.ascontiguousarray(inp["state_ret_bwd"][b, 0])
        m["cond"] = np.ascontiguousarray(np.stack([inp["c"][b], inp["c_ctx"]], axis=0))
        for n in _WNAMES:
            m[n] = inp[n]
        m.update(consts)
        in_maps.append(m)
    res = run_bass_kernel_spmd(nc, in_maps, core_ids=list(range(8)))
    R = res.results
    y_sample = np.stack([R[b]["y"][0:4096] for b in range(8)], axis=0)
    y_prompt = np.concatenate([R[b]["y"][4096:].reshape(4, 256, 1024) for b in range(8)], axis=0)
    new_ckv = np.concatenate([R[b]["ockv"].reshape(4, 1, 256, 128) for b in range(8)], axis=0)
    new_kr = np.concatenate([R[b]["okr"].reshape(4, 1, 256, 32) for b in range(8)], axis=0)
    new_rf = np.concatenate([R[b]["orf"].reshape(4, 1, 8, 64, 64) for b in range(8)], axis=0)
    new_rb = np.concatenate([R[b]["orb"].reshape(4, 1, 8, 64, 64) for b in range(8)], axis=0)
    return (y_prompt.astype(np.float32), y_sample.astype(np.float32), new_ckv.astype(np.float32),
            new_kr.astype(np.float32), new_rf.astype(np.float32), new_rb.astype(np.float32))
```
